# Optimizing a Trainium2 kernel written in Bass

```python
import math
import numpy as np
import jax
import jax.numpy as jnp
from jax import lax

D_MODEL = 2048
BATCH = 2
SEQ = 4096
DEPTH = 2

HEAD_DIM = 128
N_MIXERS = 4
HEADS_PER_MIXER = D_MODEL // (N_MIXERS * HEAD_DIM)
MIX_W = HEADS_PER_MIXER * HEAD_DIM
IDX_HEADS = 16
IDX_DIM = 64
DSA_TOPK_MAX = 256
DIFF_DIM = HEAD_DIM // 2
MOBA_BLOCK = 256
MOBA_TOPK = 3
MOBA_Q_CHUNK = 64
DILATED_PATTERNS = ((128, 1), (512, 4), (2048, 16))
Q_BLOCK = 128
ROPE_THETA = 10000.0
N_EXPERTS = 16
N_GROUPS = 4
EXPERTS_PER_GROUP = N_EXPERTS // N_GROUPS
TOP_K = 2
D_FF_EXPERT = D_MODEL // 2
EXPERT_BLOCK = 128
LN_EPS = 1e-5
DEEPNORM_ALPHA = (2 * DEPTH) ** 0.25
DEEPNORM_BETA = (8 * DEPTH) ** -0.25
COL_SIZES = (
    MIX_W, HEAD_DIM, HEAD_DIM,
    IDX_HEADS * IDX_DIM, IDX_DIM, IDX_HEADS,
    MIX_W, MIX_W, MIX_W,
    MIX_W, MIX_W, MIX_W,
    MIX_W, MIX_W, MIX_W,
)
VALUE_COLS = (2, 8, 11, 14)
D_IN = sum(COL_SIZES)

kernel_name = 'hybrid_dsa_diff_moba_dilated_moe'


def _layer_norm(x, g, b):
    xf = x.astype(jnp.float32)
    mu = jnp.mean(xf, axis=-1, keepdims=True)
    var = jnp.mean(jnp.square(xf - mu), axis=-1, keepdims=True)
    return ((xf - mu) * lax.rsqrt(var + LN_EPS) * g + b).astype(x.dtype)


def _rms_norm(x, g):
    xf = x.astype(jnp.float32)
    return (xf * lax.rsqrt(jnp.mean(jnp.square(xf), axis=-1, keepdims=True) + LN_EPS) * g).astype(x.dtype)


def _rope(x, pos):
    d = x.shape[-1]
    inv = jnp.power(ROPE_THETA, -jnp.arange(0, d, 2, dtype=jnp.float32) / d)
    ang = pos.astype(jnp.float32)[..., None] * inv
    cos = jnp.cos(ang)[:, :, None, :]
    sin = jnp.sin(ang)[:, :, None, :]
    xf = x.astype(jnp.float32)
    x1, x2 = xf[..., : d // 2], xf[..., d // 2:]
    return jnp.concatenate([x1 * cos - x2 * sin, x1 * sin + x2 * cos], axis=-1).astype(x.dtype)


def _heads(t, d):
    return t.reshape(t.shape[0], t.shape[1], -1, d)


def _unblock(o):
    o = jnp.moveaxis(o, 0, 1)
    return o.reshape((o.shape[0], -1) + o.shape[3:])


def _dsa_attention(q, k, v, iq, ik, iw):
    B, S, H, Dh = q.shape
    n_keep = min(DSA_TOPK_MAX, S // 4)
    key_pos = jnp.arange(S)
    b_idx = jnp.arange(B)[:, None, None]

    def block(i):
        t0 = i * Q_BLOCK
        qb = lax.dynamic_slice_in_dim(q, t0, Q_BLOCK, axis=1)
        iqb = lax.dynamic_slice_in_dim(iq, t0, Q_BLOCK, axis=1)
        iwb = lax.dynamic_slice_in_dim(iw, t0, Q_BLOCK, axis=1).astype(jnp.float32) * IDX_HEADS ** -0.5
        q_pos = t0 + jnp.arange(Q_BLOCK)
        rel = jax.nn.relu(jnp.einsum('bqhd,bsd->bqhs', iqb, ik).astype(jnp.float32) * IDX_DIM ** -0.5)
        score = jnp.einsum('bqh,bqhs->bqs', iwb, rel)
        score = jnp.where(key_pos[None, None, :] <= q_pos[None, :, None], score, -jnp.inf)
        _, idx = lax.top_k(score, n_keep)
        valid = idx <= q_pos[None, :, None]
        k_sel = k[b_idx, idx]
        v_sel = v[b_idx, idx]
        logits = jnp.einsum('bqhd,bqkd->bhqk', qb, k_sel).astype(jnp.float32) * Dh ** -0.5
        p = jax.nn.softmax(jnp.where(valid[:, None], logits, -jnp.inf), axis=-1).astype(v.dtype)
        return jnp.einsum('bhqk,bqkd->bqhd', p, v_sel)

    return _unblock(lax.map(block, jnp.arange(S // Q_BLOCK)))


def _diff_attention(q, k, v, lam):
    B, S, H, _, dd = q.shape
    key_pos = jnp.arange(S)

    def block(i):
        t0 = i * Q_BLOCK
        qb = lax.dynamic_slice_in_dim(q, t0, Q_BLOCK, axis=1)
        q_pos = t0 + jnp.arange(Q_BLOCK)
        causal = key_pos[None, :] <= q_pos[:, None]
        s = jnp.einsum('bqhcd,bshcd->bhcqs', qb, k).astype(jnp.float32) * dd ** -0.5
        p = jax.nn.softmax(jnp.where(causal, s, -jnp.inf), axis=-1)
        w = p[:, :, 0] - lam * p[:, :, 1]
        return jnp.einsum('bhqs,bshd->bqhd', w.astype(v.dtype), v)

    return _unblock(lax.map(block, jnp.arange(S // Q_BLOCK)))


def _moba_attention(q, k, v):
    B, S, H, Dh = q.shape
    sp = -(-S // MOBA_BLOCK) * MOBA_BLOCK
    n_blk = sp // MOBA_BLOCK
    n_sel = min(MOBA_TOPK, n_blk)
    pad = ((0, 0), (0, sp - S), (0, 0), (0, 0))
    q, k, v = jnp.pad(q, pad), jnp.pad(k, pad), jnp.pad(v, pad)
    kbh = k.reshape(B, n_blk, MOBA_BLOCK, H, Dh).transpose(0, 3, 1, 2, 4)
    vbh = v.reshape(B, n_blk, MOBA_BLOCK, H, Dh).transpose(0, 3, 1, 2, 4)
    k_mean = jnp.mean(kbh.astype(jnp.float32), axis=3).astype(k.dtype)
    b_idx = jnp.arange(B)[:, None, None, None]
    h_idx = jnp.arange(H)[None, None, :, None]
    blk_ids = jnp.arange(n_blk)
    scale = Dh ** -0.5
    n_sel_keys = n_sel * MOBA_BLOCK

    def block(i):
        t0 = i * MOBA_Q_CHUNK
        qb = lax.dynamic_slice_in_dim(q, t0, MOBA_Q_CHUNK, axis=1)
        q_pos = t0 + jnp.arange(MOBA_Q_CHUNK)
        own = t0 // MOBA_BLOCK
        gate = jnp.einsum('bqhd,bhnd->bqhn', qb, k_mean).astype(jnp.float32)
        gate = jnp.where(blk_ids < own, gate, -jnp.inf)
        _, sel = lax.top_k(gate, n_sel)
        sel_ok = sel < own
        k_sel = kbh[b_idx, h_idx, sel]
        v_sel = vbh[b_idx, h_idx, sel]
        k_own = lax.dynamic_index_in_dim(kbh, own, axis=2, keepdims=False)
        v_own = lax.dynamic_index_in_dim(vbh, own, axis=2, keepdims=False)
        own_pos = own * MOBA_BLOCK + jnp.arange(MOBA_BLOCK)
        causal = own_pos[None, :] <= q_pos[:, None]
        s_sel = jnp.einsum('bqhd,bqhnkd->bqhnk', qb, k_sel).astype(jnp.float32) * scale
        s_sel = jnp.where(sel_ok[..., None], s_sel, -jnp.inf).reshape(B, MOBA_Q_CHUNK, H, n_sel_keys)
        s_own = jnp.einsum('bqhd,bhkd->bqhk', qb, k_own).astype(jnp.float32) * scale
        s_own = jnp.where(causal[None, :, None, :], s_own, -jnp.inf)
        p = jax.nn.softmax(jnp.concatenate([s_sel, s_own], axis=-1), axis=-1).astype(v.dtype)
        p_sel = p[..., :n_sel_keys].reshape(B, MOBA_Q_CHUNK, H, n_sel, MOBA_BLOCK)
        p_own = p[..., n_sel_keys:]
        return (jnp.einsum('bqhnk,bqhnkd->bqhd', p_sel, v_sel)
                + jnp.einsum('bqhk,bhkd->bqhd', p_own, v_own))

    return _unblock(lax.map(block, jnp.arange(sp // MOBA_Q_CHUNK)))[:, :S]


def _dilated_branch(q, k, v, window, dilation):
    B, S, H, Dh = q.shape
    band = window // dilation
    n_sub = -(-S // dilation)
    nb = -(-n_sub // band)
    sp = dilation * nb * band

    def to_blocks(a):
        a = jnp.pad(a, ((0, 0), (0, sp - S), (0, 0), (0, 0)))
        a = a.reshape(B, nb * band, dilation, H, Dh).transpose(0, 2, 1, 3, 4)
        return a.reshape(B, dilation, nb, band, H, Dh)

    def with_prev(a):
        prev = jnp.pad(a[:, :, :-1], ((0, 0), (0, 0), (1, 0), (0, 0), (0, 0), (0, 0)))
        return jnp.concatenate([prev, a], axis=3)

    qb = to_blocks(q)
    kk = with_prev(to_blocks(k))
    vv = with_prev(to_blocks(v))
    qi = jnp.arange(band)[:, None]
    ki = jnp.arange(2 * band)[None, :]
    dist = qi + band - ki
    band_ok = (dist >= 0) & (dist <= band)
    mask = band_ok[None] & ((jnp.arange(nb)[:, None, None] > 0) | (ki >= band)[None])
    logits = jnp.einsum('brjqhd,brjkhd->brjhqk', qb, kk).astype(jnp.float32) * Dh ** -0.5
    logits = jnp.where(mask[:, None], logits, -jnp.inf)
    lse = jax.nn.logsumexp(logits, axis=-1, keepdims=True)
    p = jnp.exp(logits - lse).astype(v.dtype)
    o = jnp.einsum('brjhqk,brjkhd->brjqhd', p, vv)
    o = o.reshape(B, dilation, nb * band, H, Dh).transpose(0, 2, 1, 3, 4).reshape(B, sp, H, Dh)[:, :S]
    lse = lse[..., 0].transpose(0, 1, 2, 4, 3).reshape(B, dilation, nb * band, H)
    lse = lse.transpose(0, 2, 1, 3).reshape(B, sp, H)[:, :S]
    return o, lse


def _dilated_attention(q, k, v):
    outs, lses = [], []
    for window, dilation in DILATED_PATTERNS:
        o, lse = _dilated_branch(q, k, v, window, dilation)
        outs.append(o)
        lses.append(lse)
    wts = jax.nn.softmax(jnp.stack(lses, axis=0), axis=0)
    return jnp.einsum('pbsh,pbshd->bshd', wts.astype(v.dtype), jnp.stack(outs, axis=0))


def _moe(x, router_w, router_bias, w_gate, w_up, w_down):
    B, S, D = x.shape
    T = B * S
    xt = x.reshape(T, D)
    aff = jax.nn.sigmoid(jnp.dot(xt, router_w).astype(jnp.float32))
    grouped = (aff + router_bias.astype(jnp.float32)).reshape(T, N_GROUPS, EXPERTS_PER_GROUP)
    group_score = jnp.sum(lax.top_k(grouped, TOP_K)[0], axis=-1)
    g = jnp.argmax(group_score, axis=-1)
    in_group = jnp.take_along_axis(grouped, g[:, None, None], axis=1)[:, 0]
    _, local = lax.top_k(in_group, TOP_K)
    e_idx = g[:, None] * EXPERTS_PER_GROUP + local
    gate = jnp.take_along_axis(aff, e_idx, axis=1)
    gate = gate / jnp.sum(gate, axis=-1, keepdims=True)
    A = T * TOP_K
    e_flat = e_idx.reshape(A)
    tok_flat = jnp.repeat(jnp.arange(T, dtype=jnp.int32), TOP_K)
    order = jnp.argsort(e_flat)
    e_sorted = e_flat[order]
    counts = jnp.bincount(e_flat, length=N_EXPERTS)
    padded = (counts + EXPERT_BLOCK - 1) // EXPERT_BLOCK * EXPERT_BLOCK
    start = jnp.cumsum(counts) - counts
    pend = jnp.cumsum(padded)
    pstart = pend - padded
    dest = pstart[e_sorted] + (jnp.arange(A) - start[e_sorted])
    n_slots = A + N_EXPERTS * EXPERT_BLOCK
    n_blocks = n_slots // EXPERT_BLOCK
    slot_tok = jnp.full((n_slots,), T, jnp.int32).at[dest].set(tok_flat[order])
    slot_gate = jnp.zeros((n_slots,), jnp.float32).at[dest].set(gate.reshape(A)[order])
    block_expert = jnp.minimum(jnp.searchsorted(pend, jnp.arange(n_blocks) * EXPERT_BLOCK, side='right'), N_EXPERTS - 1)
    x_pad = jnp.concatenate([xt, jnp.zeros((1, D), xt.dtype)], axis=0)
    xs = x_pad[slot_tok].reshape(n_blocks, EXPERT_BLOCK, D)

    def expert_block(args):
        xb, e = args
        h = jax.nn.silu(xb @ w_gate[e]) * (xb @ w_up[e])
        return h @ w_down[e]

    ys = lax.map(expert_block, (xs, block_expert)).reshape(n_slots, D)
    ys = (ys * slot_gate[:, None]).astype(xt.dtype)
    out = jnp.zeros((T + 1, D), xt.dtype).at[slot_tok].add(ys)[:T]
    return out.reshape(B, S, D)


def setup_inputs(seed: int = 0) -> dict:
    key = jax.random.key(seed)
    ks = jax.random.split(key, 16)
    f32 = jnp.float32
    col_scale = jnp.asarray(np.concatenate([
        np.full((n,), DEEPNORM_BETA if i in VALUE_COLS else 1.0, np.float32)
        for i, n in enumerate(COL_SIZES)]))
    x = jax.random.normal(ks[0], (BATCH, SEQ, D_MODEL), f32)
    positions = jnp.broadcast_to(jnp.arange(SEQ, dtype=jnp.int32), (BATCH, SEQ))
    w_in = jax.random.normal(ks[1], (DEPTH, D_MODEL, D_IN), f32) * (D_MODEL ** -0.5) * col_scale
    w_out = jax.random.normal(ks[2], (DEPTH, D_MODEL, D_MODEL), f32) * (D_MODEL ** -0.5 * DEEPNORM_BETA)
    diff_lambda = jax.random.normal(ks[3], (DEPTH, 4, DIFF_DIM), f32) * 0.1
    diff_norm_g = 1.0 + 0.02 * jax.random.normal(ks[4], (DEPTH, HEAD_DIM), f32)
    ln_mix_g = 1.0 + 0.02 * jax.random.normal(ks[5], (DEPTH, D_MODEL), f32)
    ln_mix_b = 0.02 * jax.random.normal(ks[6], (DEPTH, D_MODEL), f32)
    router_w = jax.random.normal(ks[7], (D_MODEL, N_EXPERTS), f32) * D_MODEL ** -0.5
    router_bias = 0.01 * jax.random.normal(ks[8], (N_EXPERTS,), f32)
    w_gate = jax.random.normal(ks[9], (DEPTH, N_EXPERTS, D_MODEL, D_FF_EXPERT), f32) * (D_MODEL ** -0.5 * DEEPNORM_BETA)
    w_up = jax.random.normal(ks[10], (DEPTH, N_EXPERTS, D_MODEL, D_FF_EXPERT), f32) * (D_MODEL ** -0.5 * DEEPNORM_BETA)
    w_down = jax.random.normal(ks[11], (DEPTH, N_EXPERTS, D_FF_EXPERT, D_MODEL), f32) * (D_FF_EXPERT ** -0.5 * DEEPNORM_BETA)
    ln_ffn_g = 1.0 + 0.02 * jax.random.normal(ks[12], (DEPTH, D_MODEL), f32)
    ln_ffn_b = 0.02 * jax.random.normal(ks[13], (DEPTH, D_MODEL), f32)
    return {'x': x, 'positions': positions, 'w_in': w_in, 'w_out': w_out,
            'diff_lambda': diff_lambda, 'diff_norm_g': diff_norm_g,
            'ln_mix_g': ln_mix_g, 'ln_mix_b': ln_mix_b,
            'router_w': router_w, 'router_bias': router_bias,
            'w_gate': w_gate, 'w_up': w_up, 'w_down': w_down,
            'ln_ffn_g': ln_ffn_g, 'ln_ffn_b': ln_ffn_b}


def reference(x, positions, w_in, w_out, diff_lambda, diff_norm_g, ln_mix_g, ln_mix_b,
              router_w, router_bias, w_gate, w_up, w_down, ln_ffn_g, ln_ffn_b):
    B, S, _ = x.shape
    H = HEADS_PER_MIXER
    split_at = np.cumsum(COL_SIZES)[:-1].tolist()
    for layer in range(DEPTH):
        proj = jnp.einsum('bsd,dc->bsc', x, w_in[layer])
        (a_q, a_k, a_v, i_q, i_k, i_w, b_q, b_k, b_v,
         c_q, c_k, c_v, d_q, d_k, d_v) = jnp.split(proj, split_at, axis=-1)
        o_a = _dsa_attention(_rope(_heads(a_q, HEAD_DIM), positions),
                             _rope(a_k[:, :, None], positions)[:, :, 0], a_v,
                             _rope(_heads(i_q, IDX_DIM), positions),
                             _rope(i_k[:, :, None], positions)[:, :, 0], i_w)
        lam_init = 0.8 - 0.6 * math.exp(-0.3 * layer)
        lq1, lk1, lq2, lk2 = diff_lambda[layer].astype(jnp.float32)
        lam = jnp.exp(jnp.sum(lq1 * lk1)) - jnp.exp(jnp.sum(lq2 * lk2)) + lam_init
        bq = _rope(_heads(b_q, DIFF_DIM), positions).reshape(B, S, H, 2, DIFF_DIM)
        bk = _rope(_heads(b_k, DIFF_DIM), positions).reshape(B, S, H, 2, DIFF_DIM)
        o_b = _diff_attention(bq, bk, _heads(b_v, HEAD_DIM), lam)
        o_b = (_rms_norm(o_b, diff_norm_g[layer]) * (1.0 - lam_init)).astype(x.dtype)
        o_c = _moba_attention(_rope(_heads(c_q, HEAD_DIM), positions),
                              _rope(_heads(c_k, HEAD_DIM), positions), _heads(c_v, HEAD_DIM))
        o_d = _dilated_attention(_rope(_heads(d_q, HEAD_DIM), positions),
                                 _rope(_heads(d_k, HEAD_DIM), positions), _heads(d_v, HEAD_DIM))
        mix = jnp.concatenate([o_a.reshape(B, S, MIX_W), o_b.reshape(B, S, MIX_W),
                               o_c.reshape(B, S, MIX_W), o_d.reshape(B, S, MIX_W)], axis=-1)
        x = _layer_norm(DEEPNORM_ALPHA * x + mix @ w_out[layer], ln_mix_g[layer], ln_mix_b[layer])
        moe_out = _moe(x, router_w, router_bias, w_gate[layer], w_up[layer], w_down[layer])
        x = _layer_norm(DEEPNORM_ALPHA * x + moe_out, ln_ffn_g[layer], ln_ffn_b[layer])
    return x
```

```python
import numpy as np
import concourse.bass as bass
import concourse.mybir as mybir
from concourse.bass_utils import run_bass_kernel_spmd

F32 = mybir.dt.float32
BF16 = mybir.dt.bfloat16
I32 = mybir.dt.int32
AF = mybir.ActivationFunctionType
ALU = mybir.AluOpType
AX = mybir.AxisListType

COMPUTE = ('tensor', 'scalar', 'vector', 'gpsimd')
DMAQ = ('sync', 'scalar', 'gpsimd')


class _Op:
    __slots__ = ('eng', 'fn', 'reads', 'writes', 'dma', 'need', 'signal', 'val', 'slot', 'cc', 'bar')


class Prog:
    def __init__(self, nc, nslots=4):
        self.nc = nc
        self.ops = []
        self.nslots = nslots

    def add(self, eng, fn, reads=(), writes=(), dma=False, cc=False):
        o = _Op()
        o.cc = cc
        o.bar = False
        o.eng, o.fn, o.reads, o.writes, o.dma = eng, fn, tuple(reads), tuple(writes), dma
        o.need = set()
        o.signal = dma
        o.val = None
        o.slot = None
        self.ops.append(o)
        return o

    def dma(self, eng, out, in_, reads=(), writes=()):
        return self.add(eng, lambda e: e.dma_start(out=out, in_=in_), reads, writes, dma=True)

    def barrier(self, fn):
        o = self.add('vector', fn)
        o.bar = True
        o.signal = True
        return o

    def _assign_slots(self):
        dcount = {q: 0 for q in DMAQ}
        for o in self.ops:
            if o.dma:
                if o.cc:
                    o.slot = ('cc', 0)
                else:
                    j = dcount[o.eng]
                    dcount[o.eng] += 1
                    o.slot = (o.eng, j % self.nslots)

    def _barriers(self):
        ops = self.ops
        engs = ('sync', 'scalar', 'vector', 'gpsimd', 'tensor')
        bars = [i for i, o in enumerate(ops) if o.bar]
        for bi in bars:
            b = ops[bi]
            lastc = {}
            lasts = {}
            for i in range(bi):
                o = ops[i]
                if o.dma:
                    lasts[o.slot] = i
                else:
                    lastc[o.eng] = i
            for i in list(lastc.values()) + list(lasts.values()):
                b.need.add(i)
            seen = set()
            for i in range(bi + 1, len(ops)):
                o = ops[i]
                if o.eng not in seen:
                    seen.add(o.eng)
                    o.need.add(bi)
                    if len(seen) == len(engs):
                        break

    def _deps(self):
        last_w = {}
        rd = {}
        ops = self.ops
        for i, c in enumerate(ops):
            raw, other = set(), set()
            for k in c.reads:
                if k in last_w:
                    raw.add(last_w[k])
            for k in c.writes:
                if k in last_w:
                    other.add(last_w[k])
                for r in rd.get(k, ()):
                    other.add(r)
            for p in raw | other:
                if p == i:
                    continue
                po = ops[p]
                if po.dma or c.dma:
                    c.need.add(p)
                elif po.eng == c.eng:
                    if c.eng != 'tensor':
                        c.need.add(p)
                else:
                    c.need.add(p)
            for k in c.reads:
                rd.setdefault(k, []).append(i)
            for k in c.writes:
                last_w[k] = i
                rd[k] = []
        self._assign_slots()
        self._barriers()
        for c in ops:
            for p in c.need:
                ops[p].signal = True

    def emit(self, stack):
        nc = self.nc
        self._deps()
        ops = self.ops
        sem = {}
        for e in COMPUTE:
            sem[e] = stack.enter_context(nc.semaphore('s_' + e))
        for q in DMAQ:
            for s in range(self.nslots):
                sem[(q, s)] = stack.enter_context(nc.semaphore('d_%s%d' % (q, s)))
        if any(o.cc for o in ops):
            sem[('cc', 0)] = stack.enter_context(nc.semaphore('s_cc'))
        cnt = {e: 0 for e in COMPUTE}
        dcount = {q: 0 for q in DMAQ}
        slotcnt = {}
        prev_in_slot = {}
        for i, o in enumerate(ops):
            if o.dma:
                s = o.slot
                inc = 1 if o.cc else 16
                if s in prev_in_slot:
                    o.need.add(prev_in_slot[s])
                prev_in_slot[s] = i
                slotcnt[s] = slotcnt.get(s, 0) + inc
                o.val = slotcnt[s]
            elif o.signal:
                cnt[o.eng] += 1
                o.val = cnt[o.eng]
        final_slots = dict(slotcnt)
        block = stack.enter_context(nc.Block())

        def run_engine(ename, e):
            waited = {}
            for o in ops:
                if o.eng != ename:
                    continue
                req = {}
                for p in o.need:
                    po = ops[p]
                    sk = po.slot if po.dma else po.eng
                    if po.val > req.get(sk, 0):
                        req[sk] = po.val
                for sk, v in req.items():
                    if waited.get(sk, 0) >= v:
                        continue
                    e.wait_ge(sem[sk], v)
                    waited[sk] = v
                ins = o.fn(e)
                if o.dma:
                    ins.then_inc(sem[o.slot], 1 if o.cc else 16)
                elif o.signal:
                    ins.then_inc(sem[o.eng], 1)
            if ename == 'sync':
                for sk, v in final_slots.items():
                    if waited.get(sk, 0) < v:
                        e.wait_ge(sem[sk], v)

        @block.sync
        def _(e):
            run_engine('sync', e)

        @block.scalar
        def _(e):
            run_engine('scalar', e)

        @block.vector
        def _(e):
            run_engine('vector', e)

        @block.gpsimd
        def _(e):
            run_engine('gpsimd', e)

        @block.tensor
        def _(e):
            run_engine('tensor', e)


import numpy as np

THETA = 10000.0
OFF = {}
_sizes = (512, 128, 128, 1024, 64, 16, 512, 512, 512, 512, 512, 512, 512, 512, 512)
_names = ('a_q', 'a_k', 'a_v', 'i_q', 'i_k', 'i_w', 'b_q', 'b_k', 'b_v', 'c_q', 'c_k', 'c_v', 'd_q', 'd_k', 'd_v')
_o = 0
for _n, _s in zip(_names, _sizes):
    OFF[_n] = _o
    _o += _s

P64 = np.concatenate([np.arange(0, 32), np.arange(64, 96), np.arange(32, 64), np.arange(96, 128)])
IK2 = np.concatenate([np.arange(0, 32), np.arange(0, 32), np.arange(32, 64), np.arange(32, 64)])


def k1_consts():
    p = np.arange(128)
    c = np.zeros((128, 8), np.float32)
    c[:, 0] = THETA ** (-2.0 * (p % 64) / 128.0)
    c[:, 1] = np.where(p < 64, -1.0, 1.0)
    c[:, 2] = THETA ** (-2.0 * (p % 32) / 64.0)
    c[:, 3] = c[:, 1]
    c[:, 4] = ((p % 64) < 32).astype(np.float32)
    c[:, 5] = 1.0 - c[:, 4]
    return c


def k1_cols(j):
    qcols = np.concatenate([
        OFF['b_q'] + 128 * j + P64, OFF['b_k'] + 128 * j + P64,
        OFF['c_q'] + 128 * j + np.arange(128), OFF['c_k'] + 128 * j + np.arange(128),
        OFF['d_q'] + 128 * j + np.arange(128), OFF['d_k'] + 128 * j + np.arange(128),
        OFF['a_k'] + np.arange(128), OFF['i_k'] + IK2])
    vcols = np.concatenate([OFF['b_v'] + 128 * j + np.arange(128), OFF['c_v'] + 128 * j + np.arange(128),
                            OFF['d_v'] + 128 * j + np.arange(128), OFF['a_v'] + np.arange(128)])
    acols = np.concatenate([OFF['a_q'] + np.arange(512)] + [OFF['i_q'] + 128 * pr + P64 for pr in range(8)])
    iwcols = OFF['i_w'] + np.arange(16)
    return qcols, vcols, acols, iwcols


def own_tokens(j):
    return np.concatenate([np.arange(128 * (4 * c + j), 128 * (4 * c + j) + 128) for c in range(8)])


def k1_inputs(xb, posb, w_in_l, j):
    qcols, vcols, acols, iwcols = k1_cols(j)
    xT = np.ascontiguousarray(xb.T)
    own = own_tokens(j)
    return {
        'xT': xT, 'xTo': np.ascontiguousarray(xT[:, own]),
        'wq': np.ascontiguousarray(w_in_l[:, qcols]), 'wv': np.ascontiguousarray(w_in_l[:, vcols]),
        'wa': np.ascontiguousarray(w_in_l[:, acols]), 'wiw': np.ascontiguousarray(w_in_l[:, iwcols]),
        'pos': np.ascontiguousarray(posb[None, :].astype(np.int32)),
        'poso': np.ascontiguousarray(posb[None, own].astype(np.int32)),
        'cst': k1_consts(),
    }


def k2_consts(j, dl_l, dg_l, layer):
    import ml_dtypes, math
    bf = ml_dtypes.bfloat16
    s = np.arange(128)[:, None]
    t = np.arange(512)[None, :]
    cm = np.stack([(s + 128 * d <= t) for d in range(4)], 1).astype(np.float32)
    dms = []
    for di in range(20):
        d = (128 * di - 384) + t - s
        m = ((d >= 0) & (d <= 128)).astype(np.float32) + ((d >= 0) & (d <= 512) & (d % 4 == 0)) + ((d >= 0) & (d <= 2048) & (d % 16 == 0))
        dms.append(m.astype(np.float32))
    dm = np.stack(dms, 1)
    tq = 128 * j + np.arange(128)[:, None]
    sk = np.arange(512)[None, :]
    negc = np.where(sk > tq, -1.0e30, 0.0).astype(np.float32)
    lam_init = 0.8 - 0.6 * math.exp(-0.3 * layer)
    lc = np.zeros((128, 2), np.float32)
    lc[:, 0] = lam_init
    lc[:, 1] = 1.0 - lam_init
    return {'cmask': cm.astype(bf), 'dmask': dm.astype(bf), 'negc': negc, 'ident': np.eye(128, dtype=np.float32).astype(bf),
            'dl': np.ascontiguousarray(dl_l.reshape(1, 256)), 'dg': np.ascontiguousarray(dg_l.reshape(1, 128)), 'lc': lc}


from contextlib import ExitStack


def _ln_ops(P, z, zkey, outt, outkey, G, Bt, junk, st6, tag):
    P.add('scalar', lambda e: e.activation(out=junk[:], in_=z[:], func=AF.Copy, accum_out=st6[:, 0:1]), reads=[zkey], writes=['junk', 'st0'])
    P.add('scalar', lambda e: e.activation(out=junk[:], in_=z[:], func=AF.Square, accum_out=st6[:, 1:2]), reads=[zkey], writes=['junk', 'st1'])
    P.add('vector', lambda e: e.tensor_scalar(out=st6[:, 2:3], in0=st6[:, 0:1], scalar1=1.0 / D, scalar2=None, op0=ALU.mult), reads=['st0'], writes=['st2'])
    P.add('vector', lambda e: e.tensor_tensor(out=st6[:, 3:4], in0=st6[:, 2:3], in1=st6[:, 2:3], op=ALU.mult), reads=['st2'], writes=['st3'])
    P.add('vector', lambda e: e.scalar_tensor_tensor(out=st6[:, 4:5], in0=st6[:, 1:2], scalar=1.0 / D, in1=st6[:, 3:4], op0=ALU.mult, op1=ALU.subtract), reads=['st1', 'st3'], writes=['st4'])
    P.add('vector', lambda e: e.tensor_scalar(out=st6[:, 4:5], in0=st6[:, 4:5], scalar1=1e-5, scalar2=None, op0=ALU.add), reads=['st4'], writes=['st4'])
    P.add('scalar', lambda e: e.activation(out=st6[:, 5:6], in_=st6[:, 4:5], func=AF.Sqrt), reads=['st4'], writes=['st5'])
    P.add('vector', lambda e: e.reciprocal(out=st6[:, 5:6], in_=st6[:, 5:6]), reads=['st5'], writes=['st5'])
    P.add('vector', lambda e: e.scalar_tensor_tensor(out=st6[:, 6:7], in0=st6[:, 2:3], scalar=-1.0, in1=st6[:, 5:6], op0=ALU.mult, op1=ALU.mult), reads=['st2', 'st5'], writes=['st6'])
    P.add('scalar', lambda e: e.activation(out=outt[:], in_=z[:], func=AF.Identity, scale=st6[:, 5:6], bias=st6[:, 6:7]), reads=[zkey, 'st5', 'st6'], writes=[outkey])
    P.add('gpsimd', lambda e: e.tensor_tensor(out=outt[:], in0=outt[:], in1=G[:], op=ALU.mult), reads=[outkey, 'G'], writes=[outkey])
    P.add('vector', lambda e: e.tensor_tensor(out=outt[:], in0=outt[:], in1=Bt[:], op=ALU.add), reads=[outkey, 'Bt'], writes=[outkey])


PI = float(np.pi)
S = 4096
D = 2048
NEG = -1.0e30
ALPHA = float(4 ** 0.25)
ARENA = 51200
NQT = 8
NAO = 12
QT_VARIANT = [1, 1, 0, 0, 0, 0, 0, 1]
QT_MASKED = [True, False, False, False, False, False, False, False]
AO_VARIANT = [0, 0, 0, 0] + [1] * 8
AO_MASKED = [False] * 4 + [True] * 8
RG = [[0, 1, 2, 3], [4, 5, 6, 7]]


class Ctx:
    def __init__(self, nc, st):
        self.nc = nc
        self.arena = st.enter_context(nc.sbuf_tensor("arena", [128, ARENA], F32))
        self.ps = [st.enter_context(nc.psum_tensor("ps%d" % i, [128, 512], F32)) for i in range(8)]
        self.P = Prog(nc)
        self.off = 0
        self.bar = self.arena[:, ARENA - 8:ARENA]
        self.cnt = {'dq': 0}

    def reset(self):
        self.off = 0

    def sb(self, name, shape, dt=F32):
        n = 1
        for s_ in shape[1:]:
            n *= s_
        esz = 2 if dt == BF16 else 4
        nf = (n * esz + 3) // 4
        nf = (nf + 7) // 8 * 8
        assert self.off + nf <= ARENA - 8, ("arena overflow", name, self.off, nf)
        v = self.arena[:, self.off:self.off + nf]
        self.off += nf
        if dt != F32:
            v = v.bitcast(dt)
        v = v[:, 0:n]
        if len(shape) == 3:
            v = v.rearrange("p (a b) -> p a b", b=shape[2])
        elif len(shape) == 4:
            v = v.rearrange("p (a b c) -> p a b c", b=shape[2], c=shape[3])
        return v

    def dq(self):
        self.cnt['dq'] += 1
        return ('sync', 'gpsimd')[self.cnt['dq'] % 2]

    def barrier(self):
        bar = self.bar
        self.P.barrier(lambda e: e.memset(bar, 0.0))
        self.reset()

    def collectives(self, items):
        self.barrier()
        for (kind, op, src, dst) in items:
            self.P.add('gpsimd', lambda e, kind=kind, op=op, src=src, dst=dst: e.collective_compute(kind, op, replica_groups=RG, ins=[src.opt()], outs=[dst.opt()]),
                       dma=True, cc=True)
        self.barrier()


def stage_k1(C, io, layer1):
    nc, P, ps = C.nc, C.P, C.ps
    sb = C.sb
    dq = C.dq
    wq, wv, wa, wiw, pos, poso, cst = io['wq'], io['wv'], io['wa'], io['wiw'], io['pos'], io['poso'], io['cst']
    qt, vt, aq, iq, iw = io['qt'], io['vt'], io['aq'], io['iq'], io['iw']
    wq_b = sb("wq_b", [128, 16, NQT * 128], BF16)
    wv_b = sb("wv_b", [128, 16, 512], BF16)
    wa_b = sb("wa_b", [128, 16, NAO * 128], BF16)
    wiw_b = sb("wiw_b", [128, 16, 16], BF16)
    wst = [sb("wst%d" % i, [128, 16, 128], F32) for i in range(2)]
    xs = [sb("xs%d" % i, [128, 8, 512], F32) for i in range(2)]
    xb = sb("xb", [128, 16, 512], BF16)
    xob = sb("xob", [128, 16, 128], BF16)
    cs = sb("cs", [128, 8], F32)
    posi = sb("posi", [128, 512], I32)
    posf = sb("posf", [128, 512], F32)
    ang = sb("ang", [128, 512], F32)
    ki = sb("ki", [128, 512], I32)
    kf = sb("kf", [128, 512], F32)
    tabC = [sb("tabC%d" % v, [128, 512], F32) for v in range(2)]
    tabS = [sb("tabS%d" % v, [128, 512], F32) for v in range(2)]
    t1 = [sb("t1_%d" % i, [128, 512], F32) for i in range(2)]
    t2 = [sb("t2_%d" % i, [128, 512], F32) for i in range(2)]
    rot = [sb("rot%d" % i, [128, 512], F32) for i in range(2)]
    ob = [sb("ob%d" % i, [128, 512], BF16) for i in range(4)]
    iwt = sb("iwt", [128, 16], F32)
    cnt = {'ps': 0, 'ob': 0, 'tt': 0, 'cv': 0}

    P.dma('sync', cs, cst[:, :], writes=['cs'])

    def load_w(src, ncols, dst, dname):
        src_v = src.rearrange("(kc p) c -> p kc c", p=128)
        for c0 in range(0, ncols, 128):
            cw = min(128, ncols - c0)
            i = cnt['cv'] % 2
            cnt['cv'] += 1
            w_ = wst[i]
            P.dma(dq(), w_[:, :, 0:cw], src_v[:, :, c0:c0 + cw], writes=['wst%d' % i])
            eng = ('gpsimd', 'vector')[i]
            P.add(eng, lambda e, w_=w_, c0=c0, cw=cw, dst=dst: e.tensor_copy(out=dst[:, :, c0:c0 + cw], in_=w_[:, :, 0:cw]),
                  reads=['wst%d' % i], writes=[dname])
    load_w(wq, NQT * 128, wq_b, 'wq_b')
    load_w(wv, 512, wv_b, 'wv_b')
    load_w(wa, NAO * 128, wa_b, 'wa_b')
    load_w(wiw, 16, wiw_b, 'wiw_b')

    def tables(pos_ap, n):
        P.dma('sync', posi[:, 0:n], pos_ap.partition_broadcast(128), writes=['posi'])
        P.add('vector', lambda e: e.tensor_copy(out=posf[:, 0:n], in_=posi[:, 0:n]), reads=['posi'], writes=['posf'])
        for v in range(2):
            inv = cs[:, 2 * v:2 * v + 1]
            for which, off, tab, tkey in (('S', 0.0, tabS[v], 'tabS%d' % v), ('C', PI / 2, tabC[v], 'tabC%d' % v)):
                P.add('vector', lambda e, inv=inv, off=off: e.tensor_scalar(out=ang[:, 0:n], in0=posf[:, 0:n], scalar1=inv, scalar2=off, op0=ALU.mult, op1=ALU.add),
                      reads=['posf', 'cs'], writes=['ang'])
                P.add('vector', lambda e: e.tensor_scalar(out=ki[:, 0:n], in0=ang[:, 0:n], scalar1=1.0 / (2 * PI), scalar2=None, op0=ALU.mult),
                      reads=['ang'], writes=['ki'])
                P.add('vector', lambda e: e.tensor_copy(out=kf[:, 0:n], in_=ki[:, 0:n]), reads=['ki'], writes=['kf'])
                P.add('vector', lambda e: e.scalar_tensor_tensor(out=ang[:, 0:n], in0=kf[:, 0:n], scalar=-2 * PI, in1=ang[:, 0:n], op0=ALU.mult, op1=ALU.add),
                      reads=['kf', 'ang'], writes=['ang'])
                P.add('vector', lambda e: e.tensor_scalar(out=ang[:, 0:n], in0=ang[:, 0:n], scalar1=-3.14159, scalar2=3.14159, op0=ALU.max, op1=ALU.min),
                      reads=['ang'], writes=['ang'])
                if which == 'S':
                    P.add('scalar', lambda e, tab=tab: e.activation(out=tab[:, 0:n], in_=ang[:, 0:n], func=AF.Sin, scale=cs[:, 1:2]),
                          reads=['ang', 'cs'], writes=[tkey])
                else:
                    P.add('scalar', lambda e, tab=tab: e.activation(out=tab[:, 0:n], in_=ang[:, 0:n], func=AF.Sin),
                          reads=['ang'], writes=[tkey])

    def next_ps():
        i = cnt['ps'] % 8
        cnt['ps'] += 1
        return i

    def next_ob():
        i = cnt['ob'] % 4
        cnt['ob'] += 1
        return i

    def rope_evac(pi, n, variant, masked, dsts):
        p_ = ps[pi]
        k = cnt['tt'] % 2
        cnt['tt'] += 1
        Ct, Sg = tabC[variant], tabS[variant]
        ck, sk = 'tabC%d' % variant, 'tabS%d' % variant
        P.add('vector', lambda e: e.tensor_tensor(out=t1[k][:, 0:n], in0=p_[:, 0:n], in1=Ct[:, 0:n], op=ALU.mult),
              reads=['ps%d' % pi, ck], writes=['t1_%d' % k])
        P.add('vector', lambda e: e.tensor_tensor(out=t2[k][0:64, 0:n], in0=p_[64:128, 0:n], in1=Sg[0:64, 0:n], op=ALU.mult),
              reads=['ps%d' % pi, sk], writes=['t2_%da' % k])
        P.add('vector', lambda e: e.tensor_tensor(out=t2[k][64:128, 0:n], in0=p_[0:64, 0:n], in1=Sg[64:128, 0:n], op=ALU.mult),
              reads=['ps%d' % pi, sk], writes=['t2_%db' % k])
        if not masked:
            oi = next_ob()
            P.add('gpsimd', lambda e: e.tensor_tensor(out=ob[oi][:, 0:n], in0=t1[k][:, 0:n], in1=t2[k][:, 0:n], op=ALU.add),
                  reads=['t1_%d' % k, 't2_%da' % k, 't2_%db' % k], writes=['ob%d' % oi])
            P.dma(dq(), dsts[0], ob[oi][:, 0:n], reads=['ob%d' % oi])
        else:
            P.add('gpsimd', lambda e: e.tensor_tensor(out=rot[k][:, 0:n], in0=t1[k][:, 0:n], in1=t2[k][:, 0:n], op=ALU.add),
                  reads=['t1_%d' % k, 't2_%da' % k, 't2_%db' % k], writes=['rot%d' % k])
            for m in range(2):
                oi = next_ob()
                P.add('scalar', lambda e, oi=oi, m=m: e.activation(out=ob[oi][:, 0:n], in_=rot[k][:, 0:n], func=AF.Copy, scale=cs[:, 4 + m:5 + m]),
                      reads=['rot%d' % k, 'cs'], writes=['ob%d' % oi])
                P.dma(dq(), dsts[m], ob[oi][:, 0:n], reads=['ob%d' % oi])

    if not layer1:
        xT_v = io['xT'].rearrange("(kc p) t -> p kc t", p=128)
        xTo_v = io['xTo'].rearrange("(kc p) t -> p kc t", p=128)
    else:
        xTg_v = [a.rearrange("(q kc p) t -> q p kc t", q=4, p=128) for a in io['xTg']]
        xTl_v = [a.rearrange("(kc p) t -> p kc t", p=128) for a in io['x2T']]

    for tc in range(8):
        t0 = tc * 512
        if not layer1:
            for h in range(2):
                P.dma(dq(), xs[h], xT_v[:, 8 * h:8 * h + 8, t0:t0 + 512], writes=['xs%d' % h])
                if h == 0:
                    P.add('scalar', lambda e, h=h: e.copy(out=xb[:, 8 * h:8 * h + 8, :], in_=xs[h]), reads=['xs%d' % h], writes=['xb%d' % h])
                else:
                    P.add('gpsimd', lambda e, h=h: e.tensor_copy(out=xb[:, 8 * h:8 * h + 8, :], in_=xs[h]), reads=['xs%d' % h], writes=['xb%d' % h])
        else:
            for q in range(4):
                for k in range(4):
                    P.dma(dq(), xb[:, 4 * k:4 * k + 4, q * 128:(q + 1) * 128], xTg_v[k][q, :, :, tc * 128:(tc + 1) * 128], writes=['xb0', 'xb1'])
        tables(pos[:, t0:t0 + 512], 512)
        oidx = 0
        for ti in range(NQT):
            pi = next_ps()
            for kc in range(16):
                P.add('tensor', lambda e, pi=pi, kc=kc, ti=ti: e.matmul(ps[pi][:, :], lhsT=wq_b[:, kc, ti * 128:(ti + 1) * 128], rhs=xb[:, kc, :], start=(kc == 0), stop=(kc == 15)),
                      reads=['wq_b', 'xb0', 'xb1'], writes=['ps%d' % pi])
            nout = 2 if QT_MASKED[ti] else 1
            dsts = [qt[oidx + m, :, t0:t0 + 512] for m in range(nout)]
            rope_evac(pi, 512, QT_VARIANT[ti], QT_MASKED[ti], dsts)
            oidx += nout
        for tt in range(4):
            pi = next_ps()
            for kc in range(16):
                P.add('tensor', lambda e, pi=pi, kc=kc, tt=tt: e.matmul(ps[pi][:, :], lhsT=xb[:, kc, tt * 128:(tt + 1) * 128], rhs=wv_b[:, kc, :], start=(kc == 0), stop=(kc == 15)),
                      reads=['wv_b', 'xb0', 'xb1'], writes=['ps%d' % pi])
            oi = next_ob()
            P.add('scalar', lambda e, pi=pi, oi=oi: e.copy(out=ob[oi], in_=ps[pi][:, :]), reads=['ps%d' % pi], writes=['ob%d' % oi])
            P.dma(dq(), vt[t0 + tt * 128:t0 + (tt + 1) * 128, :], ob[oi], reads=['ob%d' % oi])

    for ob_i in range(8):
        o0 = ob_i * 128
        if not layer1:
            for h in range(2):
                P.dma(dq(), xs[h][:, :, 0:128], xTo_v[:, 8 * h:8 * h + 8, o0:o0 + 128], writes=['xs%d' % h])
                P.add('gpsimd', lambda e, h=h: e.tensor_copy(out=xob[:, 8 * h:8 * h + 8, :], in_=xs[h][:, :, 0:128]), reads=['xs%d' % h], writes=['xob%d' % h])
        else:
            for k in range(4):
                P.dma(dq(), xob[:, 4 * k:4 * k + 4, :], xTl_v[k][:, :, o0:o0 + 128], writes=['xob0', 'xob1'])
        tables(poso[:, o0:o0 + 128], 128)
        for ti in range(NAO):
            pi = next_ps()
            for kc in range(16):
                P.add('tensor', lambda e, pi=pi, kc=kc, ti=ti: e.matmul(ps[pi][:, 0:128], lhsT=wa_b[:, kc, ti * 128:(ti + 1) * 128], rhs=xob[:, kc, :], start=(kc == 0), stop=(kc == 15)),
                      reads=['wa_b', 'xob0', 'xob1'], writes=['ps%d' % pi])
            if ti < 4:
                dsts = [aq[ti, :, o0:o0 + 128]]
            else:
                pr = ti - 4
                dsts = [iq[2 * pr, :, o0:o0 + 128], iq[2 * pr + 1, :, o0:o0 + 128]]
            rope_evac(pi, 128, AO_VARIANT[ti], AO_MASKED[ti], dsts)
        pi = next_ps()
        for kc in range(16):
            P.add('tensor', lambda e, pi=pi, kc=kc: e.matmul(ps[pi][:, 0:16], lhsT=xob[:, kc, :], rhs=wiw_b[:, kc, :], start=(kc == 0), stop=(kc == 15)),
                  reads=['wiw_b', 'xob0', 'xob1'], writes=['ps%d' % pi])
        P.add('scalar', lambda e, pi=pi: e.copy(out=iwt, in_=ps[pi][:, 0:16]), reads=['ps%d' % pi], writes=['iwt'])
        P.dma(dq(), iw[o0:o0 + 128, :], iwt, reads=['iwt'])


def stage_k2(C, io):
    nc, P = C.nc, C.P
    sb = C.sb
    dq = C.dq
    qt, vt, aq, iq, iw = io['qt'], io['vt'], io['aq'], io['iq'], io['iw']
    o_bd, oa = io['o_bd'], io['oa']
    QA = sb("QA", [128, S], BF16)
    QB = sb("QB", [128, S], BF16)
    KK = sb("KK", [128, S], BF16)
    VV = sb("VV", [128, 32, 129], BF16)
    AQ = sb("AQ", [128, 8, 4, 128], BF16)
    IQ = sb("IQ", [128, 16, 1024], BF16)
    IWS = sb("IWS", [128, 8, 16], F32)
    score = sb("score", [128, S], F32)
    work = sb("work", [128, S], F32)
    selm = sb("selm", [128, S], BF16)
    maskT = sb("maskT", [128, 32, 128], BF16)
    cmask = sb("cmask_s", [128, 4, 512], BF16)
    dmask = sb("dmask_s", [128, 20, 512], BF16)
    negc = sb("negc_s", [128, 512], F32)
    ident = sb("ident_s", [128, 128], BF16)
    dl = sb("dl_s", [128, 256], F32)
    gsc = sb("gsc", [128, 128], F32)
    lc = sb("lc_s", [128, 2], F32)
    sm = sb("sm", [128, 8], F32)
    prod = sb("prod", [128, 64], F32)
    pT = [sb("pT%d" % i, [128, 512], BF16) for i in range(4)]
    rl = [sb("rl%d" % i, [128, 512], F32) for i in range(4)]
    acc = sb("acc", [128, 4, 129], F32)
    rec = sb("rec", [128, 4], F32)
    O1 = sb("O1", [128, 4, 128], F32)
    O2 = sb("O2", [128, 4, 128], F32)
    dd = sb("dd", [128, 4, 128], F32)
    sq = sb("sq", [128, 128], F32)
    ss = sb("ss", [128, 4], F32)
    m8 = sb("m8", [128, 8], F32)
    thr = sb("thr", [128, 1], F32)
    kmean = sb("kmean", [128, 16], F32)
    kmeanb = sb("kmeanb", [128, 16], BF16)
    gate = sb("gate", [128, 32, 16], F32)
    selw = sb("selw", [128, 32, 16], F32)
    g8 = sb("g8", [128, 8], F32)
    gthr = sb("gthr", [128, 1], F32)
    pst = [C.ps[0], C.ps[1], C.ps[6], C.ps[7]]
    pstk = ['pst0', 'pst1', 'px0', 'px1']
    po = [C.ps[2], C.ps[3], C.ps[4], C.ps[5]]
    px = [C.ps[6], C.ps[7]]
    pxi = [C.ps[6], C.ps[7], C.ps[0], C.ps[1]]
    pxik = ['px0', 'px1', 'pst0', 'pst1']
    cnt = {'pst': 0, 'mk': 0, 'px': 0, 'rl': 0}

    P.dma('sync', cmask, io['cmask'][:, :, :], writes=['cmask'])
    P.dma('gpsimd', dmask, io['dmask'][:, :, :], writes=['dmask'])
    P.dma('sync', negc, io['negc'][:, :], writes=['negc'])
    P.dma('sync', ident, io['ident'][:, :], writes=['ident'])
    P.dma('sync', dl, io['dl'].partition_broadcast(128), writes=['dl'])
    P.dma('sync', gsc, io['dg'].partition_broadcast(128), writes=['gsc'])
    P.dma('sync', lc, io['lc'][:, :], writes=['lc'])
    for i in range(2):
        P.add('vector', lambda e, i=i: e.tensor_tensor(out=prod, in0=dl[:, 128 * i:128 * i + 64], in1=dl[:, 128 * i + 64:128 * i + 128], op=ALU.mult),
              reads=['dl'], writes=['prod'])
        P.add('vector', lambda e, i=i: e.reduce_sum(out=sm[:, i:i + 1], in_=prod, axis=AX.X), reads=['prod'], writes=['sm%d' % i])
        P.add('scalar', lambda e, i=i: e.activation(out=sm[:, 2 + i:3 + i], in_=sm[:, i:i + 1], func=AF.Exp), reads=['sm%d' % i], writes=['sm%d' % (2 + i)])
    P.add('vector', lambda e: e.tensor_tensor(out=sm[:, 4:5], in0=sm[:, 2:3], in1=sm[:, 3:4], op=ALU.subtract), reads=['sm2', 'sm3'], writes=['sm4'])
    P.add('vector', lambda e: e.tensor_tensor(out=sm[:, 4:5], in0=sm[:, 4:5], in1=lc[:, 0:1], op=ALU.add), reads=['sm4', 'lc'], writes=['sm4'])
    P.add('vector', lambda e: e.tensor_scalar(out=sm[:, 5:6], in0=sm[:, 4:5], scalar1=-1.0, scalar2=None, op0=ALU.mult), reads=['sm4'], writes=['sm5'])
    P.add('vector', lambda e: e.tensor_scalar(out=gsc, in0=gsc, scalar1=lc[:, 1:2], scalar2=None, op0=ALU.mult), reads=['gsc', 'lc'], writes=['gsc'])

    def load_qt(dst, idx, key):
        P.dma(dq(), dst[:, 0:2048], qt[idx, :, 0:2048], writes=[key])
        P.dma(dq(), dst[:, 2048:4096], qt[idx, :, 2048:4096], writes=[key])

    def load_v(m):
        P.dma(dq(), VV[:, :, 0:128], vt.rearrange("(blk p) c -> p blk c", p=128)[:, :, m * 128:(m + 1) * 128], writes=['VV'])
        P.add('gpsimd', lambda e: e.memset(VV[:, :, 128:129], 1.0), writes=['VVone'])

    def attn_chunk(q_ap, qkeys, blocks, scale, fin):
        P.add('gpsimd', lambda e: e.memset(acc, 0.0), writes=['acc'])
        DEPTH = 3
        n = len(blocks)
        bufs = {}

        def emit_qk(bi):
            blk = blocks[bi]
            i = cnt['pst'] % 4
            cnt['pst'] += 1
            bufs[bi] = i
            P.add('tensor', lambda e, i=i, blk=blk: e.matmul(pst[i][:, :], lhsT=blk['k'], rhs=q_ap, start=True, stop=True),
                  reads=list(qkeys) + list(blk['kkeys']), writes=[pstk[i]])
            P.add('scalar', lambda e, i=i: e.activation(out=pT[i], in_=pst[i][:, :], func=AF.Exp, scale=scale),
                  reads=[pstk[i]], writes=['pT%d' % i])
            if blk.get('mask') is not None:
                for (lo, hi, m_ap, mkeys) in blk['mask']:
                    eng = ('vector', 'gpsimd')[cnt['mk'] % 2]
                    cnt['mk'] += 1
                    P.add(eng, lambda e, i=i, lo=lo, hi=hi, m_ap=m_ap: e.tensor_tensor(out=pT[i][:, lo:hi], in0=pT[i][:, lo:hi], in1=m_ap, op=ALU.mult),
                          reads=['pT%d' % i] + list(mkeys), writes=['pT%d' % i])

        def emit_pv(bi):
            blk = blocks[bi]
            i = bufs[bi]
            for sub in range(4):
                P.add('tensor', lambda e, i=i, sub=sub, blk=blk: e.matmul(po[sub][:, 0:129], lhsT=pT[i][:, sub * 128:(sub + 1) * 128], rhs=blk['v'],
                                                                         start=blk['gs'], stop=blk['ge']),
                      reads=['pT%d' % i, 'VV', 'VVone'], writes=['po%d' % sub])
            if blk['ge']:
                for sub in range(4):
                    w = blk['w'][sub]
                    wkeys = [] if isinstance(w, float) else ['selw']
                    P.add('vector', lambda e, sub=sub, w=w: e.scalar_tensor_tensor(out=acc[:, sub, :], in0=po[sub][:, 0:129], scalar=w, in1=acc[:, sub, :], op0=ALU.mult, op1=ALU.add),
                          reads=['po%d' % sub, 'acc'] + wkeys, writes=['acc'])

        for t_ in range(n + DEPTH):
            if t_ < n:
                emit_qk(t_)
            if t_ - DEPTH >= 0:
                emit_pv(t_ - DEPTH)
        fin()

    def normalize(dst, dkey):
        P.add('vector', lambda e: e.reciprocal(out=rec, in_=acc[:, :, 128]), reads=['acc'], writes=['rec'])
        for sub in range(4):
            P.add('vector', lambda e, sub=sub: e.tensor_scalar(out=dst[:, sub, :], in0=acc[:, sub, 0:128], scalar1=rec[:, sub:sub + 1], scalar2=None, op0=ALU.mult),
                  reads=['acc', 'rec'], writes=[dkey])

    def causal_blocks(tc, kbuf_key):
        blocks = []
        nb = 4 * tc + 4
        for kb in range(nb):
            d = kb - 4 * tc
            mask = None
            if d >= 0:
                mask = [(0, 512, cmask[:, d, :], ['cmask'])]
            blocks.append(dict(k=KK[:, kb * 128:(kb + 1) * 128], kkeys=[kbuf_key], v=VV[:, kb, :], mask=mask,
                               gs=(kb == 0), ge=(kb == nb - 1), w=[1.0] * 4))
        return blocks

    def store_o(tc, m):
        for d_ in range(4):
            R = d_ * 1024 + tc * 128
            P.dma(dq(), o_bd[R // 512][R % 512:R % 512 + 128, m * 128:(m + 1) * 128], O1[:, d_, :], reads=['O1'])

    load_qt(QA, 0, 'QA')
    load_qt(QB, 1, 'QB')
    load_qt(KK, 2, 'KK')
    load_v(0)
    for tc in range(8):
        t0 = tc * 512
        attn_chunk(QA[:, t0:t0 + 512], ['QA'], causal_blocks(tc, 'KK'), 64 ** -0.5, lambda: normalize(O1, 'O1'))
        attn_chunk(QB[:, t0:t0 + 512], ['QB'], causal_blocks(tc, 'KK'), 64 ** -0.5, lambda: normalize(O2, 'O2'))
        P.add('vector', lambda e: e.scalar_tensor_tensor(out=dd, in0=O2, scalar=sm[:, 5:6], in1=O1, op0=ALU.mult, op1=ALU.add),
              reads=['O1', 'O2', 'sm5'], writes=['dd'])
        for sub in range(4):
            P.add('scalar', lambda e, sub=sub: e.activation(out=sq, in_=dd[:, sub, :], func=AF.Square, accum_out=ss[:, sub:sub + 1]),
                  reads=['dd'], writes=['sq', 'ss'])
        P.add('vector', lambda e: e.tensor_scalar(out=ss, in0=ss, scalar1=1.0 / 128, scalar2=1e-5, op0=ALU.mult, op1=ALU.add), reads=['ss'], writes=['ss'])
        P.add('scalar', lambda e: e.activation(out=ss, in_=ss, func=AF.Sqrt), reads=['ss'], writes=['ss'])
        P.add('vector', lambda e: e.reciprocal(out=ss, in_=ss), reads=['ss'], writes=['ss'])
        for sub in range(4):
            P.add('vector', lambda e, sub=sub: e.scalar_tensor_tensor(out=O1[:, sub, :], in0=dd[:, sub, :], scalar=ss[:, sub:sub + 1], in1=gsc, op0=ALU.mult, op1=ALU.mult),
                  reads=['dd', 'ss', 'gsc'], writes=['O1'])
        store_o(tc, 0)

    load_qt(QA, 3, 'QA')
    load_qt(KK, 4, 'KK')
    load_v(1)
    P.add('vector', lambda e: e.tensor_reduce(out=kmean, in_=KK.rearrange("p (n k) -> p n k", k=256), axis=AX.X, op=ALU.add), reads=['KK'], writes=['kmean'])
    P.add('vector', lambda e: e.tensor_scalar(out=kmeanb, in0=kmean, scalar1=1.0 / 256, scalar2=None, op0=ALU.mult), reads=['kmean'], writes=['kmeanb'])
    for qb in range(32):
        pi = cnt['px'] % 2
        cnt['px'] += 1
        own = qb // 2
        P.add('tensor', lambda e, pi=pi, qb=qb: e.matmul(px[pi][:, 0:16], lhsT=QA[:, qb * 128:(qb + 1) * 128], rhs=kmeanb, start=True, stop=True),
              reads=['QA', 'kmeanb'], writes=['px%d' % pi])
        P.add('vector', lambda e, pi=pi, qb=qb: e.tensor_copy(out=gate[:, qb, :], in_=px[pi][:, 0:16]), reads=['px%d' % pi], writes=['gate'])
        P.add('vector', lambda e, qb=qb, own=own: e.memset(gate[:, qb, own:16], NEG), reads=['gate'], writes=['gate'])
        P.add('vector', lambda e, qb=qb: e.max(out=g8, in_=gate[:, qb, :]), reads=['gate'], writes=['g8'])
        P.add('vector', lambda e: e.tensor_scalar(out=gthr, in0=g8[:, 2:3], scalar1=-1.0e29, scalar2=None, op0=ALU.max), reads=['g8'], writes=['gthr'])
        P.add('vector', lambda e, qb=qb: e.tensor_scalar(out=selw[:, qb, :], in0=gate[:, qb, :], scalar1=gthr[:, 0:1], scalar2=None, op0=ALU.is_ge),
              reads=['gate', 'gthr'], writes=['selw'])
    for tc in range(8):
        t0 = tc * 512
        blocks = []
        for n in range(2 * tc + 2):
            for half in range(2):
                kb = 2 * n + half
                d = kb - 4 * tc
                mask = [(0, 512, cmask[:, d, :], ['cmask'])] if d >= 0 else None
                if n < 2 * tc:
                    w = [selw[:, 4 * tc + sub, n:n + 1] for sub in range(4)]
                elif n == 2 * tc:
                    w = [1.0, 1.0, selw[:, 4 * tc + 2, n:n + 1], selw[:, 4 * tc + 3, n:n + 1]]
                else:
                    w = [1.0] * 4
                blocks.append(dict(k=KK[:, kb * 128:(kb + 1) * 128], kkeys=['KK'], v=VV[:, kb, :], mask=mask,
                                   gs=(half == 0), ge=(half == 1), w=w))
        attn_chunk(QA[:, t0:t0 + 512], ['QA'], blocks, 128 ** -0.5, lambda: normalize(O1, 'O1'))
        store_o(tc, 1)

    load_qt(QA, 5, 'QA')
    load_qt(KK, 6, 'KK')
    load_v(2)
    for tc in range(8):
        t0 = tc * 512
        blocks = []
        kbs = [kb for kb in range(32) if -384 <= t0 - kb * 128 <= 2048]
        for ii, kb in enumerate(kbs):
            di = (t0 - kb * 128 + 384) // 128
            blocks.append(dict(k=KK[:, kb * 128:(kb + 1) * 128], kkeys=['KK'], v=VV[:, kb, :], mask=[(0, 512, dmask[:, di, :], ['dmask'])],
                               gs=(ii == 0), ge=(ii == len(kbs) - 1), w=[1.0] * 4))
        attn_chunk(QA[:, t0:t0 + 512], ['QA'], blocks, 128 ** -0.5, lambda: normalize(O1, 'O1'))
        store_o(tc, 2)

    load_qt(KK, 7, 'KK')
    load_qt(QB, 8, 'QB')
    load_v(3)
    P.dma(dq(), AQ, aq.rearrange("h p (ob t) -> p ob h t", t=128), writes=['AQ'])
    for h4 in range(4):
        P.dma(dq(), IQ[:, 4 * h4:4 * h4 + 4, :], iq[4 * h4:4 * h4 + 4].rearrange("h p t -> p h t"), writes=['IQ'])
    P.dma(dq(), IWS, iw.rearrange("(ob p) h -> p ob h", p=128), writes=['IWS'])
    P.add('vector', lambda e: e.tensor_scalar(out=IWS, in0=IWS, scalar1=0.25, scalar2=None, op0=ALU.mult), reads=['IWS'], writes=['IWS'])
    for c in range(8):
        L = 512 * (c + 1)
        for h in range(16):
            for kc in range(c + 1):
                pi = cnt['rl'] % 4
                ri = cnt['rl'] % 4
                cnt['rl'] += 1
                P.add('tensor', lambda e, pi=pi, h=h, kc=kc, c=c: e.matmul(pxi[pi][:, :], lhsT=IQ[:, h, c * 128:(c + 1) * 128], rhs=QB[:, kc * 512:(kc + 1) * 512], start=True, stop=True),
                      reads=['IQ', 'QB'], writes=[pxik[pi]])
                P.add('scalar', lambda e, pi=pi, ri=ri: e.activation(out=rl[ri], in_=pxi[pi][:, :], func=AF.Relu, scale=0.125),
                      reads=[pxik[pi]], writes=['rl%d' % ri])
                sk = 'score%d' % kc
                if h == 0:
                    P.add('vector', lambda e, ri=ri, kc=kc, h=h, c=c: e.tensor_scalar(out=score[:, kc * 512:(kc + 1) * 512], in0=rl[ri], scalar1=IWS[:, c, h:h + 1], scalar2=None, op0=ALU.mult),
                          reads=['rl%d' % ri, 'IWS'], writes=[sk])
                else:
                    P.add('vector', lambda e, ri=ri, kc=kc, h=h, c=c: e.scalar_tensor_tensor(out=score[:, kc * 512:(kc + 1) * 512], in0=rl[ri], scalar=IWS[:, c, h:h + 1], in1=score[:, kc * 512:(kc + 1) * 512], op0=ALU.mult, op1=ALU.add),
                          reads=['rl%d' % ri, 'IWS', sk], writes=[sk])
        P.add('vector', lambda e, c=c: e.tensor_tensor(out=score[:, c * 512:(c + 1) * 512], in0=score[:, c * 512:(c + 1) * 512], in1=negc, op=ALU.add),
              reads=['score%d' % c, 'negc'], writes=['score%d' % c])
        skeys = ['score%d' % kc for kc in range(c + 1)]
        for r in range(32):
            src = score if r == 0 else work
            srck = skeys if r == 0 else ['work']
            P.add('vector', lambda e, src=src, L=L: e.max(out=m8, in_=src[:, 0:L]), reads=srck, writes=['m8'])
            if r < 31:
                P.add('vector', lambda e, src=src, L=L: e.match_replace(out=work[:, 0:L], in_to_replace=m8, in_values=src[:, 0:L], imm_value=-3.0e38),
                      reads=srck + ['m8'], writes=['work'])
        P.add('vector', lambda e: e.tensor_scalar(out=thr, in0=m8[:, 7:8], scalar1=-1.0e29, scalar2=None, op0=ALU.max), reads=['m8'], writes=['thr'])
        P.add('vector', lambda e, L=L: e.tensor_scalar(out=selm[:, 0:L], in0=score[:, 0:L], scalar1=thr[:, 0:1], scalar2=None, op0=ALU.is_ge),
              reads=skeys + ['thr'], writes=['selm'])
        nb = 4 * (c + 1)
        for g4 in range(c + 1):
            pi = cnt['px'] % 2
            cnt['px'] += 1
            for q in range(4):
                kb = 4 * g4 + q
                P.add('tensor', lambda e, pi=pi, q=q, kb=kb: e.matmul(px[pi][:, q * 128:(q + 1) * 128], lhsT=selm[:, kb * 128:(kb + 1) * 128], rhs=ident, start=True, stop=True),
                      reads=['selm', 'ident'], writes=['px%d' % pi])
            P.add('scalar', lambda e, pi=pi, g4=g4: e.copy(out=maskT[:, 4 * g4:4 * g4 + 4, :], in_=px[pi][:, :].rearrange("p (q t) -> p q t", t=128)),
                  reads=['px%d' % pi], writes=['maskT'])
        blocks = []
        for kb in range(nb):
            mask = [(hh * 128, (hh + 1) * 128, maskT[:, kb, :], ['maskT']) for hh in range(4)]
            blocks.append(dict(k=KK[:, kb * 128:(kb + 1) * 128], kkeys=['KK'], v=VV[:, kb, :], mask=mask,
                               gs=(kb == 0), ge=(kb == nb - 1), w=[1.0] * 4))
        attn_chunk(AQ[:, c, :, :], ['AQ'], blocks, 128 ** -0.5, lambda: normalize(O1, 'O1'))
        P.dma(dq(), oa[c * 128:(c + 1) * 128, :].rearrange("p (h d) -> p h d", d=128), O1, reads=['O1'])


def stage_k3(C, io):
    nc, P, ps = C.nc, C.P, C.ps
    sb = C.sb
    dq = C.dq
    og, oa, xres = io['og'], io['oa'], io['xres']
    x1, x1T, gates = io['x1'], io['x1T'], io['gates']
    wob = sb("wob", [128, 16, D], BF16)
    stg = [sb("stg%d" % i, [128, 2048], F32) for i in range(2)]
    G = sb("G", [128, D]); Bt = sb("Bt", [128, D])
    rwf = sb("rwf", [128, 16, 16]); rbt = sb("rbt", [128, 16])
    ident = sb("identf_s", [128, 128])
    oneh = sb("oneh", [128, 4])
    xr = sb("xr", [128, D]); z = sb("z", [128, D]); junk = sb("junk", [128, D]); xo = sb("xo", [128, D])
    mixt = sb("mixt", [128, D])
    cand = [sb("cand%d" % i, [128, 4, 384]) for i in range(4)]
    mixb = sb("mixb", [128, 16, 128], BF16)
    st6 = sb("st6", [128, 8])
    xTf = sb("xTf", [128, 16, 128]); xTb = sb("xTb", [128, 16, 128], BF16)
    aff = sb("aff", [128, 16]); sel = sb("sel", [128, 16]); tmp = sb("tmp", [128, 16]); eq = sb("eq", [128, 16])
    m1 = sb("m1", [128, 4]); m2 = sb("m2", [128, 4]); gs = sb("gs", [128, 4]); gm = sb("gm", [128, 1]); oh = sb("oh", [128, 4])
    msk = sb("msk", [128, 16]); gsum = sb("gsum", [128, 1]); gout = sb("gout", [128, 16])
    cnt = {'ps': 0, 'stg': 0}

    def nps():
        i = cnt['ps'] % 8
        cnt['ps'] += 1
        return i
    P.dma('sync', G, io['lng'].partition_broadcast(128), writes=['G'])
    P.dma('sync', Bt, io['lnb'].partition_broadcast(128), writes=['Bt'])
    P.dma('sync', rwf, io['rw'].rearrange("(kc p) e -> p kc e", p=128), writes=['rwf'])
    P.dma('sync', rbt, io['rb'].partition_broadcast(128), writes=['rbt'])
    P.dma('sync', ident, io['identf'][:, :], writes=['ident'])
    P.dma('sync', oneh, io['oneh'][:, :], writes=['oneh'])
    wo_v = io['wo'].rearrange("(kc p) c -> p kc c", p=128)
    for kc in range(16):
        i = cnt['stg'] % 2
        cnt['stg'] += 1
        P.dma(dq(), stg[i], wo_v[:, kc, :], writes=['stg%d' % i])
        eng = ('vector', 'gpsimd')[i]
        P.add(eng, lambda e, i=i, kc=kc: e.tensor_copy(out=wob[:, kc, :], in_=stg[i]), reads=['stg%d' % i], writes=['wob'])
    og4 = [a.rearrange("(q r) c -> q r c", q=4) for a in og]
    for tt in range(8):
        r0 = tt * 128
        P.dma(dq(), xr, xres[r0:r0 + 128, :], writes=['xr'])
        P.dma(dq(), mixt[:, 0:512], oa[r0:r0 + 128, :], writes=['mixA'])
        for d in range(4):
            R = d * 1024 + r0
            P.dma(dq(), cand[d], og4[R // 512][:, R % 512:R % 512 + 128, :].rearrange("q p c -> p q c"), writes=['cand%d' % d])
        for d in range(4):
            for m in range(3):
                mv = mixt[:, 512 + m * 512:512 + (m + 1) * 512].rearrange("p (q c) -> p q c", q=4)
                cv = cand[d][:, :, m * 128:(m + 1) * 128]
                if d == 0:
                    P.add('vector', lambda e, cv=cv, mv=mv: e.tensor_scalar(out=mv, in0=cv, scalar1=oneh[:, 0:1], scalar2=None, op0=ALU.mult),
                          reads=['cand0', 'oneh'], writes=['mixB%d' % m])
                else:
                    P.add('vector', lambda e, cv=cv, mv=mv, d=d: e.scalar_tensor_tensor(out=mv, in0=cv, scalar=oneh[:, d:d + 1], in1=mv, op0=ALU.mult, op1=ALU.add),
                          reads=['cand%d' % d, 'oneh', 'mixB%d' % m], writes=['mixB%d' % m])
        for g4 in range(4):
            pi = nps()
            for q in range(4):
                kc = 4 * g4 + q
                P.add('tensor', lambda e, pi=pi, q=q, kc=kc: e.matmul(ps[pi][:, q * 128:(q + 1) * 128], lhsT=mixt[:, kc * 128:(kc + 1) * 128], rhs=ident, start=True, stop=True),
                      reads=['mixA', 'mixB0', 'mixB1', 'mixB2', 'ident'], writes=['ps%d' % pi])
            P.add('scalar', lambda e, pi=pi, g4=g4: e.copy(out=mixb[:, 4 * g4:4 * g4 + 4, :], in_=ps[pi][:, :].rearrange("p (q t) -> p q t", t=128)), reads=['ps%d' % pi], writes=['mixb'])
        for dc in range(4):
            pi = nps()
            for kc in range(16):
                P.add('tensor', lambda e, pi=pi, kc=kc, dc=dc: e.matmul(ps[pi][:, :], lhsT=mixb[:, kc, :], rhs=wob[:, kc, dc * 512:(dc + 1) * 512], start=(kc == 0), stop=(kc == 15)),
                      reads=['mixb', 'wob'], writes=['ps%d' % pi])
            P.add('vector', lambda e, pi=pi, dc=dc: e.scalar_tensor_tensor(out=z[:, dc * 512:(dc + 1) * 512], in0=xr[:, dc * 512:(dc + 1) * 512], scalar=ALPHA, in1=ps[pi][:, :], op0=ALU.mult, op1=ALU.add),
                  reads=['xr', 'ps%d' % pi], writes=['z'])
        _ln_ops(P, z, 'z', xo, 'xo', G, Bt, junk, st6, 'a')
        P.dma(dq(), x1[r0:r0 + 128, :], xo, reads=['xo'])
        for g4 in range(4):
            pi = nps()
            for q in range(4):
                kc = 4 * g4 + q
                P.add('tensor', lambda e, pi=pi, q=q, kc=kc: e.matmul(ps[pi][:, q * 128:(q + 1) * 128], lhsT=xo[:, kc * 128:(kc + 1) * 128], rhs=ident, start=True, stop=True),
                      reads=['xo', 'ident'], writes=['ps%d' % pi])
            P.add('scalar', lambda e, pi=pi, g4=g4: e.copy(out=xTf[:, 4 * g4:4 * g4 + 4, :], in_=ps[pi][:, :].rearrange("p (q t) -> p q t", t=128)), reads=['ps%d' % pi], writes=['xTf'])
        P.add('gpsimd', lambda e: e.tensor_copy(out=xTb, in_=xTf), reads=['xTf'], writes=['xTb'])
        for k in range(4):
            P.dma(dq(), x1T[k].rearrange("(kc p) t -> p kc t", p=128)[:, :, r0:r0 + 128], xTb[:, 4 * k:4 * k + 4, :], reads=['xTb'])
        pi = nps()
        for kc in range(16):
            P.add('tensor', lambda e, pi=pi, kc=kc: e.matmul(ps[pi][:, 0:16], lhsT=xTf[:, kc, :], rhs=rwf[:, kc, :], start=(kc == 0), stop=(kc == 15)),
                  reads=['xTf', 'rwf'], writes=['ps%d' % pi])
        P.add('scalar', lambda e, pi=pi: e.activation(out=aff, in_=ps[pi][:, 0:16], func=AF.Sigmoid), reads=['ps%d' % pi], writes=['aff'])
        P.add('vector', lambda e: e.tensor_tensor(out=sel, in0=aff, in1=rbt, op=ALU.add), reads=['aff', 'rbt'], writes=['sel'])
        P.add('vector', lambda e: e.tensor_reduce(out=m1, in_=sel.rearrange("p (g l) -> p g l", l=4), axis=AX.X, op=ALU.max), reads=['sel'], writes=['m1'])
        for g in range(4):
            P.add('vector', lambda e, g=g: e.tensor_scalar(out=eq[:, 4 * g:4 * g + 4], in0=sel[:, 4 * g:4 * g + 4], scalar1=m1[:, g:g + 1], scalar2=-1.0e9, op0=ALU.is_equal, op1=ALU.mult),
                  reads=['sel', 'm1'], writes=['eq'])
        P.add('vector', lambda e: e.tensor_tensor(out=tmp, in0=sel, in1=eq, op=ALU.add), reads=['sel', 'eq'], writes=['tmp'])
        P.add('vector', lambda e: e.tensor_reduce(out=m2, in_=tmp.rearrange("p (g l) -> p g l", l=4), axis=AX.X, op=ALU.max), reads=['tmp'], writes=['m2'])
        P.add('vector', lambda e: e.tensor_tensor(out=gs, in0=m1, in1=m2, op=ALU.add), reads=['m1', 'm2'], writes=['gs'])
        P.add('vector', lambda e: e.tensor_reduce(out=gm, in_=gs, axis=AX.X, op=ALU.max), reads=['gs'], writes=['gm'])
        P.add('vector', lambda e: e.tensor_scalar(out=oh, in0=gs, scalar1=gm[:, 0:1], scalar2=None, op0=ALU.is_equal), reads=['gs', 'gm'], writes=['oh'])
        for g in range(4):
            P.add('vector', lambda e, g=g: e.tensor_scalar(out=msk[:, 4 * g:4 * g + 4], in0=sel[:, 4 * g:4 * g + 4], scalar1=m2[:, g:g + 1], scalar2=oh[:, g:g + 1], op0=ALU.is_ge, op1=ALU.mult),
                  reads=['sel', 'm2', 'oh'], writes=['msk'])
        P.add('vector', lambda e: e.tensor_tensor(out=gout, in0=aff, in1=msk, op=ALU.mult), reads=['aff', 'msk'], writes=['gout'])
        P.add('vector', lambda e: e.reduce_sum(out=gsum, in_=gout, axis=AX.X), reads=['gout'], writes=['gsum'])
        P.add('vector', lambda e: e.reciprocal(out=gsum, in_=gsum), reads=['gsum'], writes=['gsum'])
        P.add('vector', lambda e: e.tensor_scalar(out=gout, in0=gout, scalar1=gsum[:, 0:1], scalar2=None, op0=ALU.mult), reads=['gout', 'gsum'], writes=['gout'])
        P.dma(dq(), gates[r0:r0 + 128, :], gout, reads=['gout'])


def stage_k4(C, io):
    nc, P, ps = C.nc, C.P, C.ps
    sb = C.sb
    dq = C.dq
    x1Tg, gg, wg, wu, wd, part = io['x1Tg'], io['gg'], io['wg'], io['wu'], io['wd'], io['part']
    wgb = [sb("wgb%d" % i, [128, 16, 512], BF16) for i in range(2)]
    wub = [sb("wub%d" % i, [128, 16, 512], BF16) for i in range(2)]
    wdb = [sb("wdb%d" % i, [128, 4, D], BF16) for i in range(2)]
    stg = [sb("stg%d" % i, [128, 2048], F32) for i in range(2)]
    xc = [sb("xc%d" % i, [128, 16, 512], BF16) for i in range(2)]
    hT = sb("hT", [128, 4, 512], BF16)
    prev = [sb("prev%d" % i, [128, D]) for i in range(2)]
    yt = [sb("yt%d" % i, [128, D]) for i in range(2)]
    gall = sb("gall", [128, 32, 16])
    gt = sb("gt", [128, 32, 4])
    oneh = sb("oneh", [128, 4])
    sg = [sb("sg%d" % i, [128, 512], F32) for i in range(2)]
    cnt = {'ps': 0, 'stg': 0, 'sg': 0, 'cv': 0, 'yb': 0}
    cv_eng = ('scalar', 'vector', 'scalar', 'vector', 'gpsimd')

    def nps():
        i = cnt['ps'] % 8
        cnt['ps'] += 1
        return i

    def conv(dst_ap, dkey, src_ap, is3d):
        i = cnt['stg'] % 2
        cnt['stg'] += 1
        sview = stg[i].rearrange("p (a b) -> p a b", b=128) if is3d else stg[i]
        P.dma(dq(), sview, src_ap, writes=['stg%d' % i])
        eng = cv_eng[cnt['cv'] % 5]
        cnt['cv'] += 1
        if eng == 'scalar':
            P.add('scalar', lambda e: e.copy(out=dst_ap, in_=sview), reads=['stg%d' % i], writes=[dkey])
        else:
            P.add(eng, lambda e: e.tensor_copy(out=dst_ap, in_=sview), reads=['stg%d' % i], writes=[dkey])

    def weight_jobs(he):
        ex, fh, b = he // 2, he % 2, he % 2
        wg_v = wg[ex].rearrange("(kc p) f -> p kc f", p=128)
        wu_v = wu[ex].rearrange("(kc p) f -> p kc f", p=128)
        wd_v = wd[ex].rearrange("(fc p) d -> p fc d", p=128)
        jobs = []
        for fl in range(4):
            f = 4 * fh + fl
            jobs.append(lambda fl=fl, f=f: conv(wgb[b][:, :, fl * 128:(fl + 1) * 128], 'wgb%d' % b, wg_v[:, :, f * 128:(f + 1) * 128], True))
            jobs.append(lambda fl=fl, f=f: conv(wub[b][:, :, fl * 128:(fl + 1) * 128], 'wub%d' % b, wu_v[:, :, f * 128:(f + 1) * 128], True))
        for fl in range(4):
            f = 4 * fh + fl
            jobs.append(lambda fl=fl, f=f: conv(wdb[b][:, fl, :], 'wdb%d' % b, wd_v[:, f, :], False))
        return jobs

    P.dma('sync', oneh, io['oneh'][:, :], writes=['oneh'])
    P.dma('sync', gall, gg.rearrange("(tt p) e -> p tt e", p=128), writes=['gall'])
    gv = gall.rearrange("p t (g l) -> p t g l", g=4)
    for g in range(4):
        if g == 0:
            P.add('vector', lambda e: e.tensor_scalar(out=gt, in0=gv[:, :, 0, :], scalar1=oneh[:, 0:1], scalar2=None, op0=ALU.mult), reads=['gall', 'oneh'], writes=['gt'])
        else:
            P.add('vector', lambda e, g=g: e.scalar_tensor_tensor(out=gt, in0=gv[:, :, g, :], scalar=oneh[:, g:g + 1], in1=gt, op0=ALU.mult, op1=ALU.add),
                  reads=['gall', 'oneh', 'gt'], writes=['gt'])
    x_v = [a.rearrange("(q kc p) t -> q p kc t", q=4, p=128) for a in x1Tg]
    for j_ in weight_jobs(0):
        j_()
    for he in range(8):
        ex, b = he // 2, he % 2
        nxt = weight_jobs(he + 1) if he < 7 else []
        for c8 in range(8):
            ch, half = c8 // 2, c8 % 2
            xi = c8 % 2
            for k in range(4):
                P.dma(dq(), xc[xi][:, 4 * k:4 * k + 4, :], x_v[k][ch, :, :, half * 512:(half + 1) * 512], writes=['xc%d' % xi])
            for fl in range(4):
                pg = nps()
                for kc in range(16):
                    P.add('tensor', lambda e, pg=pg, kc=kc, fl=fl, xi=xi, b=b: e.matmul(ps[pg][:, :], lhsT=wgb[b][:, kc, fl * 128:(fl + 1) * 128], rhs=xc[xi][:, kc, :], start=(kc == 0), stop=(kc == 15)),
                          reads=['wgb%d' % b, 'xc%d' % xi], writes=['ps%d' % pg])
                pu = nps()
                for kc in range(16):
                    P.add('tensor', lambda e, pu=pu, kc=kc, fl=fl, xi=xi, b=b: e.matmul(ps[pu][:, :], lhsT=wub[b][:, kc, fl * 128:(fl + 1) * 128], rhs=xc[xi][:, kc, :], start=(kc == 0), stop=(kc == 15)),
                          reads=['wub%d' % b, 'xc%d' % xi], writes=['ps%d' % pu])
                si = cnt['sg'] % 2
                cnt['sg'] += 1
                P.add('scalar', lambda e, pg=pg, si=si: e.activation(out=sg[si], in_=ps[pg][:, :], func=AF.Silu), reads=['ps%d' % pg], writes=['sg%d' % si])
                P.add('vector', lambda e, pu=pu, si=si, fl=fl: e.tensor_tensor(out=hT[:, fl, :], in0=sg[si], in1=ps[pu][:, :], op=ALU.mult),
                      reads=['sg%d' % si, 'ps%d' % pu], writes=['hT'])
            for _ in range(2):
                if nxt:
                    nxt.pop(0)()
            for tt in range(4):
                T = ch * 8 + half * 4 + tt
                row = ch * 512 + tt * 128
                yb = cnt['yb'] % 2
                cnt['yb'] += 1
                pkey = 'part_%d_%d' % (c8, tt)
                if he > 0:
                    P.dma(dq(), prev[yb], part[half][row:row + 128, :], reads=[pkey], writes=['prev%d' % yb])
                for dc in range(4):
                    pi = nps()
                    for fl in range(4):
                        P.add('tensor', lambda e, pi=pi, fl=fl, tt=tt, dc=dc, b=b: e.matmul(ps[pi][:, :], lhsT=hT[:, fl, tt * 128:(tt + 1) * 128], rhs=wdb[b][:, fl, dc * 512:(dc + 1) * 512], start=(fl == 0), stop=(fl == 3)),
                              reads=['hT', 'wdb%d' % b], writes=['ps%d' % pi])
                    gsc = gt[:, T, ex:ex + 1]
                    if he == 0:
                        P.add('vector', lambda e, pi=pi, dc=dc, gsc=gsc, yb=yb: e.tensor_scalar(out=yt[yb][:, dc * 512:(dc + 1) * 512], in0=ps[pi][:, :], scalar1=gsc, scalar2=None, op0=ALU.mult),
                              reads=['ps%d' % pi, 'gt'], writes=['yt%d' % yb])
                    else:
                        P.add('vector', lambda e, pi=pi, dc=dc, gsc=gsc, yb=yb: e.scalar_tensor_tensor(out=yt[yb][:, dc * 512:(dc + 1) * 512], in0=ps[pi][:, :], scalar=gsc, in1=prev[yb][:, dc * 512:(dc + 1) * 512], op0=ALU.mult, op1=ALU.add),
                              reads=['ps%d' % pi, 'gt', 'prev%d' % yb], writes=['yt%d' % yb])
                P.dma(dq(), part[half][row:row + 128, :], yt[yb], reads=['yt%d' % yb], writes=[pkey])
        while nxt:
            nxt.pop(0)()


def stage_k5(C, io, last):
    nc, P, ps = C.nc, C.P, C.ps
    sb = C.sb
    dq = C.dq
    G = sb("G", [128, D]); Bt = sb("Bt", [128, D])
    xr = sb("xr", [128, D]); z = sb("z", [128, D]); junk = sb("junk", [128, D]); xo = sb("xo", [128, D])
    pt = sb("pt", [128, D])
    st6 = sb("st6", [128, 8])
    ident = sb("identf_s", [128, 128])
    xTb = sb("xTb", [128, 16, 128], BF16)
    cnt = {'ps': 0}

    def nps():
        i = cnt['ps'] % 8
        cnt['ps'] += 1
        return i
    P.dma('sync', G, io['lng'].partition_broadcast(128), writes=['G'])
    P.dma('sync', Bt, io['lnb'].partition_broadcast(128), writes=['Bt'])
    P.dma('sync', ident, io['identf'][:, :], writes=['ident'])
    for tt in range(8):
        r0 = tt * 128
        P.dma(dq(), xr, io['x1'][r0:r0 + 128, :], writes=['xr'])
        P.dma(dq(), pt, io['rs'][tt // 4][(tt % 4) * 128:(tt % 4) * 128 + 128, :], writes=['pt'])
        P.add('vector', lambda e: e.scalar_tensor_tensor(out=z, in0=xr, scalar=ALPHA, in1=pt, op0=ALU.mult, op1=ALU.add), reads=['xr', 'pt'], writes=['z'])
        _ln_ops(P, z, 'z', xo, 'xo', G, Bt, junk, st6, 'a')
        P.dma(dq(), io['x2'][r0:r0 + 128, :], xo, reads=['xo'])
        if not last:
            for g4 in range(4):
                pi = nps()
                for q in range(4):
                    kc = 4 * g4 + q
                    P.add('tensor', lambda e, pi=pi, q=q, kc=kc: e.matmul(ps[pi][:, q * 128:(q + 1) * 128], lhsT=xo[:, kc * 128:(kc + 1) * 128], rhs=ident, start=True, stop=True),
                          reads=['xo', 'ident'], writes=['ps%d' % pi])
                P.add('scalar', lambda e, pi=pi, g4=g4: e.copy(out=xTb[:, 4 * g4:4 * g4 + 4, :], in_=ps[pi][:, :].rearrange("p (q t) -> p q t", t=128)), reads=['ps%d' % pi], writes=['xTb'])
            for k in range(4):
                P.dma(dq(), io['x2T'][k].rearrange("(kc p) t -> p kc t", p=128)[:, :, r0:r0 + 128], xTb[:, 4 * k:4 * k + 4, :], reads=['xTb'])


def build_fused(nlayers=2, stop=99):
    nc = bass.Bass("TRN2", target_bir_lowering=False)
    shapes = {
        'xT': ([D, S], F32), 'xTo': ([D, 1024], F32), 'xown': ([1024, D], F32),
        'pos': ([1, S], I32), 'poso': ([1, 1024], I32), 'cst': ([128, 8], F32),
        'cmask': ([128, 4, 512], BF16), 'dmask': ([128, 20, 512], BF16), 'negc': ([128, 512], F32),
        'ident': ([128, 128], BF16), 'identf': ([128, 128], F32), 'oneh': ([128, 4], F32),
        'rw': ([D, 16], F32), 'rb': ([1, 16], F32),
    }
    for l in range(nlayers):
        shapes.update({
            'wq%d' % l: ([D, NQT * 128], F32), 'wv%d' % l: ([D, 512], F32), 'wa%d' % l: ([D, NAO * 128], F32), 'wiw%d' % l: ([D, 16], F32),
            'dl%d' % l: ([1, 256], F32), 'dg%d' % l: ([1, 128], F32), 'lc%d' % l: ([128, 2], F32),
            'wo%d' % l: ([D, D], F32), 'lng%d' % l: ([1, D], F32), 'lnb%d' % l: ([1, D], F32),
            'wg%d' % l: ([4, D, 1024], F32), 'wu%d' % l: ([4, D, 1024], F32), 'wd%d' % l: ([4, 1024, D], F32),
            'lng2_%d' % l: ([1, D], F32), 'lnb2_%d' % l: ([1, D], F32)})

    class Lazy(dict):
        def __missing__(self, k):
            shp, dt = shapes[k]
            v = nc.dram_tensor(k, shp, dt, kind="ExternalInput").ap()
            self[k] = v
            return v
    ein = Lazy()

    def scr(name, shape, dt=F32):
        return nc.dram_tensor(name, shape, dt).ap()
    out = nc.dram_tensor("out", [1024, D], F32, kind="ExternalOutput").ap()
    s_qt = scr("s_qt", [9, 128, S], BF16); s_vt = scr("s_vt", [S, 512], BF16)
    s_aq = scr("s_aq", [4, 128, 1024], BF16); s_iq = scr("s_iq", [16, 128, 1024], BF16); s_iw = scr("s_iw", [1024, 16])
    s_obd = [scr("s_obd%d" % k, [512, 384]) for k in range(8)]; s_oa = scr("s_oa", [1024, 512]); s_og = [scr("s_og%d" % k, [2048, 384]) for k in range(8)]
    s_x1 = scr("s_x1", [1024, D]); s_x1T = [scr("s_x1T%d" % k, [512, 1024], BF16) for k in range(4)]; s_x1Tg = [scr("s_x1Tg%d" % k, [2048, 1024], BF16) for k in range(4)]
    s_gates = scr("s_gates", [1024, 16]); s_gg = scr("s_gg", [4 * 1024, 16])
    s_part = [scr("s_part%d" % k, [2048, D]) for k in range(2)]; s_rs = [scr("s_rs%d" % k, [512, D]) for k in range(2)]
    s_x2 = scr("s_x2", [1024, D]); s_x2T = [scr("s_x2T%d" % k, [512, 1024], BF16) for k in range(4)]; s_xTg = [scr("s_xTg%d" % k, [2048, 1024], BF16) for k in range(4)]

    with ExitStack() as st:
        C = Ctx(nc, st)
        stage = 0

        def go():
            nonlocal stage
            stage += 1
            return stage <= stop
        for l in range(nlayers):
            last = (l == nlayers - 1)
            if go():
                io1 = {'wq': ein['wq%d' % l], 'wv': ein['wv%d' % l], 'wa': ein['wa%d' % l], 'wiw': ein['wiw%d' % l],
                       'pos': ein['pos'], 'poso': ein['poso'], 'cst': ein['cst'],
                       'qt': s_qt, 'vt': s_vt, 'aq': s_aq, 'iq': s_iq, 'iw': s_iw, 'xTg': s_xTg, 'x2T': s_x2T}
                if l == 0:
                    io1['xT'] = ein['xT']; io1['xTo'] = ein['xTo']
                stage_k1(C, io1, layer1=(l > 0))
                C.barrier()
            if go():
                io2 = {'qt': s_qt, 'vt': s_vt, 'aq': s_aq, 'iq': s_iq, 'iw': s_iw, 'o_bd': s_obd, 'oa': s_oa,
                       'cmask': ein['cmask'], 'dmask': ein['dmask'], 'negc': ein['negc'], 'ident': ein['ident'],
                       'dl': ein['dl%d' % l], 'dg': ein['dg%d' % l], 'lc': ein['lc%d' % l]}
                stage_k2(C, io2)
            if go():
                C.collectives([("AllGather", ALU.bypass, s_obd[k], s_og[k]) for k in range(8)])
            if go():
                io3 = {'og': s_og, 'oa': s_oa, 'xres': ein['xown'] if l == 0 else s_x2, 'x1': s_x1, 'x1T': s_x1T, 'gates': s_gates,
                       'wo': ein['wo%d' % l], 'lng': ein['lng%d' % l], 'lnb': ein['lnb%d' % l], 'rw': ein['rw'], 'rb': ein['rb'],
                       'identf': ein['identf'], 'oneh': ein['oneh']}
                stage_k3(C, io3)
            if go():
                C.collectives([("AllGather", ALU.bypass, s_x1T[k], s_x1Tg[k]) for k in range(4)] + [("AllGather", ALU.bypass, s_gates, s_gg)])
            if go():
                io4 = {'x1Tg': s_x1Tg, 'gg': s_gg, 'wg': ein['wg%d' % l], 'wu': ein['wu%d' % l], 'wd': ein['wd%d' % l], 'part': s_part, 'oneh': ein['oneh']}
                stage_k4(C, io4)
            if go():
                C.collectives([("ReduceScatter", ALU.add, s_part[k], s_rs[k]) for k in range(2)])
            if go():
                io5 = {'x1': s_x1, 'rs': s_rs, 'x2': out if last else s_x2, 'x2T': s_x2T, 'lng': ein['lng2_%d' % l], 'lnb': ein['lnb2_%d' % l], 'identf': ein['identf']}
                stage_k5(C, io5, last)
                if not last:
                    C.collectives([("AllGather", ALU.bypass, s_x2T[k], s_xTg[k]) for k in range(4)])
        C.P.emit(st)
    nc._ext_names = list(ein.keys())
    return nc


_NC = {}


def kernel(x, positions, w_in, w_out, diff_lambda, diff_norm_g, ln_mix_g, ln_mix_b,
           router_w, router_bias, w_gate, w_up, w_down, ln_ffn_g, ln_ffn_b):
    import math
    x = np.asarray(x, np.float32)
    positions = np.asarray(positions)
    Bn, Sn, Dn = x.shape
    nl = int(np.asarray(w_in).shape[0])
    if 'nc' not in _NC:
        _NC['nc'] = build_fused(nl)
    nc = _NC['nc']
    f32 = lambda a: np.ascontiguousarray(np.asarray(a, np.float32))
    identf = np.eye(128, dtype=np.float32)
    in_maps = []
    for c in range(8):
        b, j = c // 4, c % 4
        own = own_tokens(j)
        xT = np.ascontiguousarray(x[b].T)
        k2c = k2_consts(j, np.zeros(256, np.float32), np.zeros(128, np.float32), 0)
        oneh = np.zeros((128, 4), np.float32)
        oneh[:, j] = 1.0
        m = {'xT': xT, 'xTo': np.ascontiguousarray(xT[:, own]), 'xown': np.ascontiguousarray(x[b][own]),
             'pos': np.ascontiguousarray(positions[b][None, :].astype(np.int32)),
             'poso': np.ascontiguousarray(positions[b][None, own].astype(np.int32)),
             'cst': k1_consts(), 'cmask': k2c['cmask'], 'dmask': k2c['dmask'], 'negc': k2c['negc'], 'ident': k2c['ident'],
             'identf': identf, 'oneh': oneh, 'rw': f32(router_w), 'rb': f32(np.asarray(router_bias)[None, :])}
        for l in range(nl):
            wl = np.asarray(w_in[l], np.float32)
            qcols, vcols, acols, iwcols = k1_cols(j)
            m['wq%d' % l] = np.ascontiguousarray(wl[:, qcols]); m['wv%d' % l] = np.ascontiguousarray(wl[:, vcols])
            m['wa%d' % l] = np.ascontiguousarray(wl[:, acols]); m['wiw%d' % l] = np.ascontiguousarray(wl[:, iwcols])
            lam_init = 0.8 - 0.6 * math.exp(-0.3 * l)
            lc = np.zeros((128, 2), np.float32); lc[:, 0] = lam_init; lc[:, 1] = 1.0 - lam_init
            m['dl%d' % l] = f32(np.asarray(diff_lambda[l]).reshape(1, 256)); m['dg%d' % l] = f32(np.asarray(diff_norm_g[l]).reshape(1, 128)); m['lc%d' % l] = lc
            m['wo%d' % l] = f32(w_out[l]); m['lng%d' % l] = f32(np.asarray(ln_mix_g[l])[None, :]); m['lnb%d' % l] = f32(np.asarray(ln_mix_b[l])[None, :])
            m['wg%d' % l] = f32(w_gate[l][4 * j:4 * j + 4]); m['wu%d' % l] = f32(w_up[l][4 * j:4 * j + 4]); m['wd%d' % l] = f32(w_down[l][4 * j:4 * j + 4])
            m['lng2_%d' % l] = f32(np.asarray(ln_ffn_g[l])[None, :]); m['lnb2_%d' % l] = f32(np.asarray(ln_ffn_b[l])[None, :])
        in_maps.append(m)
    res = run_bass_kernel_spmd(nc, in_maps, core_ids=list(range(8)))
    out = np.zeros((Bn, Sn, Dn), np.float32)
    for c in range(8):
        b, j = c // 4, c % 4
        out[b, own_tokens(j)] = np.asarray(res.results[c]['out'])
    return out
```

```python
import numpy as np
import concourse.bass as bass
import concourse.mybir as mybir
from concourse.bass_utils import run_bass_kernel_spmd

F32 = mybir.dt.float32
BF16 = mybir.dt.bfloat16
I32 = mybir.dt.int32
AF = mybir.ActivationFunctionType
ALU = mybir.AluOpType
AX = mybir.AxisListType

COMPUTE = ('tensor', 'scalar', 'vector', 'gpsimd')
DMAQ = ('sync', 'scalar', 'gpsimd')


class _Op:
    __slots__ = ('eng', 'fn', 'reads', 'writes', 'dma', 'need', 'signal', 'val', 'slot', 'cc', 'bar')


class Prog:
    def __init__(self, nc, nslots=4):
        self.nc = nc
        self.ops = []
        self.nslots = nslots

    def add(self, eng, fn, reads=(), writes=(), dma=False, cc=False):
        o = _Op()
        o.cc = cc
        o.bar = False
        o.eng, o.fn, o.reads, o.writes, o.dma = eng, fn, tuple(reads), tuple(writes), dma
        o.need = set()
        o.signal = dma
        o.val = None
        o.slot = None
        self.ops.append(o)
        return o

    def dma(self, eng, out, in_, reads=(), writes=()):
        return self.add(eng, lambda e: e.dma_start(out=out, in_=in_), reads, writes, dma=True)

    def barrier(self, fn):
        o = self.add('vector', fn)
        o.bar = True
        o.signal = True
        return o

    def _assign_slots(self):
        dcount = {q: 0 for q in DMAQ}
        for o in self.ops:
            if o.dma:
                if o.cc:
                    o.slot = ('cc', 0)
                else:
                    j = dcount[o.eng]
                    dcount[o.eng] += 1
                    o.slot = (o.eng, j % self.nslots)

    def _barriers(self):
        ops = self.ops
        engs = ('sync', 'scalar', 'vector', 'gpsimd', 'tensor')
        bars = [i for i, o in enumerate(ops) if o.bar]
        for bi in bars:
            b = ops[bi]
            lastc = {}
            lasts = {}
            for i in range(bi):
                o = ops[i]
                if o.dma:
                    lasts[o.slot] = i
                else:
                    lastc[o.eng] = i
            for i in list(lastc.values()) + list(lasts.values()):
                b.need.add(i)
            seen = set()
            for i in range(bi + 1, len(ops)):
                o = ops[i]
                if o.eng not in seen:
                    seen.add(o.eng)
                    o.need.add(bi)
                    if len(seen) == len(engs):
                        break

    def _deps(self):
        last_w = {}
        rd = {}
        ops = self.ops
        for i, c in enumerate(ops):
            raw, other = set(), set()
            for k in c.reads:
                if k in last_w:
                    raw.add(last_w[k])
            for k in c.writes:
                if k in last_w:
                    other.add(last_w[k])
                for r in rd.get(k, ()):
                    other.add(r)
            for p in raw | other:
                if p == i:
                    continue
                po = ops[p]
                if po.dma or c.dma:
                    c.need.add(p)
                elif po.eng == c.eng:
                    if c.eng != 'tensor':
                        c.need.add(p)
                else:
                    c.need.add(p)
            for k in c.reads:
                rd.setdefault(k, []).append(i)
            for k in c.writes:
                last_w[k] = i
                rd[k] = []
        self._assign_slots()
        self._barriers()
        for c in ops:
            for p in c.need:
                ops[p].signal = True

    def emit(self, stack):
        nc = self.nc
        self._deps()
        ops = self.ops
        sem = {}
        for e in COMPUTE:
            sem[e] = stack.enter_context(nc.semaphore('s_' + e))
        for q in DMAQ:
            for s in range(self.nslots):
                sem[(q, s)] = stack.enter_context(nc.semaphore('d_%s%d' % (q, s)))
        if any(o.cc for o in ops):
            sem[('cc', 0)] = stack.enter_context(nc.semaphore('s_cc'))
        cnt = {e: 0 for e in COMPUTE}
        dcount = {q: 0 for q in DMAQ}
        slotcnt = {}
        prev_in_slot = {}
        for i, o in enumerate(ops):
            if o.dma:
                s = o.slot
                inc = 1 if o.cc else 16
                if s in prev_in_slot:
                    o.need.add(prev_in_slot[s])
                prev_in_slot[s] = i
                slotcnt[s] = slotcnt.get(s, 0) + inc
                o.val = slotcnt[s]
            elif o.signal:
                cnt[o.eng] += 1
                o.val = cnt[o.eng]
        final_slots = dict(slotcnt)
        block = stack.enter_context(nc.Block())

        def run_engine(ename, e):
            waited = {}
            for o in ops:
                if o.eng != ename:
                    continue
                req = {}
                for p in o.need:
                    po = ops[p]
                    sk = po.slot if po.dma else po.eng
                    if po.val > req.get(sk, 0):
                        req[sk] = po.val
                for sk, v in req.items():
                    if waited.get(sk, 0) >= v:
                        continue
                    e.wait_ge(sem[sk], v)
                    waited[sk] = v
                ins = o.fn(e)
                if o.dma:
                    ins.then_inc(sem[o.slot], 1 if o.cc else 16)
                elif o.signal:
                    ins.then_inc(sem[o.eng], 1)
            if ename == 'sync':
                for sk, v in final_slots.items():
                    if waited.get(sk, 0) < v:
                        e.wait_ge(sem[sk], v)

        @block.sync
        def _(e):
            run_engine('sync', e)

        @block.scalar
        def _(e):
            run_engine('scalar', e)

        @block.vector
        def _(e):
            run_engine('vector', e)

        @block.gpsimd
        def _(e):
            run_engine('gpsimd', e)

        @block.tensor
        def _(e):
            run_engine('tensor', e)


import numpy as np

THETA = 10000.0
OFF = {}
_sizes = (512, 128, 128, 1024, 64, 16, 512, 512, 512, 512, 512, 512, 512, 512, 512)
_names = ('a_q', 'a_k', 'a_v', 'i_q', 'i_k', 'i_w', 'b_q', 'b_k', 'b_v', 'c_q', 'c_k', 'c_v', 'd_q', 'd_k', 'd_v')
_o = 0
for _n, _s in zip(_names, _sizes):
    OFF[_n] = _o
    _o += _s

P64 = np.concatenate([np.arange(0, 32), np.arange(64, 96), np.arange(32, 64), np.arange(96, 128)])
IK2 = np.concatenate([np.arange(0, 32), np.arange(0, 32), np.arange(32, 64), np.arange(32, 64)])


def k1_consts():
    p = np.arange(128)
    c = np.zeros((128, 8), np.float32)
    c[:, 0] = THETA ** (-2.0 * (p % 64) / 128.0)
    c[:, 1] = np.where(p < 64, -1.0, 1.0)
    c[:, 2] = THETA ** (-2.0 * (p % 32) / 64.0)
    c[:, 3] = c[:, 1]
    c[:, 4] = ((p % 64) < 32).astype(np.float32)
    c[:, 5] = 1.0 - c[:, 4]
    return c


def k1_cols(j):
    qcols = np.concatenate([
        OFF['b_q'] + 128 * j + P64, OFF['b_k'] + 128 * j + P64,
        OFF['c_q'] + 128 * j + np.arange(128), OFF['c_k'] + 128 * j + np.arange(128),
        OFF['d_q'] + 128 * j + np.arange(128), OFF['d_k'] + 128 * j + np.arange(128),
        OFF['a_k'] + np.arange(128), OFF['i_k'] + IK2])
    vcols = np.concatenate([OFF['b_v'] + 128 * j + np.arange(128), OFF['c_v'] + 128 * j + np.arange(128),
                            OFF['d_v'] + 128 * j + np.arange(128), OFF['a_v'] + np.arange(128)])
    acols = np.concatenate([OFF['a_q'] + np.arange(512)] + [OFF['i_q'] + 128 * pr + P64 for pr in range(8)])
    iwcols = OFF['i_w'] + np.arange(16)
    return qcols, vcols, acols, iwcols


def own_tokens(j):
    return np.concatenate([np.arange(128 * (4 * c + j), 128 * (4 * c + j) + 128) for c in range(8)])


def k1_inputs(xb, posb, w_in_l, j):
    qcols, vcols, acols, iwcols = k1_cols(j)
    xT = np.ascontiguousarray(xb.T)
    own = own_tokens(j)
    return {
        'xT': xT, 'xTo': np.ascontiguousarray(xT[:, own]),
        'wq': np.ascontiguousarray(w_in_l[:, qcols]), 'wv': np.ascontiguousarray(w_in_l[:, vcols]),
        'wa': np.ascontiguousarray(w_in_l[:, acols]), 'wiw': np.ascontiguousarray(w_in_l[:, iwcols]),
        'pos': np.ascontiguousarray(posb[None, :].astype(np.int32)),
        'poso': np.ascontiguousarray(posb[None, own].astype(np.int32)),
        'cst': k1_consts(),
    }


def k2_consts(j, dl_l, dg_l, layer):
    import ml_dtypes, math
    bf = ml_dtypes.bfloat16
    s = np.arange(128)[:, None]
    t = np.arange(512)[None, :]
    cm = np.stack([(s + 128 * d <= t) for d in range(4)], 1).astype(np.float32)
    dms = []
    for di in range(20):
        d = (128 * di - 384) + t - s
        m = ((d >= 0) & (d <= 128)).astype(np.float32) + ((d >= 0) & (d <= 512) & (d % 4 == 0)) + ((d >= 0) & (d <= 2048) & (d % 16 == 0))
        dms.append(m.astype(np.float32))
    dm = np.stack(dms, 1)
    tq = 128 * j + np.arange(128)[:, None]
    sk = np.arange(512)[None, :]
    negc = np.where(sk > tq, -1.0e30, 0.0).astype(np.float32)
    lam_init = 0.8 - 0.6 * math.exp(-0.3 * layer)
    lc = np.zeros((128, 2), np.float32)
    lc[:, 0] = lam_init
    lc[:, 1] = 1.0 - lam_init
    return {'cmask': cm.astype(bf), 'dmask': dm.astype(bf), 'negc': negc, 'ident': np.eye(128, dtype=np.float32).astype(bf),
            'dl': np.ascontiguousarray(dl_l.reshape(1, 256)), 'dg': np.ascontiguousarray(dg_l.reshape(1, 128)), 'lc': lc}


from contextlib import ExitStack


def _ln_ops(P, z, zkey, outt, outkey, G, Bt, junk, st6, tag):
    P.add('scalar', lambda e: e.activation(out=junk[:], in_=z[:], func=AF.Copy, accum_out=st6[:, 0:1]), reads=[zkey], writes=['junk', 'st0'])
    P.add('scalar', lambda e: e.activation(out=junk[:], in_=z[:], func=AF.Square, accum_out=st6[:, 1:2]), reads=[zkey], writes=['junk', 'st1'])
    P.add('vector', lambda e: e.tensor_scalar(out=st6[:, 2:3], in0=st6[:, 0:1], scalar1=1.0 / D, scalar2=None, op0=ALU.mult), reads=['st0'], writes=['st2'])
    P.add('vector', lambda e: e.tensor_tensor(out=st6[:, 3:4], in0=st6[:, 2:3], in1=st6[:, 2:3], op=ALU.mult), reads=['st2'], writes=['st3'])
    P.add('vector', lambda e: e.scalar_tensor_tensor(out=st6[:, 4:5], in0=st6[:, 1:2], scalar=1.0 / D, in1=st6[:, 3:4], op0=ALU.mult, op1=ALU.subtract), reads=['st1', 'st3'], writes=['st4'])
    P.add('vector', lambda e: e.tensor_scalar(out=st6[:, 4:5], in0=st6[:, 4:5], scalar1=1e-5, scalar2=None, op0=ALU.add), reads=['st4'], writes=['st4'])
    P.add('scalar', lambda e: e.activation(out=st6[:, 5:6], in_=st6[:, 4:5], func=AF.Sqrt), reads=['st4'], writes=['st5'])
    P.add('vector', lambda e: e.reciprocal(out=st6[:, 5:6], in_=st6[:, 5:6]), reads=['st5'], writes=['st5'])
    P.add('vector', lambda e: e.scalar_tensor_tensor(out=st6[:, 6:7], in0=st6[:, 2:3], scalar=-1.0, in1=st6[:, 5:6], op0=ALU.mult, op1=ALU.mult), reads=['st2', 'st5'], writes=['st6'])
    P.add('scalar', lambda e: e.activation(out=outt[:], in_=z[:], func=AF.Identity, scale=st6[:, 5:6], bias=st6[:, 6:7]), reads=[zkey, 'st5', 'st6'], writes=[outkey])
    P.add('gpsimd', lambda e: e.tensor_tensor(out=outt[:], in0=outt[:], in1=G[:], op=ALU.mult), reads=[outkey, 'G'], writes=[outkey])
    P.add('vector', lambda e: e.tensor_tensor(out=outt[:], in0=outt[:], in1=Bt[:], op=ALU.add), reads=[outkey, 'Bt'], writes=[outkey])


PI = float(np.pi)
S = 4096
D = 2048
NEG = -1.0e30
ALPHA = float(4 ** 0.25)
ARENA = 51200
NQT = 8
NAO = 12
QT_VARIANT = [1, 1, 0, 0, 0, 0, 0, 1]
QT_MASKED = [True, False, False, False, False, False, False, False]
AO_VARIANT = [0, 0, 0, 0] + [1] * 8
AO_MASKED = [False] * 4 + [True] * 8
RG = [[0, 1, 2, 3], [4, 5, 6, 7]]


class Ctx:
    def __init__(self, nc, st):
        self.nc = nc
        self.arena = st.enter_context(nc.sbuf_tensor("arena", [128, ARENA], F32))
        self.ps = [st.enter_context(nc.psum_tensor("ps%d" % i, [128, 512], F32)) for i in range(8)]
        self.P = Prog(nc)
        self.off = 0
        self.bar = self.arena[:, ARENA - 8:ARENA]
        self.cnt = {'dq': 0}

    def reset(self):
        self.off = 0

    def sb(self, name, shape, dt=F32):
        n = 1
        for s_ in shape[1:]:
            n *= s_
        esz = 2 if dt == BF16 else 4
        nf = (n * esz + 3) // 4
        nf = (nf + 7) // 8 * 8
        assert self.off + nf <= ARENA - 8, ("arena overflow", name, self.off, nf)
        v = self.arena[:, self.off:self.off + nf]
        self.off += nf
        if dt != F32:
            v = v.bitcast(dt)
        v = v[:, 0:n]
        if len(shape) == 3:
            v = v.rearrange("p (a b) -> p a b", b=shape[2])
        elif len(shape) == 4:
            v = v.rearrange("p (a b c) -> p a b c", b=shape[2], c=shape[3])
        return v

    def dq(self):
        self.cnt['dq'] += 1
        return ('sync', 'gpsimd')[self.cnt['dq'] % 2]

    def barrier(self):
        bar = self.bar
        self.P.barrier(lambda e: e.memset(bar, 0.0))
        self.reset()

    def collectives(self, items):
        self.barrier()
        for (kind, op, src, dst) in items:
            self.P.add('gpsimd', lambda e, kind=kind, op=op, src=src, dst=dst: e.collective_compute(kind, op, replica_groups=RG, ins=[src.opt()], outs=[dst.opt()]),
                       dma=True, cc=True)
        self.barrier()


def stage_k1(C, io, layer1):
    nc, P, ps = C.nc, C.P, C.ps
    sb = C.sb
    dq = C.dq
    wq, wv, wa, wiw, pos, poso, cst = io['wq'], io['wv'], io['wa'], io['wiw'], io['pos'], io['poso'], io['cst']
    qt, vt, aq, iq, iw = io['qt'], io['vt'], io['aq'], io['iq'], io['iw']
    wq_b = sb("wq_b", [128, 16, NQT * 128], BF16)
    wv_b = sb("wv_b", [128, 16, 512], BF16)
    wa_b = sb("wa_b", [128, 16, NAO * 128], BF16)
    wiw_b = sb("wiw_b", [128, 16, 16], BF16)
    wst = [sb("wst%d" % i, [128, 16, 128], F32) for i in range(2)]
    xs = [sb("xs%d" % i, [128, 8, 512], F32) for i in range(2)]
    xb = sb("xb", [128, 16, 512], BF16)
    xob = sb("xob", [128, 16, 128], BF16)
    cs = sb("cs", [128, 8], F32)
    posi = sb("posi", [128, 512], I32)
    posf = sb("posf", [128, 512], F32)
    ang = sb("ang", [128, 512], F32)
    ki = sb("ki", [128, 512], I32)
    kf = sb("kf", [128, 512], F32)
    tabC = [sb("tabC%d" % v, [128, 512], F32) for v in range(2)]
    tabS = [sb("tabS%d" % v, [128, 512], F32) for v in range(2)]
    t1 = [sb("t1_%d" % i, [128, 512], F32) for i in range(2)]
    t2 = [sb("t2_%d" % i, [128, 512], F32) for i in range(2)]
    rot = [sb("rot%d" % i, [128, 512], F32) for i in range(2)]
    ob = [sb("ob%d" % i, [128, 512], BF16) for i in range(4)]
    iwt = sb("iwt", [128, 16], F32)
    cnt = {'ps': 0, 'ob': 0, 'tt': 0, 'cv': 0}

    P.dma('sync', cs, cst[:, :], writes=['cs'])

    def load_w(src, ncols, dst, dname):
        src_v = src.rearrange("(kc p) c -> p kc c", p=128)
        for c0 in range(0, ncols, 128):
            cw = min(128, ncols - c0)
            i = cnt['cv'] % 2
            cnt['cv'] += 1
            w_ = wst[i]
            P.dma(dq(), w_[:, :, 0:cw], src_v[:, :, c0:c0 + cw], writes=['wst%d' % i])
            eng = ('gpsimd', 'vector')[i]
            P.add(eng, lambda e, w_=w_, c0=c0, cw=cw, dst=dst: e.tensor_copy(out=dst[:, :, c0:c0 + cw], in_=w_[:, :, 0:cw]),
                  reads=['wst%d' % i], writes=[dname])
    load_w(wq, NQT * 128, wq_b, 'wq_b')
    load_w(wv, 512, wv_b, 'wv_b')
    load_w(wa, NAO * 128, wa_b, 'wa_b')
    load_w(wiw, 16, wiw_b, 'wiw_b')

    def tables(pos_ap, n):
        P.dma('sync', posi[:, 0:n], pos_ap.partition_broadcast(128), writes=['posi'])
        P.add('vector', lambda e: e.tensor_copy(out=posf[:, 0:n], in_=posi[:, 0:n]), reads=['posi'], writes=['posf'])
        for v in range(2):
            inv = cs[:, 2 * v:2 * v + 1]
            for which, off, tab, tkey in (('S', 0.0, tabS[v], 'tabS%d' % v), ('C', PI / 2, tabC[v], 'tabC%d' % v)):
                P.add('vector', lambda e, inv=inv, off=off: e.tensor_scalar(out=ang[:, 0:n], in0=posf[:, 0:n], scalar1=inv, scalar2=off, op0=ALU.mult, op1=ALU.add),
                      reads=['posf', 'cs'], writes=['ang'])
                P.add('vector', lambda e: e.tensor_scalar(out=ki[:, 0:n], in0=ang[:, 0:n], scalar1=1.0 / (2 * PI), scalar2=None, op0=ALU.mult),
                      reads=['ang'], writes=['ki'])
                P.add('vector', lambda e: e.tensor_copy(out=kf[:, 0:n], in_=ki[:, 0:n]), reads=['ki'], writes=['kf'])
                P.add('vector', lambda e: e.scalar_tensor_tensor(out=ang[:, 0:n], in0=kf[:, 0:n], scalar=-2 * PI, in1=ang[:, 0:n], op0=ALU.mult, op1=ALU.add),
                      reads=['kf', 'ang'], writes=['ang'])
                P.add('vector', lambda e: e.tensor_scalar(out=ang[:, 0:n], in0=ang[:, 0:n], scalar1=-3.14159, scalar2=3.14159, op0=ALU.max, op1=ALU.min),
                      reads=['ang'], writes=['ang'])
                if which == 'S':
                    P.add('scalar', lambda e, tab=tab: e.activation(out=tab[:, 0:n], in_=ang[:, 0:n], func=AF.Sin, scale=cs[:, 1:2]),
                          reads=['ang', 'cs'], writes=[tkey])
                else:
                    P.add('scalar', lambda e, tab=tab: e.activation(out=tab[:, 0:n], in_=ang[:, 0:n], func=AF.Sin),
                          reads=['ang'], writes=[tkey])

    def next_ps():
        i = cnt['ps'] % 8
        cnt['ps'] += 1
        return i

    def next_ob():
        i = cnt['ob'] % 4
        cnt['ob'] += 1
        return i

    def rope_evac(pi, n, variant, masked, dsts):
        p_ = ps[pi]
        k = cnt['tt'] % 2
        cnt['tt'] += 1
        Ct, Sg = tabC[variant], tabS[variant]
        ck, sk = 'tabC%d' % variant, 'tabS%d' % variant
        P.add('vector', lambda e: e.tensor_tensor(out=t1[k][:, 0:n], in0=p_[:, 0:n], in1=Ct[:, 0:n], op=ALU.mult),
              reads=['ps%d' % pi, ck], writes=['t1_%d' % k])
        P.add('vector', lambda e: e.tensor_tensor(out=t2[k][0:64, 0:n], in0=p_[64:128, 0:n], in1=Sg[0:64, 0:n], op=ALU.mult),
              reads=['ps%d' % pi, sk], writes=['t2_%da' % k])
        P.add('vector', lambda e: e.tensor_tensor(out=t2[k][64:128, 0:n], in0=p_[0:64, 0:n], in1=Sg[64:128, 0:n], op=ALU.mult),
              reads=['ps%d' % pi, sk], writes=['t2_%db' % k])
        if not masked:
            oi = next_ob()
            P.add('gpsimd', lambda e: e.tensor_tensor(out=ob[oi][:, 0:n], in0=t1[k][:, 0:n], in1=t2[k][:, 0:n], op=ALU.add),
                  reads=['t1_%d' % k, 't2_%da' % k, 't2_%db' % k], writes=['ob%d' % oi])
            P.dma(dq(), dsts[0], ob[oi][:, 0:n], reads=['ob%d' % oi])
        else:
            P.add('gpsimd', lambda e: e.tensor_tensor(out=rot[k][:, 0:n], in0=t1[k][:, 0:n], in1=t2[k][:, 0:n], op=ALU.add),
                  reads=['t1_%d' % k, 't2_%da' % k, 't2_%db' % k], writes=['rot%d' % k])
            for m in range(2):
                oi = next_ob()
                P.add('scalar', lambda e, oi=oi, m=m: e.activation(out=ob[oi][:, 0:n], in_=rot[k][:, 0:n], func=AF.Copy, scale=cs[:, 4 + m:5 + m]),
                      reads=['rot%d' % k, 'cs'], writes=['ob%d' % oi])
                P.dma(dq(), dsts[m], ob[oi][:, 0:n], reads=['ob%d' % oi])

    if not layer1:
        xT_v = io['xT'].rearrange("(kc p) t -> p kc t", p=128)
        xTo_v = io['xTo'].rearrange("(kc p) t -> p kc t", p=128)
    else:
        xTg_v = [a.rearrange("(q kc p) t -> q p kc t", q=4, p=128) for a in io['xTg']]
        xTl_v = [a.rearrange("(kc p) t -> p kc t", p=128) for a in io['x2T']]

    for tc in range(8):
        t0 = tc * 512
        if not layer1:
            for h in range(2):
                P.dma(dq(), xs[h], xT_v[:, 8 * h:8 * h + 8, t0:t0 + 512], writes=['xs%d' % h])
                if h == 0:
                    P.add('scalar', lambda e, h=h: e.copy(out=xb[:, 8 * h:8 * h + 8, :], in_=xs[h]), reads=['xs%d' % h], writes=['xb%d' % h])
                else:
                    P.add('gpsimd', lambda e, h=h: e.tensor_copy(out=xb[:, 8 * h:8 * h + 8, :], in_=xs[h]), reads=['xs%d' % h], writes=['xb%d' % h])
        else:
            for q in range(4):
                for k in range(4):
                    P.dma(dq(), xb[:, 4 * k:4 * k + 4, q * 128:(q + 1) * 128], xTg_v[k][q, :, :, tc * 128:(tc + 1) * 128], writes=['xb0', 'xb1'])
        tables(pos[:, t0:t0 + 512], 512)
        oidx = 0
        for ti in range(NQT):
            pi = next_ps()
            for kc in range(16):
                P.add('tensor', lambda e, pi=pi, kc=kc, ti=ti: e.matmul(ps[pi][:, :], lhsT=wq_b[:, kc, ti * 128:(ti + 1) * 128], rhs=xb[:, kc, :], start=(kc == 0), stop=(kc == 15)),
                      reads=['wq_b', 'xb0', 'xb1'], writes=['ps%d' % pi])
            nout = 2 if QT_MASKED[ti] else 1
            dsts = [qt[oidx + m, :, t0:t0 + 512] for m in range(nout)]
            rope_evac(pi, 512, QT_VARIANT[ti], QT_MASKED[ti], dsts)
            oidx += nout
        for tt in range(4):
            pi = next_ps()
            for kc in range(16):
                P.add('tensor', lambda e, pi=pi, kc=kc, tt=tt: e.matmul(ps[pi][:, :], lhsT=xb[:, kc, tt * 128:(tt + 1) * 128], rhs=wv_b[:, kc, :], start=(kc == 0), stop=(kc == 15)),
                      reads=['wv_b', 'xb0', 'xb1'], writes=['ps%d' % pi])
            oi = next_ob()
            P.add('scalar', lambda e, pi=pi, oi=oi: e.copy(out=ob[oi], in_=ps[pi][:, :]), reads=['ps%d' % pi], writes=['ob%d' % oi])
            P.dma(dq(), vt[t0 + tt * 128:t0 + (tt + 1) * 128, :], ob[oi], reads=['ob%d' % oi])

    for ob_i in range(8):
        o0 = ob_i * 128
        if not layer1:
            for h in range(2):
                P.dma(dq(), xs[h][:, :, 0:128], xTo_v[:, 8 * h:8 * h + 8, o0:o0 + 128], writes=['xs%d' % h])
                P.add('gpsimd', lambda e, h=h: e.tensor_copy(out=xob[:, 8 * h:8 * h + 8, :], in_=xs[h][:, :, 0:128]), reads=['xs%d' % h], writes=['xob%d' % h])
        else:
            for k in range(4):
                P.dma(dq(), xob[:, 4 * k:4 * k + 4, :], xTl_v[k][:, :, o0:o0 + 128], writes=['xob0', 'xob1'])
        tables(poso[:, o0:o0 + 128], 128)
        for ti in range(NAO):
            pi = next_ps()
            for kc in range(16):
                P.add('tensor', lambda e, pi=pi, kc=kc, ti=ti: e.matmul(ps[pi][:, 0:128], lhsT=wa_b[:, kc, ti * 128:(ti + 1) * 128], rhs=xob[:, kc, :], start=(kc == 0), stop=(kc == 15)),
                      reads=['wa_b', 'xob0', 'xob1'], writes=['ps%d' % pi])
            if ti < 4:
                dsts = [aq[ti, :, o0:o0 + 128]]
            else:
                pr = ti - 4
                dsts = [iq[2 * pr, :, o0:o0 + 128], iq[2 * pr + 1, :, o0:o0 + 128]]
            rope_evac(pi, 128, AO_VARIANT[ti], AO_MASKED[ti], dsts)
        pi = next_ps()
        for kc in range(16):
            P.add('tensor', lambda e, pi=pi, kc=kc: e.matmul(ps[pi][:, 0:16], lhsT=xob[:, kc, :], rhs=wiw_b[:, kc, :], start=(kc == 0), stop=(kc == 15)),
                  reads=['wiw_b', 'xob0', 'xob1'], writes=['ps%d' % pi])
        P.add('scalar', lambda e, pi=pi: e.copy(out=iwt, in_=ps[pi][:, 0:16]), reads=['ps%d' % pi], writes=['iwt'])
        P.dma(dq(), iw[o0:o0 + 128, :], iwt, reads=['iwt'])


def stage_k2(C, io):
    nc, P = C.nc, C.P
    sb = C.sb
    dq = C.dq
    qt, vt, aq, iq, iw = io['qt'], io['vt'], io['aq'], io['iq'], io['iw']
    o_bd, oa = io['o_bd'], io['oa']
    QA = sb("QA", [128, S], BF16)
    QB = sb("QB", [128, S], BF16)
    KK = sb("KK", [128, S], BF16)
    VV = sb("VV", [128, 32, 129], BF16)
    AQ = sb("AQ", [128, 8, 4, 128], BF16)
    IQ = sb("IQ", [128, 16, 1024], BF16)
    IWS = sb("IWS", [128, 8, 16], F32)
    score = sb("score", [128, S], F32)
    work = sb("work", [128, S], F32)
    selm = sb("selm", [128, S], BF16)
    maskT = sb("maskT", [128, 32, 128], BF16)
    cmask = sb("cmask_s", [128, 4, 512], BF16)
    dmask = sb("dmask_s", [128, 20, 512], BF16)
    negc = sb("negc_s", [128, 512], F32)
    ident = sb("ident_s", [128, 128], BF16)
    dl = sb("dl_s", [128, 256], F32)
    gsc = sb("gsc", [128, 128], F32)
    lc = sb("lc_s", [128, 2], F32)
    sm = sb("sm", [128, 8], F32)
    prod = sb("prod", [128, 64], F32)
    pT = [sb("pT%d" % i, [128, 512], BF16) for i in range(4)]
    rl = [sb("rl%d" % i, [128, 512], F32) for i in range(4)]
    acc = sb("acc", [128, 4, 129], F32)
    rec = sb("rec", [128, 4], F32)
    O1 = sb("O1", [128, 4, 128], F32)
    O2 = sb("O2", [128, 4, 128], F32)
    dd = sb("dd", [128, 4, 128], F32)
    sq = sb("sq", [128, 128], F32)
    ss = sb("ss", [128, 4], F32)
    m8 = sb("m8", [128, 8], F32)
    thr = sb("thr", [128, 1], F32)
    kmean = sb("kmean", [128, 16], F32)
    kmeanb = sb("kmeanb", [128, 16], BF16)
    gate = sb("gate", [128, 32, 16], F32)
    selw = sb("selw", [128, 32, 16], F32)
    g8 = sb("g8", [128, 8], F32)
    gthr = sb("gthr", [128, 1], F32)
    pst = [C.ps[0], C.ps[1], C.ps[6], C.ps[7]]
    pstk = ['pst0', 'pst1', 'px0', 'px1']
    po = [C.ps[2], C.ps[3], C.ps[4], C.ps[5]]
    px = [C.ps[6], C.ps[7]]
    pxi = [C.ps[6], C.ps[7], C.ps[0], C.ps[1]]
    pxik = ['px0', 'px1', 'pst0', 'pst1']
    cnt = {'pst': 0, 'mk': 0, 'px': 0, 'rl': 0}

    P.dma('sync', cmask, io['cmask'][:, :, :], writes=['cmask'])
    P.dma('gpsimd', dmask, io['dmask'][:, :, :], writes=['dmask'])
    P.dma('sync', negc, io['negc'][:, :], writes=['negc'])
    P.dma('sync', ident, io['ident'][:, :], writes=['ident'])
    P.dma('sync', dl, io['dl'].partition_broadcast(128), writes=['dl'])
    P.dma('sync', gsc, io['dg'].partition_broadcast(128), writes=['gsc'])
    P.dma('sync', lc, io['lc'][:, :], writes=['lc'])
    for i in range(2):
        P.add('vector', lambda e, i=i: e.tensor_tensor(out=prod, in0=dl[:, 128 * i:128 * i + 64], in1=dl[:, 128 * i + 64:128 * i + 128], op=ALU.mult),
              reads=['dl'], writes=['prod'])
        P.add('vector', lambda e, i=i: e.reduce_sum(out=sm[:, i:i + 1], in_=prod, axis=AX.X), reads=['prod'], writes=['sm%d' % i])
        P.add('scalar', lambda e, i=i: e.activation(out=sm[:, 2 + i:3 + i], in_=sm[:, i:i + 1], func=AF.Exp), reads=['sm%d' % i], writes=['sm%d' % (2 + i)])
    P.add('vector', lambda e: e.tensor_tensor(out=sm[:, 4:5], in0=sm[:, 2:3], in1=sm[:, 3:4], op=ALU.subtract), reads=['sm2', 'sm3'], writes=['sm4'])
    P.add('vector', lambda e: e.tensor_tensor(out=sm[:, 4:5], in0=sm[:, 4:5], in1=lc[:, 0:1], op=ALU.add), reads=['sm4', 'lc'], writes=['sm4'])
    P.add('vector', lambda e: e.tensor_scalar(out=sm[:, 5:6], in0=sm[:, 4:5], scalar1=-1.0, scalar2=None, op0=ALU.mult), reads=['sm4'], writes=['sm5'])
    P.add('vector', lambda e: e.tensor_scalar(out=gsc, in0=gsc, scalar1=lc[:, 1:2], scalar2=None, op0=ALU.mult), reads=['gsc', 'lc'], writes=['gsc'])

    def load_qt(dst, idx, key):
        P.dma(dq(), dst[:, 0:2048], qt[idx, :, 0:2048], writes=[key])
        P.dma(dq(), dst[:, 2048:4096], qt[idx, :, 2048:4096], writes=[key])

    def load_v(m):
        P.dma(dq(), VV[:, :, 0:128], vt.rearrange("(blk p) c -> p blk c", p=128)[:, :, m * 128:(m + 1) * 128], writes=['VV'])
        P.add('gpsimd', lambda e: e.memset(VV[:, :, 128:129], 1.0), writes=['VVone'])

    def attn_chunk(q_ap, qkeys, blocks, scale, fin):
        P.add('gpsimd', lambda e: e.memset(acc, 0.0), writes=['acc'])
        DEPTH = 3
        n = len(blocks)
        bufs = {}

        def emit_qk(bi):
            blk = blocks[bi]
            i = cnt['pst'] % 4
            cnt['pst'] += 1
            bufs[bi] = i
            P.add('tensor', lambda e, i=i, blk=blk: e.matmul(pst[i][:, :], lhsT=blk['k'], rhs=q_ap, start=True, stop=True),
                  reads=list(qkeys) + list(blk['kkeys']), writes=[pstk[i]])
            P.add('scalar', lambda e, i=i: e.activation(out=pT[i], in_=pst[i][:, :], func=AF.Exp, scale=scale),
                  reads=[pstk[i]], writes=['pT%d' % i])
            if blk.get('mask') is not None:
                for (lo, hi, m_ap, mkeys) in blk['mask']:
                    eng = ('vector', 'gpsimd')[cnt['mk'] % 2]
                    cnt['mk'] += 1
                    P.add(eng, lambda e, i=i, lo=lo, hi=hi, m_ap=m_ap: e.tensor_tensor(out=pT[i][:, lo:hi], in0=pT[i][:, lo:hi], in1=m_ap, op=ALU.mult),
                          reads=['pT%d' % i] + list(mkeys), writes=['pT%d' % i])

        def emit_pv(bi):
            blk = blocks[bi]
            i = bufs[bi]
            for sub in range(4):
                P.add('tensor', lambda e, i=i, sub=sub, blk=blk: e.matmul(po[sub][:, 0:129], lhsT=pT[i][:, sub * 128:(sub + 1) * 128], rhs=blk['v'],
                                                                         start=blk['gs'], stop=blk['ge']),
                      reads=['pT%d' % i, 'VV', 'VVone'], writes=['po%d' % sub])
            if blk['ge']:
                for sub in range(4):
                    w = blk['w'][sub]
                    wkeys = [] if isinstance(w, float) else ['selw']
                    P.add('vector', lambda e, sub=sub, w=w: e.scalar_tensor_tensor(out=acc[:, sub, :], in0=po[sub][:, 0:129], scalar=w, in1=acc[:, sub, :], op0=ALU.mult, op1=ALU.add),
                          reads=['po%d' % sub, 'acc'] + wkeys, writes=['acc'])

        for t_ in range(n + DEPTH):
            if t_ < n:
                emit_qk(t_)
            if t_ - DEPTH >= 0:
                emit_pv(t_ - DEPTH)
        fin()

    def normalize(dst, dkey):
        P.add('vector', lambda e: e.reciprocal(out=rec, in_=acc[:, :, 128]), reads=['acc'], writes=['rec'])
        for sub in range(4):
            P.add('vector', lambda e, sub=sub: e.tensor_scalar(out=dst[:, sub, :], in0=acc[:, sub, 0:128], scalar1=rec[:, sub:sub + 1], scalar2=None, op0=ALU.mult),
                  reads=['acc', 'rec'], writes=[dkey])

    def causal_blocks(tc, kbuf_key):
        blocks = []
        nb = 4 * tc + 4
        for kb in range(nb):
            d = kb - 4 * tc
            mask = None
            if d >= 0:
                mask = [(0, 512, cmask[:, d, :], ['cmask'])]
            blocks.append(dict(k=KK[:, kb * 128:(kb + 1) * 128], kkeys=[kbuf_key], v=VV[:, kb, :], mask=mask,
                               gs=(kb == 0), ge=(kb == nb - 1), w=[1.0] * 4))
        return blocks

    def store_o(tc, m):
        for d_ in range(4):
            R = d_ * 1024 + tc * 128
            P.dma(dq(), o_bd[R // 512][R % 512:R % 512 + 128, m * 128:(m + 1) * 128], O1[:, d_, :], reads=['O1'])

    load_qt(QA, 0, 'QA')
    load_qt(QB, 1, 'QB')
    load_qt(KK, 2, 'KK')
    load_v(0)
    for tc in range(8):
        t0 = tc * 512
        attn_chunk(QA[:, t0:t0 + 512], ['QA'], causal_blocks(tc, 'KK'), 64 ** -0.5, lambda: normalize(O1, 'O1'))
        attn_chunk(QB[:, t0:t0 + 512], ['QB'], causal_blocks(tc, 'KK'), 64 ** -0.5, lambda: normalize(O2, 'O2'))
        P.add('vector', lambda e: e.scalar_tensor_tensor(out=dd, in0=O2, scalar=sm[:, 5:6], in1=O1, op0=ALU.mult, op1=ALU.add),
              reads=['O1', 'O2', 'sm5'], writes=['dd'])
        for sub in range(4):
            P.add('scalar', lambda e, sub=sub: e.activation(out=sq, in_=dd[:, sub, :], func=AF.Square, accum_out=ss[:, sub:sub + 1]),
                  reads=['dd'], writes=['sq', 'ss'])
        P.add('vector', lambda e: e.tensor_scalar(out=ss, in0=ss, scalar1=1.0 / 128, scalar2=1e-5, op0=ALU.mult, op1=ALU.add), reads=['ss'], writes=['ss'])
        P.add('scalar', lambda e: e.activation(out=ss, in_=ss, func=AF.Sqrt), reads=['ss'], writes=['ss'])
        P.add('vector', lambda e: e.reciprocal(out=ss, in_=ss), reads=['ss'], writes=['ss'])
        for sub in range(4):
            P.add('vector', lambda e, sub=sub: e.scalar_tensor_tensor(out=O1[:, sub, :], in0=dd[:, sub, :], scalar=ss[:, sub:sub + 1], in1=gsc, op0=ALU.mult, op1=ALU.mult),
                  reads=['dd', 'ss', 'gsc'], writes=['O1'])
        store_o(tc, 0)

    load_qt(QA, 3, 'QA')
    load_qt(KK, 4, 'KK')
    load_v(1)
    P.add('vector', lambda e: e.tensor_reduce(out=kmean, in_=KK.rearrange("p (n k) -> p n k", k=256), axis=AX.X, op=ALU.add), reads=['KK'], writes=['kmean'])
    P.add('vector', lambda e: e.tensor_scalar(out=kmeanb, in0=kmean, scalar1=1.0 / 256, scalar2=None, op0=ALU.mult), reads=['kmean'], writes=['kmeanb'])
    for qb in range(32):
        pi = cnt['px'] % 2
        cnt['px'] += 1
        own = qb // 2
        P.add('tensor', lambda e, pi=pi, qb=qb: e.matmul(px[pi][:, 0:16], lhsT=QA[:, qb * 128:(qb + 1) * 128], rhs=kmeanb, start=True, stop=True),
              reads=['QA', 'kmeanb'], writes=['px%d' % pi])
        P.add('vector', lambda e, pi=pi, qb=qb: e.tensor_copy(out=gate[:, qb, :], in_=px[pi][:, 0:16]), reads=['px%d' % pi], writes=['gate'])
        P.add('vector', lambda e, qb=qb, own=own: e.memset(gate[:, qb, own:16], NEG), reads=['gate'], writes=['gate'])
        P.add('vector', lambda e, qb=qb: e.max(out=g8, in_=gate[:, qb, :]), reads=['gate'], writes=['g8'])
        P.add('vector', lambda e: e.tensor_scalar(out=gthr, in0=g8[:, 2:3], scalar1=-1.0e29, scalar2=None, op0=ALU.max), reads=['g8'], writes=['gthr'])
        P.add('vector', lambda e, qb=qb: e.tensor_scalar(out=selw[:, qb, :], in0=gate[:, qb, :], scalar1=gthr[:, 0:1], scalar2=None, op0=ALU.is_ge),
              reads=['gate', 'gthr'], writes=['selw'])
    for tc in range(8):
        t0 = tc * 512
        blocks = []
        for n in range(2 * tc + 2):
            for half in range(2):
                kb = 2 * n + half
                d = kb - 4 * tc
                mask = [(0, 512, cmask[:, d, :], ['cmask'])] if d >= 0 else None
                if n < 2 * tc:
                    w = [selw[:, 4 * tc + sub, n:n + 1] for sub in range(4)]
                elif n == 2 * tc:
                    w = [1.0, 1.0, selw[:, 4 * tc + 2, n:n + 1], selw[:, 4 * tc + 3, n:n + 1]]
                else:
                    w = [1.0] * 4
                blocks.append(dict(k=KK[:, kb * 128:(kb + 1) * 128], kkeys=['KK'], v=VV[:, kb, :], mask=mask,
                                   gs=(half == 0), ge=(half == 1), w=w))
        attn_chunk(QA[:, t0:t0 + 512], ['QA'], blocks, 128 ** -0.5, lambda: normalize(O1, 'O1'))
        store_o(tc, 1)

    load_qt(QA, 5, 'QA')
    load_qt(KK, 6, 'KK')
    load_v(2)
    for tc in range(8):
        t0 = tc * 512
        blocks = []
        kbs = [kb for kb in range(32) if -384 <= t0 - kb * 128 <= 2048]
        for ii, kb in enumerate(kbs):
            di = (t0 - kb * 128 + 384) // 128
            blocks.append(dict(k=KK[:, kb * 128:(kb + 1) * 128], kkeys=['KK'], v=VV[:, kb, :], mask=[(0, 512, dmask[:, di, :], ['dmask'])],
                               gs=(ii == 0), ge=(ii == len(kbs) - 1), w=[1.0] * 4))
        attn_chunk(QA[:, t0:t0 + 512], ['QA'], blocks, 128 ** -0.5, lambda: normalize(O1, 'O1'))
        store_o(tc, 2)

    load_qt(KK, 7, 'KK')
    load_qt(QB, 8, 'QB')
    load_v(3)
    P.dma(dq(), AQ, aq.rearrange("h p (ob t) -> p ob h t", t=128), writes=['AQ'])
    for h4 in range(4):
        P.dma(dq(), IQ[:, 4 * h4:4 * h4 + 4, :], iq[4 * h4:4 * h4 + 4].rearrange("h p t -> p h t"), writes=['IQ'])
    P.dma(dq(), IWS, iw.rearrange("(ob p) h -> p ob h", p=128), writes=['IWS'])
    P.add('vector', lambda e: e.tensor_scalar(out=IWS, in0=IWS, scalar1=0.25, scalar2=None, op0=ALU.mult), reads=['IWS'], writes=['IWS'])
    for c in range(8):
        L = 512 * (c + 1)
        for h in range(16):
            for kc in range(c + 1):
                pi = cnt['rl'] % 4
                ri = cnt['rl'] % 4
                cnt['rl'] += 1
                P.add('tensor', lambda e, pi=pi, h=h, kc=kc, c=c: e.matmul(pxi[pi][:, :], lhsT=IQ[:, h, c * 128:(c + 1) * 128], rhs=QB[:, kc * 512:(kc + 1) * 512], start=True, stop=True),
                      reads=['IQ', 'QB'], writes=[pxik[pi]])
                P.add('scalar', lambda e, pi=pi, ri=ri: e.activation(out=rl[ri], in_=pxi[pi][:, :], func=AF.Relu, scale=0.125),
                      reads=[pxik[pi]], writes=['rl%d' % ri])
                sk = 'score%d' % kc
                if h == 0:
                    P.add('vector', lambda e, ri=ri, kc=kc, h=h, c=c: e.tensor_scalar(out=score[:, kc * 512:(kc + 1) * 512], in0=rl[ri], scalar1=IWS[:, c, h:h + 1], scalar2=None, op0=ALU.mult),
                          reads=['rl%d' % ri, 'IWS'], writes=[sk])
                else:
                    P.add('vector', lambda e, ri=ri, kc=kc, h=h, c=c: e.scalar_tensor_tensor(out=score[:, kc * 512:(kc + 1) * 512], in0=rl[ri], scalar=IWS[:, c, h:h + 1], in1=score[:, kc * 512:(kc + 1) * 512], op0=ALU.mult, op1=ALU.add),
                          reads=['rl%d' % ri, 'IWS', sk], writes=[sk])
        P.add('vector', lambda e, c=c: e.tensor_tensor(out=score[:, c * 512:(c + 1) * 512], in0=score[:, c * 512:(c + 1) * 512], in1=negc, op=ALU.add),
              reads=['score%d' % c, 'negc'], writes=['score%d' % c])
        skeys = ['score%d' % kc for kc in range(c + 1)]
        for r in range(32):
            src = score if r == 0 else work
            srck = skeys if r == 0 else ['work']
            P.add('vector', lambda e, src=src, L=L: e.max(out=m8, in_=src[:, 0:L]), reads=srck, writes=['m8'])
            if r < 31:
                P.add('vector', lambda e, src=src, L=L: e.match_replace(out=work[:, 0:L], in_to_replace=m8, in_values=src[:, 0:L], imm_value=-3.0e38),
                      reads=srck + ['m8'], writes=['work'])
        P.add('vector', lambda e: e.tensor_scalar(out=thr, in0=m8[:, 7:8], scalar1=-1.0e29, scalar2=None, op0=ALU.max), reads=['m8'], writes=['thr'])
        P.add('vector', lambda e, L=L: e.tensor_scalar(out=selm[:, 0:L], in0=score[:, 0:L], scalar1=thr[:, 0:1], scalar2=None, op0=ALU.is_ge),
              reads=skeys + ['thr'], writes=['selm'])
        nb = 4 * (c + 1)
        for g4 in range(c + 1):
            pi = cnt['px'] % 2
            cnt['px'] += 1
            for q in range(4):
                kb = 4 * g4 + q
                P.add('tensor', lambda e, pi=pi, q=q, kb=kb: e.matmul(px[pi][:, q * 128:(q + 1) * 128], lhsT=selm[:, kb * 128:(kb + 1) * 128], rhs=ident, start=True, stop=True),
                      reads=['selm', 'ident'], writes=['px%d' % pi])
            P.add('scalar', lambda e, pi=pi, g4=g4: e.copy(out=maskT[:, 4 * g4:4 * g4 + 4, :], in_=px[pi][:, :].rearrange("p (q t) -> p q t", t=128)),
                  reads=['px%d' % pi], writes=['maskT'])
        blocks = []
        for kb in range(nb):
            mask = [(hh * 128, (hh + 1) * 128, maskT[:, kb, :], ['maskT']) for hh in range(4)]
            blocks.append(dict(k=KK[:, kb * 128:(kb + 1) * 128], kkeys=['KK'], v=VV[:, kb, :], mask=mask,
                               gs=(kb == 0), ge=(kb == nb - 1), w=[1.0] * 4))
        attn_chunk(AQ[:, c, :, :], ['AQ'], blocks, 128 ** -0.5, lambda: normalize(O1, 'O1'))
        P.dma(dq(), oa[c * 128:(c + 1) * 128, :].rearrange("p (h d) -> p h d", d=128), O1, reads=['O1'])


def stage_k3(C, io):
    nc, P, ps = C.nc, C.P, C.ps
    sb = C.sb
    dq = C.dq
    og, oa, xres = io['og'], io['oa'], io['xres']
    x1, x1T, gates = io['x1'], io['x1T'], io['gates']
    wob = sb("wob", [128, 16, D], BF16)
    stg = [sb("stg%d" % i, [128, 2048], F32) for i in range(2)]
    G = sb("G", [128, D]); Bt = sb("Bt", [128, D])
    rwf = sb("rwf", [128, 16, 16]); rbt = sb("rbt", [128, 16])
    ident = sb("identf_s", [128, 128])
    oneh = sb("oneh", [128, 4])
    xr = sb("xr", [128, D]); z = sb("z", [128, D]); junk = sb("junk", [128, D]); xo = sb("xo", [128, D])
    mixt = sb("mixt", [128, D])
    cand = [sb("cand%d" % i, [128, 4, 384]) for i in range(4)]
    mixb = sb("mixb", [128, 16, 128], BF16)
    st6 = sb("st6", [128, 8])
    xTf = sb("xTf", [128, 16, 128]); xTb = sb("xTb", [128, 16, 128], BF16)
    aff = sb("aff", [128, 16]); sel = sb("sel", [128, 16]); tmp = sb("tmp", [128, 16]); eq = sb("eq", [128, 16])
    m1 = sb("m1", [128, 4]); m2 = sb("m2", [128, 4]); gs = sb("gs", [128, 4]); gm = sb("gm", [128, 1]); oh = sb("oh", [128, 4])
    msk = sb("msk", [128, 16]); gsum = sb("gsum", [128, 1]); gout = sb("gout", [128, 16])
    cnt = {'ps': 0, 'stg': 0}

    def nps():
        i = cnt['ps'] % 8
        cnt['ps'] += 1
        return i
    P.dma('sync', G, io['lng'].partition_broadcast(128), writes=['G'])
    P.dma('sync', Bt, io['lnb'].partition_broadcast(128), writes=['Bt'])
    P.dma('sync', rwf, io['rw'].rearrange("(kc p) e -> p kc e", p=128), writes=['rwf'])
    P.dma('sync', rbt, io['rb'].partition_broadcast(128), writes=['rbt'])
    P.dma('sync', ident, io['identf'][:, :], writes=['ident'])
    P.dma('sync', oneh, io['oneh'][:, :], writes=['oneh'])
    wo_v = io['wo'].rearrange("(kc p) c -> p kc c", p=128)
    for kc in range(16):
        i = cnt['stg'] % 2
        cnt['stg'] += 1
        P.dma(dq(), stg[i], wo_v[:, kc, :], writes=['stg%d' % i])
        eng = ('vector', 'gpsimd')[i]
        P.add(eng, lambda e, i=i, kc=kc: e.tensor_copy(out=wob[:, kc, :], in_=stg[i]), reads=['stg%d' % i], writes=['wob'])
    og4 = [a.rearrange("(q r) c -> q r c", q=4) for a in og]
    for tt in range(8):
        r0 = tt * 128
        P.dma(dq(), xr, xres[r0:r0 + 128, :], writes=['xr'])
        P.dma(dq(), mixt[:, 0:512], oa[r0:r0 + 128, :], writes=['mixA'])
        for d in range(4):
            R = d * 1024 + r0
            P.dma(dq(), cand[d], og4[R // 512][:, R % 512:R % 512 + 128, :].rearrange("q p c -> p q c"), writes=['cand%d' % d])
        for d in range(4):
            for m in range(3):
                mv = mixt[:, 512 + m * 512:512 + (m + 1) * 512].rearrange("p (q c) -> p q c", q=4)
                cv = cand[d][:, :, m * 128:(m + 1) * 128]
                if d == 0:
                    P.add('vector', lambda e, cv=cv, mv=mv: e.tensor_scalar(out=mv, in0=cv, scalar1=oneh[:, 0:1], scalar2=None, op0=ALU.mult),
                          reads=['cand0', 'oneh'], writes=['mixB%d' % m])
                else:
                    P.add('vector', lambda e, cv=cv, mv=mv, d=d: e.scalar_tensor_tensor(out=mv, in0=cv, scalar=oneh[:, d:d + 1], in1=mv, op0=ALU.mult, op1=ALU.add),
                          reads=['cand%d' % d, 'oneh', 'mixB%d' % m], writes=['mixB%d' % m])
        for g4 in range(4):
            pi = nps()
            for q in range(4):
                kc = 4 * g4 + q
                P.add('tensor', lambda e, pi=pi, q=q, kc=kc: e.matmul(ps[pi][:, q * 128:(q + 1) * 128], lhsT=mixt[:, kc * 128:(kc + 1) * 128], rhs=ident, start=True, stop=True),
                      reads=['mixA', 'mixB0', 'mixB1', 'mixB2', 'ident'], writes=['ps%d' % pi])
            P.add('scalar', lambda e, pi=pi, g4=g4: e.copy(out=mixb[:, 4 * g4:4 * g4 + 4, :], in_=ps[pi][:, :].rearrange("p (q t) -> p q t", t=128)), reads=['ps%d' % pi], writes=['mixb'])
        for dc in range(4):
            pi = nps()
            for kc in range(16):
                P.add('tensor', lambda e, pi=pi, kc=kc, dc=dc: e.matmul(ps[pi][:, :], lhsT=mixb[:, kc, :], rhs=wob[:, kc, dc * 512:(dc + 1) * 512], start=(kc == 0), stop=(kc == 15)),
                      reads=['mixb', 'wob'], writes=['ps%d' % pi])
            P.add('vector', lambda e, pi=pi, dc=dc: e.scalar_tensor_tensor(out=z[:, dc * 512:(dc + 1) * 512], in0=xr[:, dc * 512:(dc + 1) * 512], scalar=ALPHA, in1=ps[pi][:, :], op0=ALU.mult, op1=ALU.add),
                  reads=['xr', 'ps%d' % pi], writes=['z'])
        _ln_ops(P, z, 'z', xo, 'xo', G, Bt, junk, st6, 'a')
        P.dma(dq(), x1[r0:r0 + 128, :], xo, reads=['xo'])
        for g4 in range(4):
            pi = nps()
            for q in range(4):
                kc = 4 * g4 + q
                P.add('tensor', lambda e, pi=pi, q=q, kc=kc: e.matmul(ps[pi][:, q * 128:(q + 1) * 128], lhsT=xo[:, kc * 128:(kc + 1) * 128], rhs=ident, start=True, stop=True),
                      reads=['xo', 'ident'], writes=['ps%d' % pi])
            P.add('scalar', lambda e, pi=pi, g4=g4: e.copy(out=xTf[:, 4 * g4:4 * g4 + 4, :], in_=ps[pi][:, :].rearrange("p (q t) -> p q t", t=128)), reads=['ps%d' % pi], writes=['xTf'])
        P.add('gpsimd', lambda e: e.tensor_copy(out=xTb, in_=xTf), reads=['xTf'], writes=['xTb'])
        for k in range(4):
            P.dma(dq(), x1T[k].rearrange("(kc p) t -> p kc t", p=128)[:, :, r0:r0 + 128], xTb[:, 4 * k:4 * k + 4, :], reads=['xTb'])
        pi = nps()
        for kc in range(16):
            P.add('tensor', lambda e, pi=pi, kc=kc: e.matmul(ps[pi][:, 0:16], lhsT=xTf[:, kc, :], rhs=rwf[:, kc, :], start=(kc == 0), stop=(kc == 15)),
                  reads=['xTf', 'rwf'], writes=['ps%d' % pi])
        P.add('scalar', lambda e, pi=pi: e.activation(out=aff, in_=ps[pi][:, 0:16], func=AF.Sigmoid), reads=['ps%d' % pi], writes=['aff'])
        P.add('vector', lambda e: e.tensor_tensor(out=sel, in0=aff, in1=rbt, op=ALU.add), reads=['aff', 'rbt'], writes=['sel'])
        P.add('vector', lambda e: e.tensor_reduce(out=m1, in_=sel.rearrange("p (g l) -> p g l", l=4), axis=AX.X, op=ALU.max), reads=['sel'], writes=['m1'])
        for g in range(4):
            P.add('vector', lambda e, g=g: e.tensor_scalar(out=eq[:, 4 * g:4 * g + 4], in0=sel[:, 4 * g:4 * g + 4], scalar1=m1[:, g:g + 1], scalar2=-1.0e9, op0=ALU.is_equal, op1=ALU.mult),
                  reads=['sel', 'm1'], writes=['eq'])
        P.add('vector', lambda e: e.tensor_tensor(out=tmp, in0=sel, in1=eq, op=ALU.add), reads=['sel', 'eq'], writes=['tmp'])
        P.add('vector', lambda e: e.tensor_reduce(out=m2, in_=tmp.rearrange("p (g l) -> p g l", l=4), axis=AX.X, op=ALU.max), reads=['tmp'], writes=['m2'])
        P.add('vector', lambda e: e.tensor_tensor(out=gs, in0=m1, in1=m2, op=ALU.add), reads=['m1', 'm2'], writes=['gs'])
        P.add('vector', lambda e: e.tensor_reduce(out=gm, in_=gs, axis=AX.X, op=ALU.max), reads=['gs'], writes=['gm'])
        P.add('vector', lambda e: e.tensor_scalar(out=oh, in0=gs, scalar1=gm[:, 0:1], scalar2=None, op0=ALU.is_equal), reads=['gs', 'gm'], writes=['oh'])
        for g in range(4):
            P.add('vector', lambda e, g=g: e.tensor_scalar(out=msk[:, 4 * g:4 * g + 4], in0=sel[:, 4 * g:4 * g + 4], scalar1=m2[:, g:g + 1], scalar2=oh[:, g:g + 1], op0=ALU.is_ge, op1=ALU.mult),
                  reads=['sel', 'm2', 'oh'], writes=['msk'])
        P.add('vector', lambda e: e.tensor_tensor(out=gout, in0=aff, in1=msk, op=ALU.mult), reads=['aff', 'msk'], writes=['gout'])
        P.add('vector', lambda e: e.reduce_sum(out=gsum, in_=gout, axis=AX.X), reads=['gout'], writes=['gsum'])
        P.add('vector', lambda e: e.reciprocal(out=gsum, in_=gsum), reads=['gsum'], writes=['gsum'])
        P.add('vector', lambda e: e.tensor_scalar(out=gout, in0=gout, scalar1=gsum[:, 0:1], scalar2=None, op0=ALU.mult), reads=['gout', 'gsum'], writes=['gout'])
        P.dma(dq(), gates[r0:r0 + 128, :], gout, reads=['gout'])


def stage_k4(C, io):
    nc, P, ps = C.nc, C.P, C.ps
    sb = C.sb
    dq = C.dq
    x1Tg, gg, wg, wu, wd, part = io['x1Tg'], io['gg'], io['wg'], io['wu'], io['wd'], io['part']
    wgb = sb("wgb", [128, 16, 1024], BF16)
    wub = sb("wub", [128, 16, 1024], BF16)
    wdb = sb("wdb", [128, 8, D], BF16)
    stg = [sb("stg%d" % i, [128, 2048], F32) for i in range(3)]
    xc = [sb("xc%d" % i, [128, 16, 512], BF16) for i in range(2)]
    hT = sb("hT", [128, 8, 512], BF16)
    prev = [sb("prev%d" % i, [128, D]) for i in range(2)]
    yt = [sb("yt%d" % i, [128, D]) for i in range(2)]
    gall = sb("gall", [128, 32, 16])
    gt = sb("gt", [128, 32, 4])
    oneh = sb("oneh", [128, 4])
    sg = [sb("sg%d" % i, [128, 512], F32) for i in range(2)]
    cnt = {'ps': 0, 'stg': 0, 'sg': 0, 'cv': 0, 'yb': 0}
    cv_eng = ('scalar', 'vector', 'scalar', 'vector', 'gpsimd')

    def nps():
        i = cnt['ps'] % 8
        cnt['ps'] += 1
        return i

    def conv(dst_ap, dkey, src_ap, is3d):
        i = cnt['stg'] % 3
        cnt['stg'] += 1
        sview = stg[i].rearrange("p (a b) -> p a b", b=128) if is3d else stg[i]
        P.dma(dq(), sview, src_ap, writes=['stg%d' % i])
        eng = cv_eng[cnt['cv'] % 5]
        cnt['cv'] += 1
        if eng == 'scalar':
            P.add('scalar', lambda e: e.copy(out=dst_ap, in_=sview), reads=['stg%d' % i], writes=[dkey])
        else:
            P.add(eng, lambda e: e.tensor_copy(out=dst_ap, in_=sview), reads=['stg%d' % i], writes=[dkey])

    P.dma('sync', oneh, io['oneh'][:, :], writes=['oneh'])
    P.dma('sync', gall, gg.rearrange("(tt p) e -> p tt e", p=128), writes=['gall'])
    gv = gall.rearrange("p t (g l) -> p t g l", g=4)
    for g in range(4):
        if g == 0:
            P.add('vector', lambda e: e.tensor_scalar(out=gt, in0=gv[:, :, 0, :], scalar1=oneh[:, 0:1], scalar2=None, op0=ALU.mult), reads=['gall', 'oneh'], writes=['gt'])
        else:
            P.add('vector', lambda e, g=g: e.scalar_tensor_tensor(out=gt, in0=gv[:, :, g, :], scalar=oneh[:, g:g + 1], in1=gt, op0=ALU.mult, op1=ALU.add),
                  reads=['gall', 'oneh', 'gt'], writes=['gt'])
    x_v = [a.rearrange("(q kc p) t -> q p kc t", q=4, p=128) for a in x1Tg]
    def jobs_gu(ex):
        wg_v = wg[ex].rearrange("(kc p) f -> p kc f", p=128)
        wu_v = wu[ex].rearrange("(kc p) f -> p kc f", p=128)
        for f in range(8):
            conv(wgb[:, :, f * 128:(f + 1) * 128], 'wgb', wg_v[:, :, f * 128:(f + 1) * 128], True)
            conv(wub[:, :, f * 128:(f + 1) * 128], 'wub', wu_v[:, :, f * 128:(f + 1) * 128], True)

    def jobs_d(ex):
        wd_v = wd[ex].rearrange("(fc p) d -> p fc d", p=128)
        for f in range(8):
            conv(wdb[:, f, :], 'wdb', wd_v[:, f, :], False)

    jobs_gu(0)
    jobs_d(0)
    for ex in range(4):
        for c8 in range(8):
            ch, half = c8 // 2, c8 % 2
            xi = c8 % 2
            for k in range(4):
                P.dma(dq(), xc[xi][:, 4 * k:4 * k + 4, :], x_v[k][ch, :, :, half * 512:(half + 1) * 512], writes=['xc%d' % xi])
            for f in range(8):
                pg = nps()
                for kc in range(16):
                    P.add('tensor', lambda e, pg=pg, kc=kc, f=f, xi=xi: e.matmul(ps[pg][:, :], lhsT=wgb[:, kc, f * 128:(f + 1) * 128], rhs=xc[xi][:, kc, :], start=(kc == 0), stop=(kc == 15)),
                          reads=['wgb', 'xc%d' % xi], writes=['ps%d' % pg])
                pu = nps()
                for kc in range(16):
                    P.add('tensor', lambda e, pu=pu, kc=kc, f=f, xi=xi: e.matmul(ps[pu][:, :], lhsT=wub[:, kc, f * 128:(f + 1) * 128], rhs=xc[xi][:, kc, :], start=(kc == 0), stop=(kc == 15)),
                          reads=['wub', 'xc%d' % xi], writes=['ps%d' % pu])
                si = cnt['sg'] % 2
                cnt['sg'] += 1
                P.add('scalar', lambda e, pg=pg, si=si: e.activation(out=sg[si], in_=ps[pg][:, :], func=AF.Silu), reads=['ps%d' % pg], writes=['sg%d' % si])
                P.add('vector', lambda e, pu=pu, si=si, f=f: e.tensor_tensor(out=hT[:, f, :], in0=sg[si], in1=ps[pu][:, :], op=ALU.mult),
                      reads=['sg%d' % si, 'ps%d' % pu], writes=['hT'])
            if c8 == 7 and ex < 3:
                jobs_gu(ex + 1)
            for tt in range(4):
                T = ch * 8 + half * 4 + tt
                row = ch * 512 + tt * 128
                yb = cnt['yb'] % 2
                cnt['yb'] += 1
                pkey = 'part_%d_%d' % (c8, tt)
                if ex > 0:
                    P.dma(dq(), prev[yb], part[half][row:row + 128, :], reads=[pkey], writes=['prev%d' % yb])
                for dc in range(4):
                    pi = nps()
                    for f in range(8):
                        P.add('tensor', lambda e, pi=pi, f=f, tt=tt, dc=dc: e.matmul(ps[pi][:, :], lhsT=hT[:, f, tt * 128:(tt + 1) * 128], rhs=wdb[:, f, dc * 512:(dc + 1) * 512], start=(f == 0), stop=(f == 7)),
                              reads=['hT', 'wdb'], writes=['ps%d' % pi])
                    gsc = gt[:, T, ex:ex + 1]
                    if ex == 0:
                        P.add('vector', lambda e, pi=pi, dc=dc, gsc=gsc, yb=yb: e.tensor_scalar(out=yt[yb][:, dc * 512:(dc + 1) * 512], in0=ps[pi][:, :], scalar1=gsc, scalar2=None, op0=ALU.mult),
                              reads=['ps%d' % pi, 'gt'], writes=['yt%d' % yb])
                    else:
                        P.add('vector', lambda e, pi=pi, dc=dc, gsc=gsc, yb=yb: e.scalar_tensor_tensor(out=yt[yb][:, dc * 512:(dc + 1) * 512], in0=ps[pi][:, :], scalar=gsc, in1=prev[yb][:, dc * 512:(dc + 1) * 512], op0=ALU.mult, op1=ALU.add),
                              reads=['ps%d' % pi, 'gt', 'prev%d' % yb], writes=['yt%d' % yb])
                P.dma(dq(), part[half][row:row + 128, :], yt[yb], reads=['yt%d' % yb], writes=[pkey])
            if c8 == 7 and ex < 3:
                jobs_d(ex + 1)


def stage_k5(C, io, last):
    nc, P, ps = C.nc, C.P, C.ps
    sb = C.sb
    dq = C.dq
    G = sb("G", [128, D]); Bt = sb("Bt", [128, D])
    xr = sb("xr", [128, D]); z = sb("z", [128, D]); junk = sb("junk", [128, D]); xo = sb("xo", [128, D])
    pt = sb("pt", [128, D])
    st6 = sb("st6", [128, 8])
    ident = sb("identf_s", [128, 128])
    xTb = sb("xTb", [128, 16, 128], BF16)
    cnt = {'ps': 0}

    def nps():
        i = cnt['ps'] % 8
        cnt['ps'] += 1
        return i
    P.dma('sync', G, io['lng'].partition_broadcast(128), writes=['G'])
    P.dma('sync', Bt, io['lnb'].partition_broadcast(128), writes=['Bt'])
    P.dma('sync', ident, io['identf'][:, :], writes=['ident'])
    for tt in range(8):
        r0 = tt * 128
        P.dma(dq(), xr, io['x1'][r0:r0 + 128, :], writes=['xr'])
        P.dma(dq(), pt, io['rs'][tt // 4][(tt % 4) * 128:(tt % 4) * 128 + 128, :], writes=['pt'])
        P.add('vector', lambda e: e.scalar_tensor_tensor(out=z, in0=xr, scalar=ALPHA, in1=pt, op0=ALU.mult, op1=ALU.add), reads=['xr', 'pt'], writes=['z'])
        _ln_ops(P, z, 'z', xo, 'xo', G, Bt, junk, st6, 'a')
        P.dma(dq(), io['x2'][r0:r0 + 128, :], xo, reads=['xo'])
        if not last:
            for g4 in range(4):
                pi = nps()
                for q in range(4):
                    kc = 4 * g4 + q
                    P.add('tensor', lambda e, pi=pi, q=q, kc=kc: e.matmul(ps[pi][:, q * 128:(q + 1) * 128], lhsT=xo[:, kc * 128:(kc + 1) * 128], rhs=ident, start=True, stop=True),
                          reads=['xo', 'ident'], writes=['ps%d' % pi])
                P.add('scalar', lambda e, pi=pi, g4=g4: e.copy(out=xTb[:, 4 * g4:4 * g4 + 4, :], in_=ps[pi][:, :].rearrange("p (q t) -> p q t", t=128)), reads=['ps%d' % pi], writes=['xTb'])
            for k in range(4):
                P.dma(dq(), io['x2T'][k].rearrange("(kc p) t -> p kc t", p=128)[:, :, r0:r0 + 128], xTb[:, 4 * k:4 * k + 4, :], reads=['xTb'])


def build_fused(nlayers=2, stop=99):
    nc = bass.Bass("TRN2", target_bir_lowering=False)
    shapes = {
        'xT': ([D, S], F32), 'xTo': ([D, 1024], F32), 'xown': ([1024, D], F32),
        'pos': ([1, S], I32), 'poso': ([1, 1024], I32), 'cst': ([128, 8], F32),
        'cmask': ([128, 4, 512], BF16), 'dmask': ([128, 20, 512], BF16), 'negc': ([128, 512], F32),
        'ident': ([128, 128], BF16), 'identf': ([128, 128], F32), 'oneh': ([128, 4], F32),
        'rw': ([D, 16], F32), 'rb': ([1, 16], F32),
    }
    for l in range(nlayers):
        shapes.update({
            'wq%d' % l: ([D, NQT * 128], F32), 'wv%d' % l: ([D, 512], F32), 'wa%d' % l: ([D, NAO * 128], F32), 'wiw%d' % l: ([D, 16], F32),
            'dl%d' % l: ([1, 256], F32), 'dg%d' % l: ([1, 128], F32), 'lc%d' % l: ([128, 2], F32),
            'wo%d' % l: ([D, D], F32), 'lng%d' % l: ([1, D], F32), 'lnb%d' % l: ([1, D], F32),
            'wg%d' % l: ([4, D, 1024], F32), 'wu%d' % l: ([4, D, 1024], F32), 'wd%d' % l: ([4, 1024, D], F32),
            'lng2_%d' % l: ([1, D], F32), 'lnb2_%d' % l: ([1, D], F32)})

    class Lazy(dict):
        def __missing__(self, k):
            shp, dt = shapes[k]
            v = nc.dram_tensor(k, shp, dt, kind="ExternalInput").ap()
            self[k] = v
            return v
    ein = Lazy()

    def scr(name, shape, dt=F32):
        return nc.dram_tensor(name, shape, dt).ap()
    out = nc.dram_tensor("out", [1024, D], F32, kind="ExternalOutput").ap()
    s_qt = scr("s_qt", [9, 128, S], BF16); s_vt = scr("s_vt", [S, 512], BF16)
    s_aq = scr("s_aq", [4, 128, 1024], BF16); s_iq = scr("s_iq", [16, 128, 1024], BF16); s_iw = scr("s_iw", [1024, 16])
    s_obd = [scr("s_obd%d" % k, [512, 384]) for k in range(8)]; s_oa = scr("s_oa", [1024, 512]); s_og = [scr("s_og%d" % k, [2048, 384]) for k in range(8)]
    s_x1 = scr("s_x1", [1024, D]); s_x1T = [scr("s_x1T%d" % k, [512, 1024], BF16) for k in range(4)]; s_x1Tg = [scr("s_x1Tg%d" % k, [2048, 1024], BF16) for k in range(4)]
    s_gates = scr("s_gates", [1024, 16]); s_gg = scr("s_gg", [4 * 1024, 16])
    s_part = [scr("s_part%d" % k, [2048, D]) for k in range(2)]; s_rs = [scr("s_rs%d" % k, [512, D]) for k in range(2)]
    s_x2 = scr("s_x2", [1024, D]); s_x2T = [scr("s_x2T%d" % k, [512, 1024], BF16) for k in range(4)]; s_xTg = [scr("s_xTg%d" % k, [2048, 1024], BF16) for k in range(4)]

    with ExitStack() as st:
        C = Ctx(nc, st)
        stage = 0

        def go():
            nonlocal stage
            stage += 1
            return stage <= stop
        for l in range(nlayers):
            last = (l == nlayers - 1)
            if go():
                io1 = {'wq': ein['wq%d' % l], 'wv': ein['wv%d' % l], 'wa': ein['wa%d' % l], 'wiw': ein['wiw%d' % l],
                       'pos': ein['pos'], 'poso': ein['poso'], 'cst': ein['cst'],
                       'qt': s_qt, 'vt': s_vt, 'aq': s_aq, 'iq': s_iq, 'iw': s_iw, 'xTg': s_xTg, 'x2T': s_x2T}
                if l == 0:
                    io1['xT'] = ein['xT']; io1['xTo'] = ein['xTo']
                stage_k1(C, io1, layer1=(l > 0))
                C.barrier()
            if go():
                io2 = {'qt': s_qt, 'vt': s_vt, 'aq': s_aq, 'iq': s_iq, 'iw': s_iw, 'o_bd': s_obd, 'oa': s_oa,
                       'cmask': ein['cmask'], 'dmask': ein['dmask'], 'negc': ein['negc'], 'ident': ein['ident'],
                       'dl': ein['dl%d' % l], 'dg': ein['dg%d' % l], 'lc': ein['lc%d' % l]}
                stage_k2(C, io2)
            if go():
                C.collectives([("AllGather", ALU.bypass, s_obd[k], s_og[k]) for k in range(8)])
            if go():
                io3 = {'og': s_og, 'oa': s_oa, 'xres': ein['xown'] if l == 0 else s_x2, 'x1': s_x1, 'x1T': s_x1T, 'gates': s_gates,
                       'wo': ein['wo%d' % l], 'lng': ein['lng%d' % l], 'lnb': ein['lnb%d' % l], 'rw': ein['rw'], 'rb': ein['rb'],
                       'identf': ein['identf'], 'oneh': ein['oneh']}
                stage_k3(C, io3)
            if go():
                C.collectives([("AllGather", ALU.bypass, s_x1T[k], s_x1Tg[k]) for k in range(4)] + [("AllGather", ALU.bypass, s_gates, s_gg)])
            if go():
                io4 = {'x1Tg': s_x1Tg, 'gg': s_gg, 'wg': ein['wg%d' % l], 'wu': ein['wu%d' % l], 'wd': ein['wd%d' % l], 'part': s_part, 'oneh': ein['oneh']}
                stage_k4(C, io4)
            if go():
                C.collectives([("ReduceScatter", ALU.add, s_part[k], s_rs[k]) for k in range(2)])
            if go():
                io5 = {'x1': s_x1, 'rs': s_rs, 'x2': out if last else s_x2, 'x2T': s_x2T, 'lng': ein['lng2_%d' % l], 'lnb': ein['lnb2_%d' % l], 'identf': ein['identf']}
                stage_k5(C, io5, last)
                if not last:
                    C.collectives([("AllGather", ALU.bypass, s_x2T[k], s_xTg[k]) for k in range(4)])
        C.P.emit(st)
    nc._ext_names = list(ein.keys())
    return nc


_NC = {}


def kernel(x, positions, w_in, w_out, diff_lambda, diff_norm_g, ln_mix_g, ln_mix_b,
           router_w, router_bias, w_gate, w_up, w_down, ln_ffn_g, ln_ffn_b):
    import math
    x = np.asarray(x, np.float32)
    positions = np.asarray(positions)
    Bn, Sn, Dn = x.shape
    nl = int(np.asarray(w_in).shape[0])
    if 'nc' not in _NC:
        _NC['nc'] = build_fused(nl)
    nc = _NC['nc']
    f32 = lambda a: np.ascontiguousarray(np.asarray(a, np.float32))
    identf = np.eye(128, dtype=np.float32)
    in_maps = []
    for c in range(8):
        b, j = c // 4, c % 4
        own = own_tokens(j)
        xT = np.ascontiguousarray(x[b].T)
        k2c = k2_consts(j, np.zeros(256, np.float32), np.zeros(128, np.float32), 0)
        oneh = np.zeros((128, 4), np.float32)
        oneh[:, j] = 1.0
        m = {'xT': xT, 'xTo': np.ascontiguousarray(xT[:, own]), 'xown': np.ascontiguousarray(x[b][own]),
             'pos': np.ascontiguousarray(positions[b][None, :].astype(np.int32)),
             'poso': np.ascontiguousarray(positions[b][None, own].astype(np.int32)),
             'cst': k1_consts(), 'cmask': k2c['cmask'], 'dmask': k2c['dmask'], 'negc': k2c['negc'], 'ident': k2c['ident'],
             'identf': identf, 'oneh': oneh, 'rw': f32(router_w), 'rb': f32(np.asarray(router_bias)[None, :])}
        for l in range(nl):
            wl = np.asarray(w_in[l], np.float32)
            qcols, vcols, acols, iwcols = k1_cols(j)
            m['wq%d' % l] = np.ascontiguousarray(wl[:, qcols]); m['wv%d' % l] = np.ascontiguousarray(wl[:, vcols])
            m['wa%d' % l] = np.ascontiguousarray(wl[:, acols]); m['wiw%d' % l] = np.ascontiguousarray(wl[:, iwcols])
            lam_init = 0.8 - 0.6 * math.exp(-0.3 * l)
            lc = np.zeros((128, 2), np.float32); lc[:, 0] = lam_init; lc[:, 1] = 1.0 - lam_init
            m['dl%d' % l] = f32(np.asarray(diff_lambda[l]).reshape(1, 256)); m['dg%d' % l] = f32(np.asarray(diff_norm_g[l]).reshape(1, 128)); m['lc%d' % l] = lc
            m['wo%d' % l] = f32(w_out[l]); m['lng%d' % l] = f32(np.asarray(ln_mix_g[l])[None, :]); m['lnb%d' % l] = f32(np.asarray(ln_mix_b[l])[None, :])
            m['wg%d' % l] = f32(w_gate[l][4 * j:4 * j + 4]); m['wu%d' % l] = f32(w_up[l][4 * j:4 * j + 4]); m['wd%d' % l] = f32(w_down[l][4 * j:4 * j + 4])
            m['lng2_%d' % l] = f32(np.asarray(ln_ffn_g[l])[None, :]); m['lnb2_%d' % l] = f32(np.asarray(ln_ffn_b[l])[None, :])
        in_maps.append(m)
    res = run_bass_kernel_spmd(nc, in_maps, core_ids=list(range(8)))
    out = np.zeros((Bn, Sn, Dn), np.float32)
    for c in range(8):
        b, j = c // 4, c % 4
        out[b, own_tokens(j)] = np.asarray(res.results[c]['out'])
    return out
```

```python
import numpy as np
import concourse.bass as bass
import concourse.mybir as mybir
from concourse.bass_utils import run_bass_kernel_spmd

F32 = mybir.dt.float32
BF16 = mybir.dt.bfloat16
I32 = mybir.dt.int32
AF = mybir.ActivationFunctionType
ALU = mybir.AluOpType
AX = mybir.AxisListType

COMPUTE = ('tensor', 'scalar', 'vector', 'gpsimd')
DMAQ = ('sync', 'scalar', 'gpsimd')


class _Op:
    __slots__ = ('eng', 'fn', 'reads', 'writes', 'dma', 'need', 'signal', 'val', 'slot', 'cc', 'bar')


class Prog:
    def __init__(self, nc, nslots=4):
        self.nc = nc
        self.ops = []
        self.nslots = nslots

    def add(self, eng, fn, reads=(), writes=(), dma=False, cc=False):
        o = _Op()
        o.cc = cc
        o.bar = False
        o.eng, o.fn, o.reads, o.writes, o.dma = eng, fn, tuple(reads), tuple(writes), dma
        o.need = set()
        o.signal = dma
        o.val = None
        o.slot = None
        self.ops.append(o)
        return o

    def dma(self, eng, out, in_, reads=(), writes=()):
        return self.add(eng, lambda e: e.dma_start(out=out, in_=in_), reads, writes, dma=True)

    def barrier(self, fn):
        o = self.add('vector', fn)
        o.bar = True
        o.signal = True
        return o

    def _assign_slots(self):
        dcount = {q: 0 for q in DMAQ}
        for o in self.ops:
            if o.dma:
                if o.cc:
                    o.slot = ('cc', 0)
                else:
                    j = dcount[o.eng]
                    dcount[o.eng] += 1
                    o.slot = (o.eng, j % self.nslots)

    def _barriers(self):
        ops = self.ops
        engs = ('sync', 'scalar', 'vector', 'gpsimd', 'tensor')
        bars = [i for i, o in enumerate(ops) if o.bar]
        for bi in bars:
            b = ops[bi]
            lastc = {}
            lasts = {}
            for i in range(bi):
                o = ops[i]
                if o.dma:
                    lasts[o.slot] = i
                else:
                    lastc[o.eng] = i
            for i in list(lastc.values()) + list(lasts.values()):
                b.need.add(i)
            seen = set()
            for i in range(bi + 1, len(ops)):
                o = ops[i]
                if o.eng not in seen:
                    seen.add(o.eng)
                    o.need.add(bi)
                    if len(seen) == len(engs):
                        break

    def _deps(self):
        last_w = {}
        rd = {}
        ops = self.ops
        for i, c in enumerate(ops):
            raw, other = set(), set()
            for k in c.reads:
                if k in last_w:
                    raw.add(last_w[k])
            for k in c.writes:
                if k in last_w:
                    other.add(last_w[k])
                for r in rd.get(k, ()):
                    other.add(r)
            for p in raw | other:
                if p == i:
                    continue
                po = ops[p]
                if po.dma or c.dma:
                    c.need.add(p)
                elif po.eng == c.eng:
                    if c.eng != 'tensor':
                        c.need.add(p)
                else:
                    c.need.add(p)
            for k in c.reads:
                rd.setdefault(k, []).append(i)
            for k in c.writes:
                last_w[k] = i
                rd[k] = []
        self._assign_slots()
        self._barriers()
        for c in ops:
            for p in c.need:
                ops[p].signal = True

    def emit(self, stack):
        nc = self.nc
        self._deps()
        ops = self.ops
        sem = {}
        for e in COMPUTE:
            sem[e] = stack.enter_context(nc.semaphore('s_' + e))
        for q in DMAQ:
            for s in range(self.nslots):
                sem[(q, s)] = stack.enter_context(nc.semaphore('d_%s%d' % (q, s)))
        if any(o.cc for o in ops):
            sem[('cc', 0)] = stack.enter_context(nc.semaphore('s_cc'))
        cnt = {e: 0 for e in COMPUTE}
        dcount = {q: 0 for q in DMAQ}
        slotcnt = {}
        prev_in_slot = {}
        for i, o in enumerate(ops):
            if o.dma:
                s = o.slot
                inc = 1 if o.cc else 16
                if s in prev_in_slot:
                    o.need.add(prev_in_slot[s])
                prev_in_slot[s] = i
                slotcnt[s] = slotcnt.get(s, 0) + inc
                o.val = slotcnt[s]
            elif o.signal:
                cnt[o.eng] += 1
                o.val = cnt[o.eng]
        final_slots = dict(slotcnt)
        block = stack.enter_context(nc.Block())

        def run_engine(ename, e):
            waited = {}
            for o in ops:
                if o.eng != ename:
                    continue
                req = {}
                for p in o.need:
                    po = ops[p]
                    sk = po.slot if po.dma else po.eng
                    if po.val > req.get(sk, 0):
                        req[sk] = po.val
                for sk, v in req.items():
                    if waited.get(sk, 0) >= v:
                        continue
                    e.wait_ge(sem[sk], v)
                    waited[sk] = v
                ins = o.fn(e)
                if o.dma:
                    ins.then_inc(sem[o.slot], 1 if o.cc else 16)
                elif o.signal:
                    ins.then_inc(sem[o.eng], 1)
            if ename == 'sync':
                for sk, v in final_slots.items():
                    if waited.get(sk, 0) < v:
                        e.wait_ge(sem[sk], v)

        @block.sync
        def _(e):
            run_engine('sync', e)

        @block.scalar
        def _(e):
            run_engine('scalar', e)

        @block.vector
        def _(e):
            run_engine('vector', e)

        @block.gpsimd
        def _(e):
            run_engine('gpsimd', e)

        @block.tensor
        def _(e):
            run_engine('tensor', e)


import numpy as np

THETA = 10000.0
OFF = {}
_sizes = (512, 128, 128, 1024, 64, 16, 512, 512, 512, 512, 512, 512, 512, 512, 512)
_names = ('a_q', 'a_k', 'a_v', 'i_q', 'i_k', 'i_w', 'b_q', 'b_k', 'b_v', 'c_q', 'c_k', 'c_v', 'd_q', 'd_k', 'd_v')
_o = 0
for _n, _s in zip(_names, _sizes):
    OFF[_n] = _o
    _o += _s

P64 = np.concatenate([np.arange(0, 32), np.arange(64, 96), np.arange(32, 64), np.arange(96, 128)])
IK2 = np.concatenate([np.arange(0, 32), np.arange(0, 32), np.arange(32, 64), np.arange(32, 64)])


def k1_consts():
    p = np.arange(128)
    c = np.zeros((128, 8), np.float32)
    c[:, 0] = THETA ** (-2.0 * (p % 64) / 128.0)
    c[:, 1] = np.where(p < 64, -1.0, 1.0)
    c[:, 2] = THETA ** (-2.0 * (p % 32) / 64.0)
    c[:, 3] = c[:, 1]
    c[:, 4] = ((p % 64) < 32).astype(np.float32)
    c[:, 5] = 1.0 - c[:, 4]
    return c


def k1_cols(j):
    qcols = np.concatenate([
        OFF['b_q'] + 128 * j + P64, OFF['b_k'] + 128 * j + P64,
        OFF['c_q'] + 128 * j + np.arange(128), OFF['c_k'] + 128 * j + np.arange(128),
        OFF['d_q'] + 128 * j + np.arange(128), OFF['d_k'] + 128 * j + np.arange(128),
        OFF['a_k'] + np.arange(128), OFF['i_k'] + IK2])
    vcols = np.concatenate([OFF['b_v'] + 128 * j + np.arange(128), OFF['c_v'] + 128 * j + np.arange(128),
                            OFF['d_v'] + 128 * j + np.arange(128), OFF['a_v'] + np.arange(128)])
    acols = np.concatenate([OFF['a_q'] + np.arange(512)] + [OFF['i_q'] + 128 * pr + P64 for pr in range(8)])
    iwcols = OFF['i_w'] + np.arange(16)
    return qcols, vcols, acols, iwcols


def own_tokens(j):
    return np.concatenate([np.arange(128 * (4 * c + j), 128 * (4 * c + j) + 128) for c in range(8)])


def k1_inputs(xb, posb, w_in_l, j):
    qcols, vcols, acols, iwcols = k1_cols(j)
    xT = np.ascontiguousarray(xb.T)
    own = own_tokens(j)
    return {
        'xT': xT, 'xTo': np.ascontiguousarray(xT[:, own]),
        'wq': np.ascontiguousarray(w_in_l[:, qcols]), 'wv': np.ascontiguousarray(w_in_l[:, vcols]),
        'wa': np.ascontiguousarray(w_in_l[:, acols]), 'wiw': np.ascontiguousarray(w_in_l[:, iwcols]),
        'pos': np.ascontiguousarray(posb[None, :].astype(np.int32)),
        'poso': np.ascontiguousarray(posb[None, own].astype(np.int32)),
        'cst': k1_consts(),
    }


def k2_consts(j, dl_l, dg_l, layer):
    import ml_dtypes, math
    bf = ml_dtypes.bfloat16
    s = np.arange(128)[:, None]
    t = np.arange(512)[None, :]
    cm = np.stack([(s + 128 * d <= t) for d in range(4)], 1).astype(np.float32)
    dms = []
    for di in range(20):
        d = (128 * di - 384) + t - s
        m = ((d >= 0) & (d <= 128)).astype(np.float32) + ((d >= 0) & (d <= 512) & (d % 4 == 0)) + ((d >= 0) & (d <= 2048) & (d % 16 == 0))
        dms.append(m.astype(np.float32))
    dm = np.stack(dms, 1)
    tq = 128 * j + np.arange(128)[:, None]
    sk = np.arange(512)[None, :]
    negc = np.where(sk > tq, -1.0e30, 0.0).astype(np.float32)
    lam_init = 0.8 - 0.6 * math.exp(-0.3 * layer)
    lc = np.zeros((128, 2), np.float32)
    lc[:, 0] = lam_init
    lc[:, 1] = 1.0 - lam_init
    return {'cmask': cm.astype(bf), 'dmask': dm.astype(bf), 'negc': negc, 'ident': np.eye(128, dtype=np.float32).astype(bf),
            'dl': np.ascontiguousarray(dl_l.reshape(1, 256)), 'dg': np.ascontiguousarray(dg_l.reshape(1, 128)), 'lc': lc}


from contextlib import ExitStack


def _ln_ops(P, z, zkey, outt, outkey, G, Bt, junk, st6, tag):
    P.add('scalar', lambda e: e.activation(out=junk[:], in_=z[:], func=AF.Copy, accum_out=st6[:, 0:1]), reads=[zkey], writes=['junk', 'st0'])
    P.add('scalar', lambda e: e.activation(out=junk[:], in_=z[:], func=AF.Square, accum_out=st6[:, 1:2]), reads=[zkey], writes=['junk', 'st1'])
    P.add('vector', lambda e: e.tensor_scalar(out=st6[:, 2:3], in0=st6[:, 0:1], scalar1=1.0 / D, scalar2=None, op0=ALU.mult), reads=['st0'], writes=['st2'])
    P.add('vector', lambda e: e.tensor_tensor(out=st6[:, 3:4], in0=st6[:, 2:3], in1=st6[:, 2:3], op=ALU.mult), reads=['st2'], writes=['st3'])
    P.add('vector', lambda e: e.scalar_tensor_tensor(out=st6[:, 4:5], in0=st6[:, 1:2], scalar=1.0 / D, in1=st6[:, 3:4], op0=ALU.mult, op1=ALU.subtract), reads=['st1', 'st3'], writes=['st4'])
    P.add('vector', lambda e: e.tensor_scalar(out=st6[:, 4:5], in0=st6[:, 4:5], scalar1=1e-5, scalar2=None, op0=ALU.add), reads=['st4'], writes=['st4'])
    P.add('scalar', lambda e: e.activation(out=st6[:, 5:6], in_=st6[:, 4:5], func=AF.Sqrt), reads=['st4'], writes=['st5'])
    P.add('vector', lambda e: e.reciprocal(out=st6[:, 5:6], in_=st6[:, 5:6]), reads=['st5'], writes=['st5'])
    P.add('vector', lambda e: e.scalar_tensor_tensor(out=st6[:, 6:7], in0=st6[:, 2:3], scalar=-1.0, in1=st6[:, 5:6], op0=ALU.mult, op1=ALU.mult), reads=['st2', 'st5'], writes=['st6'])
    P.add('scalar', lambda e: e.activation(out=outt[:], in_=z[:], func=AF.Identity, scale=st6[:, 5:6], bias=st6[:, 6:7]), reads=[zkey, 'st5', 'st6'], writes=[outkey])
    P.add('gpsimd', lambda e: e.tensor_tensor(out=outt[:], in0=outt[:], in1=G[:], op=ALU.mult), reads=[outkey, 'G'], writes=[outkey])
    P.add('vector', lambda e: e.tensor_tensor(out=outt[:], in0=outt[:], in1=Bt[:], op=ALU.add), reads=[outkey, 'Bt'], writes=[outkey])


PI = float(np.pi)
S = 4096
D = 2048
NEG = -1.0e30
ALPHA = float(4 ** 0.25)
ARENA = 51200
NQT = 8
NAO = 12
QT_VARIANT = [1, 1, 0, 0, 0, 0, 0, 1]
QT_MASKED = [True, False, False, False, False, False, False, False]
AO_VARIANT = [0, 0, 0, 0] + [1] * 8
AO_MASKED = [False] * 4 + [True] * 8
RG = [[0, 1, 2, 3], [4, 5, 6, 7]]


class Ctx:
    def __init__(self, nc, st):
        self.nc = nc
        self.arena = st.enter_context(nc.sbuf_tensor("arena", [128, ARENA], F32))
        self.ps = [st.enter_context(nc.psum_tensor("ps%d" % i, [128, 512], F32)) for i in range(8)]
        self.P = Prog(nc)
        self.off = 0
        self.bar = self.arena[:, ARENA - 8:ARENA]
        self.cnt = {'dq': 0}

    def reset(self):
        self.off = 0

    def sb(self, name, shape, dt=F32):
        n = 1
        for s_ in shape[1:]:
            n *= s_
        esz = 2 if dt == BF16 else 4
        nf = (n * esz + 3) // 4
        nf = (nf + 7) // 8 * 8
        assert self.off + nf <= ARENA - 8, ("arena overflow", name, self.off, nf)
        v = self.arena[:, self.off:self.off + nf]
        self.off += nf
        if dt != F32:
            v = v.bitcast(dt)
        v = v[:, 0:n]
        if len(shape) == 3:
            v = v.rearrange("p (a b) -> p a b", b=shape[2])
        elif len(shape) == 4:
            v = v.rearrange("p (a b c) -> p a b c", b=shape[2], c=shape[3])
        return v

    def dq(self):
        self.cnt['dq'] += 1
        return ('sync', 'gpsimd')[self.cnt['dq'] % 2]

    def barrier(self):
        bar = self.bar
        self.P.barrier(lambda e: e.memset(bar, 0.0))
        self.reset()

    def collectives(self, items):
        self.barrier()
        for (kind, op, src, dst) in items:
            self.P.add('gpsimd', lambda e, kind=kind, op=op, src=src, dst=dst: e.collective_compute(kind, op, replica_groups=RG, ins=[src.opt()], outs=[dst.opt()]),
                       dma=True, cc=True)
        self.barrier()


def stage_k1(C, io, layer1):
    nc, P, ps = C.nc, C.P, C.ps
    sb = C.sb
    dq = C.dq
    wq, wv, wa, wiw, pos, poso, cst = io['wq'], io['wv'], io['wa'], io['wiw'], io['pos'], io['poso'], io['cst']
    qt, vt, aq, iq, iw = io['qt'], io['vt'], io['aq'], io['iq'], io['iw']
    wq_b = sb("wq_b", [128, 16, NQT * 128], BF16)
    wv_b = sb("wv_b", [128, 16, 512], BF16)
    wa_b = sb("wa_b", [128, 16, NAO * 128], BF16)
    wiw_b = sb("wiw_b", [128, 16, 16], BF16)
    wst = [sb("wst%d" % i, [128, 16, 128], F32) for i in range(2)]
    xs = [sb("xs%d" % i, [128, 8, 512], F32) for i in range(2)]
    xb = sb("xb", [128, 16, 512], BF16)
    xob = sb("xob", [128, 16, 128], BF16)
    cs = sb("cs", [128, 8], F32)
    posi = sb("posi", [128, 512], I32)
    posf = sb("posf", [128, 512], F32)
    ang = sb("ang", [128, 512], F32)
    ki = sb("ki", [128, 512], I32)
    kf = sb("kf", [128, 512], F32)
    tabC = [sb("tabC%d" % v, [128, 512], F32) for v in range(2)]
    tabS = [sb("tabS%d" % v, [128, 512], F32) for v in range(2)]
    t1 = [sb("t1_%d" % i, [128, 512], F32) for i in range(2)]
    t2 = [sb("t2_%d" % i, [128, 512], F32) for i in range(2)]
    rot = [sb("rot%d" % i, [128, 512], F32) for i in range(2)]
    ob = [sb("ob%d" % i, [128, 512], BF16) for i in range(4)]
    iwt = sb("iwt", [128, 16], F32)
    cnt = {'ps': 0, 'ob': 0, 'tt': 0, 'cv': 0}

    P.dma('sync', cs, cst[:, :], writes=['cs'])

    def load_w(src, ncols, dst, dname):
        src_v = src.rearrange("(kc p) c -> p kc c", p=128)
        for c0 in range(0, ncols, 128):
            cw = min(128, ncols - c0)
            i = cnt['cv'] % 2
            cnt['cv'] += 1
            w_ = wst[i]
            P.dma(dq(), w_[:, :, 0:cw], src_v[:, :, c0:c0 + cw], writes=['wst%d' % i])
            eng = ('scalar', 'vector')[i]
            P.add(eng, lambda e, w_=w_, c0=c0, cw=cw, dst=dst, i=i: (e.copy if i == 0 else e.tensor_copy)(out=dst[:, :, c0:c0 + cw], in_=w_[:, :, 0:cw]),
                  reads=['wst%d' % i], writes=[dname])
    load_w(wq, NQT * 128, wq_b, 'wq_b')
    load_w(wv, 512, wv_b, 'wv_b')
    load_w(wa, NAO * 128, wa_b, 'wa_b')
    load_w(wiw, 16, wiw_b, 'wiw_b')

    def tables(pos_ap, n):
        P.dma('sync', posi[:, 0:n], pos_ap.partition_broadcast(128), writes=['posi'])
        P.add('vector', lambda e: e.tensor_copy(out=posf[:, 0:n], in_=posi[:, 0:n]), reads=['posi'], writes=['posf'])
        for v in range(2):
            inv = cs[:, 2 * v:2 * v + 1]
            for which, off, tab, tkey in (('S', 0.0, tabS[v], 'tabS%d' % v), ('C', PI / 2, tabC[v], 'tabC%d' % v)):
                P.add('vector', lambda e, inv=inv, off=off: e.tensor_scalar(out=ang[:, 0:n], in0=posf[:, 0:n], scalar1=inv, scalar2=off, op0=ALU.mult, op1=ALU.add),
                      reads=['posf', 'cs'], writes=['ang'])
                P.add('vector', lambda e: e.tensor_scalar(out=ki[:, 0:n], in0=ang[:, 0:n], scalar1=1.0 / (2 * PI), scalar2=None, op0=ALU.mult),
                      reads=['ang'], writes=['ki'])
                P.add('vector', lambda e: e.tensor_copy(out=kf[:, 0:n], in_=ki[:, 0:n]), reads=['ki'], writes=['kf'])
                P.add('vector', lambda e: e.scalar_tensor_tensor(out=ang[:, 0:n], in0=kf[:, 0:n], scalar=-2 * PI, in1=ang[:, 0:n], op0=ALU.mult, op1=ALU.add),
                      reads=['kf', 'ang'], writes=['ang'])
                P.add('vector', lambda e: e.tensor_scalar(out=ang[:, 0:n], in0=ang[:, 0:n], scalar1=-3.14159, scalar2=3.14159, op0=ALU.max, op1=ALU.min),
                      reads=['ang'], writes=['ang'])
                if which == 'S':
                    P.add('scalar', lambda e, tab=tab: e.activation(out=tab[:, 0:n], in_=ang[:, 0:n], func=AF.Sin, scale=cs[:, 1:2]),
                          reads=['ang', 'cs'], writes=[tkey])
                else:
                    P.add('scalar', lambda e, tab=tab: e.activation(out=tab[:, 0:n], in_=ang[:, 0:n], func=AF.Sin),
                          reads=['ang'], writes=[tkey])

    def next_ps():
        i = cnt['ps'] % 8
        cnt['ps'] += 1
        return i

    def next_ob():
        i = cnt['ob'] % 4
        cnt['ob'] += 1
        return i

    def rope_evac(pi, n, variant, masked, dsts):
        p_ = ps[pi]
        k = cnt['tt'] % 2
        cnt['tt'] += 1
        Ct, Sg = tabC[variant], tabS[variant]
        ck, sk = 'tabC%d' % variant, 'tabS%d' % variant
        P.add('vector', lambda e: e.tensor_tensor(out=t1[k][:, 0:n], in0=p_[:, 0:n], in1=Ct[:, 0:n], op=ALU.mult),
              reads=['ps%d' % pi, ck], writes=['t1_%d' % k])
        P.add('vector', lambda e: e.tensor_tensor(out=t2[k][0:64, 0:n], in0=p_[64:128, 0:n], in1=Sg[0:64, 0:n], op=ALU.mult),
              reads=['ps%d' % pi, sk], writes=['t2_%da' % k])
        P.add('vector', lambda e: e.tensor_tensor(out=t2[k][64:128, 0:n], in0=p_[0:64, 0:n], in1=Sg[64:128, 0:n], op=ALU.mult),
              reads=['ps%d' % pi, sk], writes=['t2_%db' % k])
        if not masked:
            oi = next_ob()
            P.add('gpsimd', lambda e: e.tensor_tensor(out=ob[oi][:, 0:n], in0=t1[k][:, 0:n], in1=t2[k][:, 0:n], op=ALU.add),
                  reads=['t1_%d' % k, 't2_%da' % k, 't2_%db' % k], writes=['ob%d' % oi])
            P.dma(dq(), dsts[0], ob[oi][:, 0:n], reads=['ob%d' % oi])
        else:
            P.add('gpsimd', lambda e: e.tensor_tensor(out=rot[k][:, 0:n], in0=t1[k][:, 0:n], in1=t2[k][:, 0:n], op=ALU.add),
                  reads=['t1_%d' % k, 't2_%da' % k, 't2_%db' % k], writes=['rot%d' % k])
            for m in range(2):
                oi = next_ob()
                P.add('scalar', lambda e, oi=oi, m=m: e.activation(out=ob[oi][:, 0:n], in_=rot[k][:, 0:n], func=AF.Copy, scale=cs[:, 4 + m:5 + m]),
                      reads=['rot%d' % k, 'cs'], writes=['ob%d' % oi])
                P.dma(dq(), dsts[m], ob[oi][:, 0:n], reads=['ob%d' % oi])

    if not layer1:
        xT_v = io['xT'].rearrange("(kc p) t -> p kc t", p=128)
        xTo_v = io['xTo'].rearrange("(kc p) t -> p kc t", p=128)
    else:
        xTg_v = [a.rearrange("(q kc p) t -> q p kc t", q=4, p=128) for a in io['xTg']]
        xTl_v = [a.rearrange("(kc p) t -> p kc t", p=128) for a in io['x2T']]

    for tc in range(8):
        t0 = tc * 512
        if not layer1:
            for h in range(2):
                P.dma(dq(), xs[h], xT_v[:, 8 * h:8 * h + 8, t0:t0 + 512], writes=['xs%d' % h])
                if h == 0:
                    P.add('scalar', lambda e, h=h: e.copy(out=xb[:, 8 * h:8 * h + 8, :], in_=xs[h]), reads=['xs%d' % h], writes=['xb%d' % h])
                else:
                    P.add('gpsimd', lambda e, h=h: e.tensor_copy(out=xb[:, 8 * h:8 * h + 8, :], in_=xs[h]), reads=['xs%d' % h], writes=['xb%d' % h])
        else:
            for q in range(4):
                for k in range(4):
                    P.dma(dq(), xb[:, 4 * k:4 * k + 4, q * 128:(q + 1) * 128], xTg_v[k][q, :, :, tc * 128:(tc + 1) * 128], writes=['xb0', 'xb1'])
        tables(pos[:, t0:t0 + 512], 512)
        oidx = 0
        for ti in range(NQT):
            pi = next_ps()
            for kc in range(16):
                P.add('tensor', lambda e, pi=pi, kc=kc, ti=ti: e.matmul(ps[pi][:, :], lhsT=wq_b[:, kc, ti * 128:(ti + 1) * 128], rhs=xb[:, kc, :], start=(kc == 0), stop=(kc == 15)),
                      reads=['wq_b', 'xb0', 'xb1'], writes=['ps%d' % pi])
            nout = 2 if QT_MASKED[ti] else 1
            dsts = [qt[oidx + m, :, t0:t0 + 512] for m in range(nout)]
            rope_evac(pi, 512, QT_VARIANT[ti], QT_MASKED[ti], dsts)
            oidx += nout
        for tt in range(4):
            pi = next_ps()
            for kc in range(16):
                P.add('tensor', lambda e, pi=pi, kc=kc, tt=tt: e.matmul(ps[pi][:, :], lhsT=xb[:, kc, tt * 128:(tt + 1) * 128], rhs=wv_b[:, kc, :], start=(kc == 0), stop=(kc == 15)),
                      reads=['wv_b', 'xb0', 'xb1'], writes=['ps%d' % pi])
            oi = next_ob()
            P.add('scalar', lambda e, pi=pi, oi=oi: e.copy(out=ob[oi], in_=ps[pi][:, :]), reads=['ps%d' % pi], writes=['ob%d' % oi])
            P.dma(dq(), vt[t0 + tt * 128:t0 + (tt + 1) * 128, :], ob[oi], reads=['ob%d' % oi])

    for ob_i in range(8):
        o0 = ob_i * 128
        if not layer1:
            for h in range(2):
                P.dma(dq(), xs[h][:, :, 0:128], xTo_v[:, 8 * h:8 * h + 8, o0:o0 + 128], writes=['xs%d' % h])
                P.add('gpsimd', lambda e, h=h: e.tensor_copy(out=xob[:, 8 * h:8 * h + 8, :], in_=xs[h][:, :, 0:128]), reads=['xs%d' % h], writes=['xob%d' % h])
        else:
            for k in range(4):
                P.dma(dq(), xob[:, 4 * k:4 * k + 4, :], xTl_v[k][:, :, o0:o0 + 128], writes=['xob0', 'xob1'])
        tables(poso[:, o0:o0 + 128], 128)
        for ti in range(NAO):
            pi = next_ps()
            for kc in range(16):
                P.add('tensor', lambda e, pi=pi, kc=kc, ti=ti: e.matmul(ps[pi][:, 0:128], lhsT=wa_b[:, kc, ti * 128:(ti + 1) * 128], rhs=xob[:, kc, :], start=(kc == 0), stop=(kc == 15)),
                      reads=['wa_b', 'xob0', 'xob1'], writes=['ps%d' % pi])
            if ti < 4:
                dsts = [aq[ti, :, o0:o0 + 128]]
            else:
                pr = ti - 4
                dsts = [iq[2 * pr, :, o0:o0 + 128], iq[2 * pr + 1, :, o0:o0 + 128]]
            rope_evac(pi, 128, AO_VARIANT[ti], AO_MASKED[ti], dsts)
        pi = next_ps()
        for kc in range(16):
            P.add('tensor', lambda e, pi=pi, kc=kc: e.matmul(ps[pi][:, 0:16], lhsT=xob[:, kc, :], rhs=wiw_b[:, kc, :], start=(kc == 0), stop=(kc == 15)),
                  reads=['wiw_b', 'xob0', 'xob1'], writes=['ps%d' % pi])
        P.add('scalar', lambda e, pi=pi: e.copy(out=iwt, in_=ps[pi][:, 0:16]), reads=['ps%d' % pi], writes=['iwt'])
        P.dma(dq(), iw[o0:o0 + 128, :], iwt, reads=['iwt'])


def stage_k2(C, io):
    nc, P = C.nc, C.P
    sb = C.sb
    dq = C.dq
    qt, vt, aq, iq, iw = io['qt'], io['vt'], io['aq'], io['iq'], io['iw']
    o_bd, oa = io['o_bd'], io['oa']
    QA = sb("QA", [128, S], BF16)
    QB = sb("QB", [128, S], BF16)
    KK = sb("KK", [128, S], BF16)
    VV = sb("VV", [128, 32, 129], BF16)
    AQ = sb("AQ", [128, 8, 4, 128], BF16)
    IQ = sb("IQ", [128, 16, 1024], BF16)
    IWS = sb("IWS", [128, 8, 16], F32)
    score = sb("score", [128, S], F32)
    work = sb("work", [128, S], F32)
    selm = sb("selm", [128, S], BF16)
    maskT = sb("maskT", [128, 32, 128], BF16)
    cmask = sb("cmask_s", [128, 4, 512], BF16)
    dmask = sb("dmask_s", [128, 20, 512], BF16)
    negc = sb("negc_s", [128, 512], F32)
    ident = sb("ident_s", [128, 128], BF16)
    dl = sb("dl_s", [128, 256], F32)
    gsc = sb("gsc", [128, 128], F32)
    lc = sb("lc_s", [128, 2], F32)
    sm = sb("sm", [128, 8], F32)
    prod = sb("prod", [128, 64], F32)
    pT = [sb("pT%d" % i, [128, 512], BF16) for i in range(4)]
    rl = [sb("rl%d" % i, [128, 512], F32) for i in range(4)]
    acc = sb("acc", [128, 4, 129], F32)
    rec = sb("rec", [128, 4], F32)
    O1 = sb("O1", [128, 4, 128], F32)
    O2 = sb("O2", [128, 4, 128], F32)
    dd = sb("dd", [128, 4, 128], F32)
    sq = sb("sq", [128, 128], F32)
    ss = sb("ss", [128, 4], F32)
    m8 = sb("m8", [128, 8], F32)
    thr = sb("thr", [128, 1], F32)
    kmean = sb("kmean", [128, 16], F32)
    kmeanb = sb("kmeanb", [128, 16], BF16)
    gate = sb("gate", [128, 32, 16], F32)
    selw = sb("selw", [128, 32, 16], F32)
    g8 = sb("g8", [128, 8], F32)
    gthr = sb("gthr", [128, 1], F32)
    pst = [C.ps[0], C.ps[1], C.ps[6], C.ps[7]]
    pstk = ['pst0', 'pst1', 'px0', 'px1']
    po = [C.ps[2], C.ps[3], C.ps[4], C.ps[5]]
    px = [C.ps[6], C.ps[7]]
    pxi = [C.ps[6], C.ps[7], C.ps[0], C.ps[1]]
    pxik = ['px0', 'px1', 'pst0', 'pst1']
    cnt = {'pst': 0, 'mk': 0, 'px': 0, 'rl': 0}

    P.dma('sync', cmask, io['cmask'][:, :, :], writes=['cmask'])
    P.dma('gpsimd', dmask, io['dmask'][:, :, :], writes=['dmask'])
    P.dma('sync', negc, io['negc'][:, :], writes=['negc'])
    P.dma('sync', ident, io['ident'][:, :], writes=['ident'])
    P.dma('sync', dl, io['dl'].partition_broadcast(128), writes=['dl'])
    P.dma('sync', gsc, io['dg'].partition_broadcast(128), writes=['gsc'])
    P.dma('sync', lc, io['lc'][:, :], writes=['lc'])
    for i in range(2):
        P.add('vector', lambda e, i=i: e.tensor_tensor(out=prod, in0=dl[:, 128 * i:128 * i + 64], in1=dl[:, 128 * i + 64:128 * i + 128], op=ALU.mult),
              reads=['dl'], writes=['prod'])
        P.add('vector', lambda e, i=i: e.reduce_sum(out=sm[:, i:i + 1], in_=prod, axis=AX.X), reads=['prod'], writes=['sm%d' % i])
        P.add('scalar', lambda e, i=i: e.activation(out=sm[:, 2 + i:3 + i], in_=sm[:, i:i + 1], func=AF.Exp), reads=['sm%d' % i], writes=['sm%d' % (2 + i)])
    P.add('vector', lambda e: e.tensor_tensor(out=sm[:, 4:5], in0=sm[:, 2:3], in1=sm[:, 3:4], op=ALU.subtract), reads=['sm2', 'sm3'], writes=['sm4'])
    P.add('vector', lambda e: e.tensor_tensor(out=sm[:, 4:5], in0=sm[:, 4:5], in1=lc[:, 0:1], op=ALU.add), reads=['sm4', 'lc'], writes=['sm4'])
    P.add('vector', lambda e: e.tensor_scalar(out=sm[:, 5:6], in0=sm[:, 4:5], scalar1=-1.0, scalar2=None, op0=ALU.mult), reads=['sm4'], writes=['sm5'])
    P.add('vector', lambda e: e.tensor_scalar(out=gsc, in0=gsc, scalar1=lc[:, 1:2], scalar2=None, op0=ALU.mult), reads=['gsc', 'lc'], writes=['gsc'])

    def load_qt(dst, idx, key):
        P.dma(dq(), dst[:, 0:2048], qt[idx, :, 0:2048], writes=[key])
        P.dma(dq(), dst[:, 2048:4096], qt[idx, :, 2048:4096], writes=[key])

    def load_v(m):
        P.dma(dq(), VV[:, :, 0:128], vt.rearrange("(blk p) c -> p blk c", p=128)[:, :, m * 128:(m + 1) * 128], writes=['VV'])
        P.add('gpsimd', lambda e: e.memset(VV[:, :, 128:129], 1.0), writes=['VVone'])

    def attn_chunk(q_ap, qkeys, blocks, scale, fin):
        P.add('gpsimd', lambda e: e.memset(acc, 0.0), writes=['acc'])
        DEPTH = 3
        n = len(blocks)
        bufs = {}

        def emit_qk(bi):
            blk = blocks[bi]
            i = cnt['pst'] % 4
            cnt['pst'] += 1
            bufs[bi] = i
            P.add('tensor', lambda e, i=i, blk=blk: e.matmul(pst[i][:, :], lhsT=blk['k'], rhs=q_ap, start=True, stop=True),
                  reads=list(qkeys) + list(blk['kkeys']), writes=[pstk[i]])
            P.add('scalar', lambda e, i=i: e.activation(out=pT[i], in_=pst[i][:, :], func=AF.Exp, scale=scale),
                  reads=[pstk[i]], writes=['pT%d' % i])
            if blk.get('mask') is not None:
                for (lo, hi, m_ap, mkeys) in blk['mask']:
                    eng = ('vector', 'gpsimd')[cnt['mk'] % 2]
                    cnt['mk'] += 1
                    P.add(eng, lambda e, i=i, lo=lo, hi=hi, m_ap=m_ap: e.tensor_tensor(out=pT[i][:, lo:hi], in0=pT[i][:, lo:hi], in1=m_ap, op=ALU.mult),
                          reads=['pT%d' % i] + list(mkeys), writes=['pT%d' % i])

        def emit_pv(bi):
            blk = blocks[bi]
            i = bufs[bi]
            for sub in range(4):
                P.add('tensor', lambda e, i=i, sub=sub, blk=blk: e.matmul(po[sub][:, 0:129], lhsT=pT[i][:, sub * 128:(sub + 1) * 128], rhs=blk['v'],
                                                                         start=blk['gs'], stop=blk['ge']),
                      reads=['pT%d' % i, 'VV', 'VVone'], writes=['po%d' % sub])
            if blk['ge']:
                for sub in range(4):
                    w = blk['w'][sub]
                    wkeys = [] if isinstance(w, float) else ['selw']
                    P.add('vector', lambda e, sub=sub, w=w: e.scalar_tensor_tensor(out=acc[:, sub, :], in0=po[sub][:, 0:129], scalar=w, in1=acc[:, sub, :], op0=ALU.mult, op1=ALU.add),
                          reads=['po%d' % sub, 'acc'] + wkeys, writes=['acc'])

        for t_ in range(n + DEPTH):
            if t_ < n:
                emit_qk(t_)
            if t_ - DEPTH >= 0:
                emit_pv(t_ - DEPTH)
        fin()

    def normalize(dst, dkey):
        P.add('vector', lambda e: e.reciprocal(out=rec, in_=acc[:, :, 128]), reads=['acc'], writes=['rec'])
        for sub in range(4):
            P.add('vector', lambda e, sub=sub: e.tensor_scalar(out=dst[:, sub, :], in0=acc[:, sub, 0:128], scalar1=rec[:, sub:sub + 1], scalar2=None, op0=ALU.mult),
                  reads=['acc', 'rec'], writes=[dkey])

    def causal_blocks(tc, kbuf_key):
        blocks = []
        nb = 4 * tc + 4
        for kb in range(nb):
            d = kb - 4 * tc
            mask = None
            if d >= 0:
                mask = [(0, 512, cmask[:, d, :], ['cmask'])]
            blocks.append(dict(k=KK[:, kb * 128:(kb + 1) * 128], kkeys=[kbuf_key], v=VV[:, kb, :], mask=mask,
                               gs=(kb == 0), ge=(kb == nb - 1), w=[1.0] * 4))
        return blocks

    def store_o(tc, m):
        for d_ in range(4):
            R = d_ * 1024 + tc * 128
            P.dma(dq(), o_bd[R // 512][R % 512:R % 512 + 128, m * 128:(m + 1) * 128], O1[:, d_, :], reads=['O1'])

    load_qt(QA, 0, 'QA')
    load_qt(QB, 1, 'QB')
    load_qt(KK, 2, 'KK')
    load_v(0)
    for tc in range(8):
        t0 = tc * 512
        attn_chunk(QA[:, t0:t0 + 512], ['QA'], causal_blocks(tc, 'KK'), 64 ** -0.5, lambda: normalize(O1, 'O1'))
        attn_chunk(QB[:, t0:t0 + 512], ['QB'], causal_blocks(tc, 'KK'), 64 ** -0.5, lambda: normalize(O2, 'O2'))
        P.add('vector', lambda e: e.scalar_tensor_tensor(out=dd, in0=O2, scalar=sm[:, 5:6], in1=O1, op0=ALU.mult, op1=ALU.add),
              reads=['O1', 'O2', 'sm5'], writes=['dd'])
        for sub in range(4):
            P.add('scalar', lambda e, sub=sub: e.activation(out=sq, in_=dd[:, sub, :], func=AF.Square, accum_out=ss[:, sub:sub + 1]),
                  reads=['dd'], writes=['sq', 'ss'])
        P.add('vector', lambda e: e.tensor_scalar(out=ss, in0=ss, scalar1=1.0 / 128, scalar2=1e-5, op0=ALU.mult, op1=ALU.add), reads=['ss'], writes=['ss'])
        P.add('scalar', lambda e: e.activation(out=ss, in_=ss, func=AF.Sqrt), reads=['ss'], writes=['ss'])
        P.add('vector', lambda e: e.reciprocal(out=ss, in_=ss), reads=['ss'], writes=['ss'])
        for sub in range(4):
            P.add('vector', lambda e, sub=sub: e.scalar_tensor_tensor(out=O1[:, sub, :], in0=dd[:, sub, :], scalar=ss[:, sub:sub + 1], in1=gsc, op0=ALU.mult, op1=ALU.mult),
                  reads=['dd', 'ss', 'gsc'], writes=['O1'])
        store_o(tc, 0)

    load_qt(QA, 3, 'QA')
    load_qt(KK, 4, 'KK')
    load_v(1)
    P.add('vector', lambda e: e.tensor_reduce(out=kmean, in_=KK.rearrange("p (n k) -> p n k", k=256), axis=AX.X, op=ALU.add), reads=['KK'], writes=['kmean'])
    P.add('vector', lambda e: e.tensor_scalar(out=kmeanb, in0=kmean, scalar1=1.0 / 256, scalar2=None, op0=ALU.mult), reads=['kmean'], writes=['kmeanb'])
    for qb in range(32):
        pi = cnt['px'] % 2
        cnt['px'] += 1
        own = qb // 2
        P.add('tensor', lambda e, pi=pi, qb=qb: e.matmul(px[pi][:, 0:16], lhsT=QA[:, qb * 128:(qb + 1) * 128], rhs=kmeanb, start=True, stop=True),
              reads=['QA', 'kmeanb'], writes=['px%d' % pi])
        P.add('vector', lambda e, pi=pi, qb=qb: e.tensor_copy(out=gate[:, qb, :], in_=px[pi][:, 0:16]), reads=['px%d' % pi], writes=['gate'])
        P.add('vector', lambda e, qb=qb, own=own: e.memset(gate[:, qb, own:16], NEG), reads=['gate'], writes=['gate'])
        P.add('vector', lambda e, qb=qb: e.max(out=g8, in_=gate[:, qb, :]), reads=['gate'], writes=['g8'])
        P.add('vector', lambda e: e.tensor_scalar(out=gthr, in0=g8[:, 2:3], scalar1=-1.0e29, scalar2=None, op0=ALU.max), reads=['g8'], writes=['gthr'])
        P.add('vector', lambda e, qb=qb: e.tensor_scalar(out=selw[:, qb, :], in0=gate[:, qb, :], scalar1=gthr[:, 0:1], scalar2=None, op0=ALU.is_ge),
              reads=['gate', 'gthr'], writes=['selw'])
    for tc in range(8):
        t0 = tc * 512
        blocks = []
        for n in range(2 * tc + 2):
            for half in range(2):
                kb = 2 * n + half
                d = kb - 4 * tc
                mask = [(0, 512, cmask[:, d, :], ['cmask'])] if d >= 0 else None
                if n < 2 * tc:
                    w = [selw[:, 4 * tc + sub, n:n + 1] for sub in range(4)]
                elif n == 2 * tc:
                    w = [1.0, 1.0, selw[:, 4 * tc + 2, n:n + 1], selw[:, 4 * tc + 3, n:n + 1]]
                else:
                    w = [1.0] * 4
                blocks.append(dict(k=KK[:, kb * 128:(kb + 1) * 128], kkeys=['KK'], v=VV[:, kb, :], mask=mask,
                                   gs=(half == 0), ge=(half == 1), w=w))
        attn_chunk(QA[:, t0:t0 + 512], ['QA'], blocks, 128 ** -0.5, lambda: normalize(O1, 'O1'))
        store_o(tc, 1)

    load_qt(QA, 5, 'QA')
    load_qt(KK, 6, 'KK')
    load_v(2)
    for tc in range(8):
        t0 = tc * 512
        blocks = []
        kbs = [kb for kb in range(32) if -384 <= t0 - kb * 128 <= 2048]
        for ii, kb in enumerate(kbs):
            di = (t0 - kb * 128 + 384) // 128
            blocks.append(dict(k=KK[:, kb * 128:(kb + 1) * 128], kkeys=['KK'], v=VV[:, kb, :], mask=[(0, 512, dmask[:, di, :], ['dmask'])],
                               gs=(ii == 0), ge=(ii == len(kbs) - 1), w=[1.0] * 4))
        attn_chunk(QA[:, t0:t0 + 512], ['QA'], blocks, 128 ** -0.5, lambda: normalize(O1, 'O1'))
        store_o(tc, 2)

    load_qt(KK, 7, 'KK')
    load_qt(QB, 8, 'QB')
    load_v(3)
    P.dma(dq(), AQ, aq.rearrange("h p (ob t) -> p ob h t", t=128), writes=['AQ'])
    for h4 in range(4):
        P.dma(dq(), IQ[:, 4 * h4:4 * h4 + 4, :], iq[4 * h4:4 * h4 + 4].rearrange("h p t -> p h t"), writes=['IQ'])
    P.dma(dq(), IWS, iw.rearrange("(ob p) h -> p ob h", p=128), writes=['IWS'])
    P.add('vector', lambda e: e.tensor_scalar(out=IWS, in0=IWS, scalar1=0.25, scalar2=None, op0=ALU.mult), reads=['IWS'], writes=['IWS'])
    for c in range(8):
        L = 512 * (c + 1)
        for h in range(16):
            for kc in range(c + 1):
                pi = cnt['rl'] % 4
                ri = cnt['rl'] % 4
                cnt['rl'] += 1
                P.add('tensor', lambda e, pi=pi, h=h, kc=kc, c=c: e.matmul(pxi[pi][:, :], lhsT=IQ[:, h, c * 128:(c + 1) * 128], rhs=QB[:, kc * 512:(kc + 1) * 512], start=True, stop=True),
                      reads=['IQ', 'QB'], writes=[pxik[pi]])
                P.add('scalar', lambda e, pi=pi, ri=ri: e.activation(out=rl[ri], in_=pxi[pi][:, :], func=AF.Relu, scale=0.125),
                      reads=[pxik[pi]], writes=['rl%d' % ri])
                sk = 'score%d' % kc
                if h == 0:
                    P.add('vector', lambda e, ri=ri, kc=kc, h=h, c=c: e.tensor_scalar(out=score[:, kc * 512:(kc + 1) * 512], in0=rl[ri], scalar1=IWS[:, c, h:h + 1], scalar2=None, op0=ALU.mult),
                          reads=['rl%d' % ri, 'IWS'], writes=[sk])
                else:
                    P.add('vector', lambda e, ri=ri, kc=kc, h=h, c=c: e.scalar_tensor_tensor(out=score[:, kc * 512:(kc + 1) * 512], in0=rl[ri], scalar=IWS[:, c, h:h + 1], in1=score[:, kc * 512:(kc + 1) * 512], op0=ALU.mult, op1=ALU.add),
                          reads=['rl%d' % ri, 'IWS', sk], writes=[sk])
        P.add('vector', lambda e, c=c: e.tensor_tensor(out=score[:, c * 512:(c + 1) * 512], in0=score[:, c * 512:(c + 1) * 512], in1=negc, op=ALU.add),
              reads=['score%d' % c, 'negc'], writes=['score%d' % c])
        skeys = ['score%d' % kc for kc in range(c + 1)]
        for r in range(32):
            src = score if r == 0 else work
            srck = skeys if r == 0 else ['work']
            P.add('vector', lambda e, src=src, L=L: e.max(out=m8, in_=src[:, 0:L]), reads=srck, writes=['m8'])
            if r < 31:
                P.add('vector', lambda e, src=src, L=L: e.match_replace(out=work[:, 0:L], in_to_replace=m8, in_values=src[:, 0:L], imm_value=-3.0e38),
                      reads=srck + ['m8'], writes=['work'])
        P.add('vector', lambda e: e.tensor_scalar(out=thr, in0=m8[:, 7:8], scalar1=-1.0e29, scalar2=None, op0=ALU.max), reads=['m8'], writes=['thr'])
        P.add('vector', lambda e, L=L: e.tensor_scalar(out=selm[:, 0:L], in0=score[:, 0:L], scalar1=thr[:, 0:1], scalar2=None, op0=ALU.is_ge),
              reads=skeys + ['thr'], writes=['selm'])
        nb = 4 * (c + 1)
        for g4 in range(c + 1):
            pi = cnt['px'] % 2
            cnt['px'] += 1
            for q in range(4):
                kb = 4 * g4 + q
                P.add('tensor', lambda e, pi=pi, q=q, kb=kb: e.matmul(px[pi][:, q * 128:(q + 1) * 128], lhsT=selm[:, kb * 128:(kb + 1) * 128], rhs=ident, start=True, stop=True),
                      reads=['selm', 'ident'], writes=['px%d' % pi])
            P.add('scalar', lambda e, pi=pi, g4=g4: e.copy(out=maskT[:, 4 * g4:4 * g4 + 4, :], in_=px[pi][:, :].rearrange("p (q t) -> p q t", t=128)),
                  reads=['px%d' % pi], writes=['maskT'])
        blocks = []
        for kb in range(nb):
            mask = [(hh * 128, (hh + 1) * 128, maskT[:, kb, :], ['maskT']) for hh in range(4)]
            blocks.append(dict(k=KK[:, kb * 128:(kb + 1) * 128], kkeys=['KK'], v=VV[:, kb, :], mask=mask,
                               gs=(kb == 0), ge=(kb == nb - 1), w=[1.0] * 4))
        attn_chunk(AQ[:, c, :, :], ['AQ'], blocks, 128 ** -0.5, lambda: normalize(O1, 'O1'))
        P.dma(dq(), oa[c * 128:(c + 1) * 128, :].rearrange("p (h d) -> p h d", d=128), O1, reads=['O1'])


def stage_k3(C, io):
    nc, P, ps = C.nc, C.P, C.ps
    sb = C.sb
    dq = C.dq
    og, oa, xres = io['og'], io['oa'], io['xres']
    x1, x1T, gates = io['x1'], io['x1T'], io['gates']
    wob = sb("wob", [128, 16, D], BF16)
    stg = [sb("stg%d" % i, [128, 2048], F32) for i in range(2)]
    G = sb("G", [128, D]); Bt = sb("Bt", [128, D])
    rwf = sb("rwf", [128, 16, 16]); rbt = sb("rbt", [128, 16])
    ident = sb("identf_s", [128, 128])
    oneh = sb("oneh", [128, 4])
    xr = sb("xr", [128, D]); z = sb("z", [128, D]); junk = sb("junk", [128, D]); xo = sb("xo", [128, D])
    mixt = sb("mixt", [128, D])
    cand = [sb("cand%d" % i, [128, 4, 384]) for i in range(4)]
    mixb = sb("mixb", [128, 16, 128], BF16)
    st6 = sb("st6", [128, 8])
    xTf = sb("xTf", [128, 16, 128]); xTb = sb("xTb", [128, 16, 128], BF16)
    aff = sb("aff", [128, 16]); sel = sb("sel", [128, 16]); tmp = sb("tmp", [128, 16]); eq = sb("eq", [128, 16])
    m1 = sb("m1", [128, 4]); m2 = sb("m2", [128, 4]); gs = sb("gs", [128, 4]); gm = sb("gm", [128, 1]); oh = sb("oh", [128, 4])
    msk = sb("msk", [128, 16]); gsum = sb("gsum", [128, 1]); gout = sb("gout", [128, 16])
    cnt = {'ps': 0, 'stg': 0}

    def nps():
        i = cnt['ps'] % 8
        cnt['ps'] += 1
        return i
    P.dma('sync', G, io['lng'].partition_broadcast(128), writes=['G'])
    P.dma('sync', Bt, io['lnb'].partition_broadcast(128), writes=['Bt'])
    P.dma('sync', rwf, io['rw'].rearrange("(kc p) e -> p kc e", p=128), writes=['rwf'])
    P.dma('sync', rbt, io['rb'].partition_broadcast(128), writes=['rbt'])
    P.dma('sync', ident, io['identf'][:, :], writes=['ident'])
    P.dma('sync', oneh, io['oneh'][:, :], writes=['oneh'])
    wo_v = io['wo'].rearrange("(kc p) c -> p kc c", p=128)
    for kc in range(16):
        i = cnt['stg'] % 2
        cnt['stg'] += 1
        P.dma(dq(), stg[i], wo_v[:, kc, :], writes=['stg%d' % i])
        eng = ('vector', 'scalar')[i]
        P.add(eng, lambda e, i=i, kc=kc: (e.tensor_copy if i == 0 else e.copy)(out=wob[:, kc, :], in_=stg[i]), reads=['stg%d' % i], writes=['wob'])
    og4 = [a.rearrange("(q r) c -> q r c", q=4) for a in og]
    for tt in range(8):
        r0 = tt * 128
        P.dma(dq(), xr, xres[r0:r0 + 128, :], writes=['xr'])
        P.dma(dq(), mixt[:, 0:512], oa[r0:r0 + 128, :], writes=['mixA'])
        for d in range(4):
            R = d * 1024 + r0
            P.dma(dq(), cand[d], og4[R // 512][:, R % 512:R % 512 + 128, :].rearrange("q p c -> p q c"), writes=['cand%d' % d])
        for d in range(4):
            for m in range(3):
                mv = mixt[:, 512 + m * 512:512 + (m + 1) * 512].rearrange("p (q c) -> p q c", q=4)
                cv = cand[d][:, :, m * 128:(m + 1) * 128]
                if d == 0:
                    P.add('vector', lambda e, cv=cv, mv=mv: e.tensor_scalar(out=mv, in0=cv, scalar1=oneh[:, 0:1], scalar2=None, op0=ALU.mult),
                          reads=['cand0', 'oneh'], writes=['mixB%d' % m])
                else:
                    P.add('vector', lambda e, cv=cv, mv=mv, d=d: e.scalar_tensor_tensor(out=mv, in0=cv, scalar=oneh[:, d:d + 1], in1=mv, op0=ALU.mult, op1=ALU.add),
                          reads=['cand%d' % d, 'oneh', 'mixB%d' % m], writes=['mixB%d' % m])
        for g4 in range(4):
            pi = nps()
            for q in range(4):
                kc = 4 * g4 + q
                P.add('tensor', lambda e, pi=pi, q=q, kc=kc: e.matmul(ps[pi][:, q * 128:(q + 1) * 128], lhsT=mixt[:, kc * 128:(kc + 1) * 128], rhs=ident, start=True, stop=True),
                      reads=['mixA', 'mixB0', 'mixB1', 'mixB2', 'ident'], writes=['ps%d' % pi])
            P.add('scalar', lambda e, pi=pi, g4=g4: e.copy(out=mixb[:, 4 * g4:4 * g4 + 4, :], in_=ps[pi][:, :].rearrange("p (q t) -> p q t", t=128)), reads=['ps%d' % pi], writes=['mixb'])
        for dc in range(4):
            pi = nps()
            for kc in range(16):
                P.add('tensor', lambda e, pi=pi, kc=kc, dc=dc: e.matmul(ps[pi][:, :], lhsT=mixb[:, kc, :], rhs=wob[:, kc, dc * 512:(dc + 1) * 512], start=(kc == 0), stop=(kc == 15)),
                      reads=['mixb', 'wob'], writes=['ps%d' % pi])
            P.add('vector', lambda e, pi=pi, dc=dc: e.scalar_tensor_tensor(out=z[:, dc * 512:(dc + 1) * 512], in0=xr[:, dc * 512:(dc + 1) * 512], scalar=ALPHA, in1=ps[pi][:, :], op0=ALU.mult, op1=ALU.add),
                  reads=['xr', 'ps%d' % pi], writes=['z'])
        _ln_ops(P, z, 'z', xo, 'xo', G, Bt, junk, st6, 'a')
        P.dma(dq(), x1[r0:r0 + 128, :], xo, reads=['xo'])
        for g4 in range(4):
            pi = nps()
            for q in range(4):
                kc = 4 * g4 + q
                P.add('tensor', lambda e, pi=pi, q=q, kc=kc: e.matmul(ps[pi][:, q * 128:(q + 1) * 128], lhsT=xo[:, kc * 128:(kc + 1) * 128], rhs=ident, start=True, stop=True),
                      reads=['xo', 'ident'], writes=['ps%d' % pi])
            P.add('scalar', lambda e, pi=pi, g4=g4: e.copy(out=xTf[:, 4 * g4:4 * g4 + 4, :], in_=ps[pi][:, :].rearrange("p (q t) -> p q t", t=128)), reads=['ps%d' % pi], writes=['xTf'])
        P.add('gpsimd', lambda e: e.tensor_copy(out=xTb, in_=xTf), reads=['xTf'], writes=['xTb'])
        for k in range(4):
            P.dma(dq(), x1T[k].rearrange("(kc p) t -> p kc t", p=128)[:, :, r0:r0 + 128], xTb[:, 4 * k:4 * k + 4, :], reads=['xTb'])
        pi = nps()
        for kc in range(16):
            P.add('tensor', lambda e, pi=pi, kc=kc: e.matmul(ps[pi][:, 0:16], lhsT=xTf[:, kc, :], rhs=rwf[:, kc, :], start=(kc == 0), stop=(kc == 15)),
                  reads=['xTf', 'rwf'], writes=['ps%d' % pi])
        P.add('scalar', lambda e, pi=pi: e.activation(out=aff, in_=ps[pi][:, 0:16], func=AF.Sigmoid), reads=['ps%d' % pi], writes=['aff'])
        P.add('vector', lambda e: e.tensor_tensor(out=sel, in0=aff, in1=rbt, op=ALU.add), reads=['aff', 'rbt'], writes=['sel'])
        P.add('vector', lambda e: e.tensor_reduce(out=m1, in_=sel.rearrange("p (g l) -> p g l", l=4), axis=AX.X, op=ALU.max), reads=['sel'], writes=['m1'])
        for g in range(4):
            P.add('vector', lambda e, g=g: e.tensor_scalar(out=eq[:, 4 * g:4 * g + 4], in0=sel[:, 4 * g:4 * g + 4], scalar1=m1[:, g:g + 1], scalar2=-1.0e9, op0=ALU.is_equal, op1=ALU.mult),
                  reads=['sel', 'm1'], writes=['eq'])
        P.add('vector', lambda e: e.tensor_tensor(out=tmp, in0=sel, in1=eq, op=ALU.add), reads=['sel', 'eq'], writes=['tmp'])
        P.add('vector', lambda e: e.tensor_reduce(out=m2, in_=tmp.rearrange("p (g l) -> p g l", l=4), axis=AX.X, op=ALU.max), reads=['tmp'], writes=['m2'])
        P.add('vector', lambda e: e.tensor_tensor(out=gs, in0=m1, in1=m2, op=ALU.add), reads=['m1', 'm2'], writes=['gs'])
        P.add('vector', lambda e: e.tensor_reduce(out=gm, in_=gs, axis=AX.X, op=ALU.max), reads=['gs'], writes=['gm'])
        P.add('vector', lambda e: e.tensor_scalar(out=oh, in0=gs, scalar1=gm[:, 0:1], scalar2=None, op0=ALU.is_equal), reads=['gs', 'gm'], writes=['oh'])
        for g in range(4):
            P.add('vector', lambda e, g=g: e.tensor_scalar(out=msk[:, 4 * g:4 * g + 4], in0=sel[:, 4 * g:4 * g + 4], scalar1=m2[:, g:g + 1], scalar2=oh[:, g:g + 1], op0=ALU.is_ge, op1=ALU.mult),
                  reads=['sel', 'm2', 'oh'], writes=['msk'])
        P.add('vector', lambda e: e.tensor_tensor(out=gout, in0=aff, in1=msk, op=ALU.mult), reads=['aff', 'msk'], writes=['gout'])
        P.add('vector', lambda e: e.reduce_sum(out=gsum, in_=gout, axis=AX.X), reads=['gout'], writes=['gsum'])
        P.add('vector', lambda e: e.reciprocal(out=gsum, in_=gsum), reads=['gsum'], writes=['gsum'])
        P.add('vector', lambda e: e.tensor_scalar(out=gout, in0=gout, scalar1=gsum[:, 0:1], scalar2=None, op0=ALU.mult), reads=['gout', 'gsum'], writes=['gout'])
        P.dma(dq(), gates[r0:r0 + 128, :], gout, reads=['gout'])


def stage_k4(C, io):
    nc, P, ps = C.nc, C.P, C.ps
    sb = C.sb
    dq = C.dq
    x1Tg, gg, wg, wu, wd, part = io['x1Tg'], io['gg'], io['wg'], io['wu'], io['wd'], io['part']
    wgb = sb("wgb", [128, 16, 1024], BF16)
    wub = sb("wub", [128, 16, 1024], BF16)
    wdb = sb("wdb", [128, 8, D], BF16)
    stg = [sb("stg%d" % i, [128, 2048], F32) for i in range(3)]
    xc = [sb("xc%d" % i, [128, 16, 512], BF16) for i in range(2)]
    hT = sb("hT", [128, 8, 512], BF16)
    prev = [sb("prev%d" % i, [128, D]) for i in range(2)]
    yt = [sb("yt%d" % i, [128, D]) for i in range(2)]
    gall = sb("gall", [128, 32, 16])
    gt = sb("gt", [128, 32, 4])
    oneh = sb("oneh", [128, 4])
    sg = [sb("sg%d" % i, [128, 512], F32) for i in range(2)]
    cnt = {'ps': 0, 'stg': 0, 'sg': 0, 'cv': 0, 'yb': 0}
    cv_eng = ('scalar', 'vector', 'scalar', 'vector', 'gpsimd')

    def nps():
        i = cnt['ps'] % 8
        cnt['ps'] += 1
        return i

    def conv(dst_ap, dkey, src_ap, is3d):
        i = cnt['stg'] % 3
        cnt['stg'] += 1
        sview = stg[i].rearrange("p (a b) -> p a b", b=128) if is3d else stg[i]
        P.dma(dq(), sview, src_ap, writes=['stg%d' % i])
        eng = cv_eng[cnt['cv'] % 5]
        cnt['cv'] += 1
        if eng == 'scalar':
            P.add('scalar', lambda e: e.copy(out=dst_ap, in_=sview), reads=['stg%d' % i], writes=[dkey])
        else:
            P.add(eng, lambda e: e.tensor_copy(out=dst_ap, in_=sview), reads=['stg%d' % i], writes=[dkey])

    P.dma('sync', oneh, io['oneh'][:, :], writes=['oneh'])
    P.dma('sync', gall, gg.rearrange("(tt p) e -> p tt e", p=128), writes=['gall'])
    gv = gall.rearrange("p t (g l) -> p t g l", g=4)
    for g in range(4):
        if g == 0:
            P.add('vector', lambda e: e.tensor_scalar(out=gt, in0=gv[:, :, 0, :], scalar1=oneh[:, 0:1], scalar2=None, op0=ALU.mult), reads=['gall', 'oneh'], writes=['gt'])
        else:
            P.add('vector', lambda e, g=g: e.scalar_tensor_tensor(out=gt, in0=gv[:, :, g, :], scalar=oneh[:, g:g + 1], in1=gt, op0=ALU.mult, op1=ALU.add),
                  reads=['gall', 'oneh', 'gt'], writes=['gt'])
    x_v = [a.rearrange("(q kc p) t -> q p kc t", q=4, p=128) for a in x1Tg]
    def jobs_gu(ex):
        wg_v = wg[ex].rearrange("(kc p) f -> p kc f", p=128)
        wu_v = wu[ex].rearrange("(kc p) f -> p kc f", p=128)
        for f in range(8):
            conv(wgb[:, :, f * 128:(f + 1) * 128], 'wgb', wg_v[:, :, f * 128:(f + 1) * 128], True)
            conv(wub[:, :, f * 128:(f + 1) * 128], 'wub', wu_v[:, :, f * 128:(f + 1) * 128], True)

    def jobs_d(ex):
        wd_v = wd[ex].rearrange("(fc p) d -> p fc d", p=128)
        for f in range(8):
            conv(wdb[:, f, :], 'wdb', wd_v[:, f, :], False)

    jobs_gu(0)
    jobs_d(0)
    for ex in range(4):
        for c8 in range(8):
            ch, half = c8 // 2, c8 % 2
            xi = c8 % 2
            for k in range(4):
                P.dma(dq(), xc[xi][:, 4 * k:4 * k + 4, :], x_v[k][ch, :, :, half * 512:(half + 1) * 512], writes=['xc%d' % xi])
            for f in range(8):
                pg = nps()
                for kc in range(16):
                    P.add('tensor', lambda e, pg=pg, kc=kc, f=f, xi=xi: e.matmul(ps[pg][:, :], lhsT=wgb[:, kc, f * 128:(f + 1) * 128], rhs=xc[xi][:, kc, :], start=(kc == 0), stop=(kc == 15)),
                          reads=['wgb', 'xc%d' % xi], writes=['ps%d' % pg])
                pu = nps()
                for kc in range(16):
                    P.add('tensor', lambda e, pu=pu, kc=kc, f=f, xi=xi: e.matmul(ps[pu][:, :], lhsT=wub[:, kc, f * 128:(f + 1) * 128], rhs=xc[xi][:, kc, :], start=(kc == 0), stop=(kc == 15)),
                          reads=['wub', 'xc%d' % xi], writes=['ps%d' % pu])
                si = cnt['sg'] % 2
                cnt['sg'] += 1
                P.add('scalar', lambda e, pg=pg, si=si: e.activation(out=sg[si], in_=ps[pg][:, :], func=AF.Silu), reads=['ps%d' % pg], writes=['sg%d' % si])
                P.add('vector', lambda e, pu=pu, si=si, f=f: e.tensor_tensor(out=hT[:, f, :], in0=sg[si], in1=ps[pu][:, :], op=ALU.mult),
                      reads=['sg%d' % si, 'ps%d' % pu], writes=['hT'])
            if c8 == 7 and ex < 3:
                jobs_gu(ex + 1)
            for tt in range(4):
                T = ch * 8 + half * 4 + tt
                row = ch * 512 + tt * 128
                yb = cnt['yb'] % 2
                cnt['yb'] += 1
                pkey = 'part_%d_%d' % (c8, tt)
                if ex > 0:
                    P.dma(dq(), prev[yb], part[half][row:row + 128, :], reads=[pkey], writes=['prev%d' % yb])
                for dc in range(4):
                    pi = nps()
                    for f in range(8):
                        P.add('tensor', lambda e, pi=pi, f=f, tt=tt, dc=dc: e.matmul(ps[pi][:, :], lhsT=hT[:, f, tt * 128:(tt + 1) * 128], rhs=wdb[:, f, dc * 512:(dc + 1) * 512], start=(f == 0), stop=(f == 7)),
                              reads=['hT', 'wdb'], writes=['ps%d' % pi])
                    gsc = gt[:, T, ex:ex + 1]
                    if ex == 0:
                        P.add('vector', lambda e, pi=pi, dc=dc, gsc=gsc, yb=yb: e.tensor_scalar(out=yt[yb][:, dc * 512:(dc + 1) * 512], in0=ps[pi][:, :], scalar1=gsc, scalar2=None, op0=ALU.mult),
                              reads=['ps%d' % pi, 'gt'], writes=['yt%d' % yb])
                    else:
                        P.add('vector', lambda e, pi=pi, dc=dc, gsc=gsc, yb=yb: e.scalar_tensor_tensor(out=yt[yb][:, dc * 512:(dc + 1) * 512], in0=ps[pi][:, :], scalar=gsc, in1=prev[yb][:, dc * 512:(dc + 1) * 512], op0=ALU.mult, op1=ALU.add),
                              reads=['ps%d' % pi, 'gt', 'prev%d' % yb], writes=['yt%d' % yb])
                P.dma(dq(), part[half][row:row + 128, :], yt[yb], reads=['yt%d' % yb], writes=[pkey])
            if c8 == 7 and ex < 3:
                jobs_d(ex + 1)


def stage_k5(C, io, last):
    nc, P, ps = C.nc, C.P, C.ps
    sb = C.sb
    dq = C.dq
    G = sb("G", [128, D]); Bt = sb("Bt", [128, D])
    xr = sb("xr", [128, D]); z = sb("z", [128, D]); junk = sb("junk", [128, D]); xo = sb("xo", [128, D])
    pt = sb("pt", [128, D])
    st6 = sb("st6", [128, 8])
    ident = sb("identf_s", [128, 128])
    xTb = sb("xTb", [128, 16, 128], BF16)
    cnt = {'ps': 0}

    def nps():
        i = cnt['ps'] % 8
        cnt['ps'] += 1
        return i
    P.dma('sync', G, io['lng'].partition_broadcast(128), writes=['G'])
    P.dma('sync', Bt, io['lnb'].partition_broadcast(128), writes=['Bt'])
    P.dma('sync', ident, io['identf'][:, :], writes=['ident'])
    for tt in range(8):
        r0 = tt * 128
        P.dma(dq(), xr, io['x1'][r0:r0 + 128, :], writes=['xr'])
        P.dma(dq(), pt, io['rs'][tt // 4][(tt % 4) * 128:(tt % 4) * 128 + 128, :], writes=['pt'])
        P.add('vector', lambda e: e.scalar_tensor_tensor(out=z, in0=xr, scalar=ALPHA, in1=pt, op0=ALU.mult, op1=ALU.add), reads=['xr', 'pt'], writes=['z'])
        _ln_ops(P, z, 'z', xo, 'xo', G, Bt, junk, st6, 'a')
        P.dma(dq(), io['x2'][r0:r0 + 128, :], xo, reads=['xo'])
        if not last:
            for g4 in range(4):
                pi = nps()
                for q in range(4):
                    kc = 4 * g4 + q
                    P.add('tensor', lambda e, pi=pi, q=q, kc=kc: e.matmul(ps[pi][:, q * 128:(q + 1) * 128], lhsT=xo[:, kc * 128:(kc + 1) * 128], rhs=ident, start=True, stop=True),
                          reads=['xo', 'ident'], writes=['ps%d' % pi])
                P.add('scalar', lambda e, pi=pi, g4=g4: e.copy(out=xTb[:, 4 * g4:4 * g4 + 4, :], in_=ps[pi][:, :].rearrange("p (q t) -> p q t", t=128)), reads=['ps%d' % pi], writes=['xTb'])
            for k in range(4):
                P.dma(dq(), io['x2T'][k].rearrange("(kc p) t -> p kc t", p=128)[:, :, r0:r0 + 128], xTb[:, 4 * k:4 * k + 4, :], reads=['xTb'])


def build_fused(nlayers=2, stop=99):
    nc = bass.Bass("TRN2", target_bir_lowering=False)
    shapes = {
        'xT': ([D, S], F32), 'xTo': ([D, 1024], F32), 'xown': ([1024, D], F32),
        'pos': ([1, S], I32), 'poso': ([1, 1024], I32), 'cst': ([128, 8], F32),
        'cmask': ([128, 4, 512], BF16), 'dmask': ([128, 20, 512], BF16), 'negc': ([128, 512], F32),
        'ident': ([128, 128], BF16), 'identf': ([128, 128], F32), 'oneh': ([128, 4], F32),
        'rw': ([D, 16], F32), 'rb': ([1, 16], F32),
    }
    for l in range(nlayers):
        shapes.update({
            'wq%d' % l: ([D, NQT * 128], F32), 'wv%d' % l: ([D, 512], F32), 'wa%d' % l: ([D, NAO * 128], F32), 'wiw%d' % l: ([D, 16], F32),
            'dl%d' % l: ([1, 256], F32), 'dg%d' % l: ([1, 128], F32), 'lc%d' % l: ([128, 2], F32),
            'wo%d' % l: ([D, D], F32), 'lng%d' % l: ([1, D], F32), 'lnb%d' % l: ([1, D], F32),
            'wg%d' % l: ([4, D, 1024], F32), 'wu%d' % l: ([4, D, 1024], F32), 'wd%d' % l: ([4, 1024, D], F32),
            'lng2_%d' % l: ([1, D], F32), 'lnb2_%d' % l: ([1, D], F32)})

    class Lazy(dict):
        def __missing__(self, k):
            shp, dt = shapes[k]
            v = nc.dram_tensor(k, shp, dt, kind="ExternalInput").ap()
            self[k] = v
            return v
    ein = Lazy()

    def scr(name, shape, dt=F32):
        return nc.dram_tensor(name, shape, dt).ap()
    out = nc.dram_tensor("out", [1024, D], F32, kind="ExternalOutput").ap()
    s_qt = scr("s_qt", [9, 128, S], BF16); s_vt = scr("s_vt", [S, 512], BF16)
    s_aq = scr("s_aq", [4, 128, 1024], BF16); s_iq = scr("s_iq", [16, 128, 1024], BF16); s_iw = scr("s_iw", [1024, 16])
    s_obd = [scr("s_obd%d" % k, [512, 384]) for k in range(8)]; s_oa = scr("s_oa", [1024, 512]); s_og = [scr("s_og%d" % k, [2048, 384]) for k in range(8)]
    s_x1 = scr("s_x1", [1024, D]); s_x1T = [scr("s_x1T%d" % k, [512, 1024], BF16) for k in range(4)]; s_x1Tg = [scr("s_x1Tg%d" % k, [2048, 1024], BF16) for k in range(4)]
    s_gates = scr("s_gates", [1024, 16]); s_gg = scr("s_gg", [4 * 1024, 16])
    s_part = [scr("s_part%d" % k, [2048, D]) for k in range(2)]; s_rs = [scr("s_rs%d" % k, [512, D]) for k in range(2)]
    s_x2 = scr("s_x2", [1024, D]); s_x2T = [scr("s_x2T%d" % k, [512, 1024], BF16) for k in range(4)]; s_xTg = [scr("s_xTg%d" % k, [2048, 1024], BF16) for k in range(4)]

    with ExitStack() as st:
        C = Ctx(nc, st)
        stage = 0

        def go():
            nonlocal stage
            stage += 1
            return stage <= stop
        for l in range(nlayers):
            last = (l == nlayers - 1)
            if go():
                io1 = {'wq': ein['wq%d' % l], 'wv': ein['wv%d' % l], 'wa': ein['wa%d' % l], 'wiw': ein['wiw%d' % l],
                       'pos': ein['pos'], 'poso': ein['poso'], 'cst': ein['cst'],
                       'qt': s_qt, 'vt': s_vt, 'aq': s_aq, 'iq': s_iq, 'iw': s_iw, 'xTg': s_xTg, 'x2T': s_x2T}
                if l == 0:
                    io1['xT'] = ein['xT']; io1['xTo'] = ein['xTo']
                stage_k1(C, io1, layer1=(l > 0))
                C.barrier()
            if go():
                io2 = {'qt': s_qt, 'vt': s_vt, 'aq': s_aq, 'iq': s_iq, 'iw': s_iw, 'o_bd': s_obd, 'oa': s_oa,
                       'cmask': ein['cmask'], 'dmask': ein['dmask'], 'negc': ein['negc'], 'ident': ein['ident'],
                       'dl': ein['dl%d' % l], 'dg': ein['dg%d' % l], 'lc': ein['lc%d' % l]}
                stage_k2(C, io2)
            if go():
                C.collectives([("AllGather", ALU.bypass, s_obd[k], s_og[k]) for k in range(8)])
            if go():
                io3 = {'og': s_og, 'oa': s_oa, 'xres': ein['xown'] if l == 0 else s_x2, 'x1': s_x1, 'x1T': s_x1T, 'gates': s_gates,
                       'wo': ein['wo%d' % l], 'lng': ein['lng%d' % l], 'lnb': ein['lnb%d' % l], 'rw': ein['rw'], 'rb': ein['rb'],
                       'identf': ein['identf'], 'oneh': ein['oneh']}
                stage_k3(C, io3)
            if go():
                C.collectives([("AllGather", ALU.bypass, s_x1T[k], s_x1Tg[k]) for k in range(4)] + [("AllGather", ALU.bypass, s_gates, s_gg)])
            if go():
                io4 = {'x1Tg': s_x1Tg, 'gg': s_gg, 'wg': ein['wg%d' % l], 'wu': ein['wu%d' % l], 'wd': ein['wd%d' % l], 'part': s_part, 'oneh': ein['oneh']}
                stage_k4(C, io4)
            if go():
                C.collectives([("ReduceScatter", ALU.add, s_part[k], s_rs[k]) for k in range(2)])
            if go():
                io5 = {'x1': s_x1, 'rs': s_rs, 'x2': out if last else s_x2, 'x2T': s_x2T, 'lng': ein['lng2_%d' % l], 'lnb': ein['lnb2_%d' % l], 'identf': ein['identf']}
                stage_k5(C, io5, last)
                if not last:
                    C.collectives([("AllGather", ALU.bypass, s_x2T[k], s_xTg[k]) for k in range(4)])
        C.P.emit(st)
    nc._ext_names = list(ein.keys())
    return nc


_NC = {}


def kernel(x, positions, w_in, w_out, diff_lambda, diff_norm_g, ln_mix_g, ln_mix_b,
           router_w, router_bias, w_gate, w_up, w_down, ln_ffn_g, ln_ffn_b):
    import math
    x = np.asarray(x, np.float32)
    positions = np.asarray(positions)
    Bn, Sn, Dn = x.shape
    nl = int(np.asarray(w_in).shape[0])
    if 'nc' not in _NC:
        _NC['nc'] = build_fused(nl)
    nc = _NC['nc']
    f32 = lambda a: np.ascontiguousarray(np.asarray(a, np.float32))
    identf = np.eye(128, dtype=np.float32)
    in_maps = []
    for c in range(8):
        b, j = c // 4, c % 4
        own = own_tokens(j)
        xT = np.ascontiguousarray(x[b].T)
        k2c = k2_consts(j, np.zeros(256, np.float32), np.zeros(128, np.float32), 0)
        oneh = np.zeros((128, 4), np.float32)
        oneh[:, j] = 1.0
        m = {'xT': xT, 'xTo': np.ascontiguousarray(xT[:, own]), 'xown': np.ascontiguousarray(x[b][own]),
             'pos': np.ascontiguousarray(positions[b][None, :].astype(np.int32)),
             'poso': np.ascontiguousarray(positions[b][None, own].astype(np.int32)),
             'cst': k1_consts(), 'cmask': k2c['cmask'], 'dmask': k2c['dmask'], 'negc': k2c['negc'], 'ident': k2c['ident'],
             'identf': identf, 'oneh': oneh, 'rw': f32(router_w), 'rb': f32(np.asarray(router_bias)[None, :])}
        for l in range(nl):
            wl = np.asarray(w_in[l], np.float32)
            qcols, vcols, acols, iwcols = k1_cols(j)
            m['wq%d' % l] = np.ascontiguousarray(wl[:, qcols]); m['wv%d' % l] = np.ascontiguousarray(wl[:, vcols])
            m['wa%d' % l] = np.ascontiguousarray(wl[:, acols]); m['wiw%d' % l] = np.ascontiguousarray(wl[:, iwcols])
            lam_init = 0.8 - 0.6 * math.exp(-0.3 * l)
            lc = np.zeros((128, 2), np.float32); lc[:, 0] = lam_init; lc[:, 1] = 1.0 - lam_init
            m['dl%d' % l] = f32(np.asarray(diff_lambda[l]).reshape(1, 256)); m['dg%d' % l] = f32(np.asarray(diff_norm_g[l]).reshape(1, 128)); m['lc%d' % l] = lc
            m['wo%d' % l] = f32(w_out[l]); m['lng%d' % l] = f32(np.asarray(ln_mix_g[l])[None, :]); m['lnb%d' % l] = f32(np.asarray(ln_mix_b[l])[None, :])
            m['wg%d' % l] = f32(w_gate[l][4 * j:4 * j + 4]); m['wu%d' % l] = f32(w_up[l][4 * j:4 * j + 4]); m['wd%d' % l] = f32(w_down[l][4 * j:4 * j + 4])
            m['lng2_%d' % l] = f32(np.asarray(ln_ffn_g[l])[None, :]); m['lnb2_%d' % l] = f32(np.asarray(ln_ffn_b[l])[None, :])
        in_maps.append(m)
    res = run_bass_kernel_spmd(nc, in_maps, core_ids=list(range(8)))
    out = np.zeros((Bn, Sn, Dn), np.float32)
    for c in range(8):
        b, j = c // 4, c % 4
        out[b, own_tokens(j)] = np.asarray(res.results[c]['out'])
    return out
```
